# Optimizing a Trainium2 kernel written in Bass

```python
import jax, jax.numpy as jnp
from jax import lax
import numpy as np

D_MODEL = 1024
BATCH = 32
SEQ = 2048
DEPTH = 1

SB_HEADS = 8
SB_HEAD_DIM = 128
SB_WIDTH = SB_HEADS * SB_HEAD_DIM
SB_BLOCK = 128
GDN_HEADS = 8
GDN_HEAD_DIM = 128
GDN_WIDTH = GDN_HEADS * GDN_HEAD_DIM
GDN_CONV = 4
GDN_CHUNK = 64
PEER_HEADS = 8
PEER_N_KEYS = 128
PEER_N_EXPERTS = PEER_N_KEYS * PEER_N_KEYS
PEER_D_KEY = 256
PEER_HALF = PEER_D_KEY // 2
PEER_TOPK = 16
PEER_TOKEN_BLOCK = 128
IN_WIDTH = 3 * SB_WIDTH + 3 * GDN_WIDTH + GDN_WIDTH + 2 * GDN_HEADS + 2 * D_MODEL
EPS = 1e-6

kernel_name = "hybrid_stickbreak_gdn_peer_block"


def rms_norm(x, w):
    xf = x.astype(jnp.float32)
    y = xf * lax.rsqrt(jnp.mean(xf * xf, axis=-1, keepdims=True) + EPS)
    return (y * w.astype(jnp.float32)).astype(x.dtype)


def l2_normalize(x):
    xf = x.astype(jnp.float32)
    return xf * lax.rsqrt(jnp.sum(xf * xf, axis=-1, keepdims=True) + EPS)


def stick_breaking_attention(q, k, v):
    S, Dh = q.shape[2], q.shape[3]
    scale = Dh ** -0.5
    qf, kf, vf = q.astype(jnp.float32), k.astype(jnp.float32), v.astype(jnp.float32)
    outs = []
    for blk in range(S // SB_BLOCK):
        t0 = blk * SB_BLOCK
        t1 = t0 + SB_BLOCK
        z = jnp.einsum('bhqd,bhkd->bhqk', qf[:, :, t0:t1], kf[:, :, :t1]) * scale
        t_idx = t0 + jnp.arange(SB_BLOCK)[:, None]
        s_idx = jnp.arange(t1)[None, :]
        causal = s_idx < t_idx
        log_1mb = jnp.where(causal, jax.nn.log_sigmoid(-z), 0.0)
        suffix = lax.cumsum(log_1mb, axis=3, reverse=True) - log_1mb
        log_a = jax.nn.log_sigmoid(z) + suffix
        a = jnp.where(causal, jnp.exp(log_a), 0.0)
        outs.append(jnp.einsum('bhqk,bhkd->bhqd', a, vf[:, :, :t1]))
    return jnp.concatenate(outs, axis=2).astype(q.dtype)


def causal_depthwise_conv(x, w):
    K = w.shape[0]
    S = x.shape[1]
    xp = jnp.pad(x, ((0, 0), (K - 1, 0), (0, 0)))
    return sum(xp[:, i:i + S] * w[i] for i in range(K))


def gated_delta_rule_chunked(q, k, v, g, beta):
    B, H, S, Dk = q.shape
    Dv = v.shape[-1]
    C = GDN_CHUNK
    n = S // C
    q = q * (Dk ** -0.5)
    qc = q.reshape(B, H, n, C, Dk)
    kc = k.reshape(B, H, n, C, Dk)
    vc = v.reshape(B, H, n, C, Dv)
    gc = jnp.cumsum(g.reshape(B, H, n, C), axis=-1)
    bc = beta.reshape(B, H, n, C)
    k_beta = kc * bc[..., None]
    v_beta = vc * bc[..., None]
    tril = jnp.tril(jnp.ones((C, C), dtype=bool))
    strict = jnp.tril(jnp.ones((C, C), dtype=bool), -1)
    gdiff = gc[..., :, None] - gc[..., None, :]
    decay = jnp.where(tril, jnp.exp(jnp.where(tril, gdiff, 0.0)), 0.0)
    L = jnp.where(strict, jnp.einsum('bhnid,bhnjd->bhnij', k_beta, kc) * decay, 0.0)
    eye = jnp.eye(C, dtype=L.dtype)
    T = lax.linalg.triangular_solve(eye + L, jnp.broadcast_to(eye, L.shape),
                                    left_side=True, lower=True, unit_diagonal=True)
    v_corr = jnp.einsum('bhnij,bhnjd->bhnid', T, v_beta)
    k_cumdecay = jnp.einsum('bhnij,bhnjd->bhnid', T, k_beta * jnp.exp(gc)[..., None])
    attn_intra = jnp.where(tril, jnp.einsum('bhnid,bhnjd->bhnij', qc, kc) * decay, 0.0)

    def step(state, inp):
        q_i, k_i, v_i, kcd_i, g_i, a_i = inp
        v_new = v_i - jnp.einsum('bhcd,bhde->bhce', kcd_i, state)
        o = (jnp.einsum('bhcd,bhde->bhce', q_i * jnp.exp(g_i)[..., None], state)
             + jnp.einsum('bhij,bhje->bhie', a_i, v_new))
        g_last = g_i[..., -1]
        k_w = k_i * jnp.exp(g_last[..., None] - g_i)[..., None]
        state = state * jnp.exp(g_last)[..., None, None] + jnp.einsum('bhcd,bhce->bhde', k_w, v_new)
        return state, o

    to_front = lambda t: jnp.moveaxis(t, 2, 0)
    xs = (to_front(qc), to_front(kc), to_front(v_corr), to_front(k_cumdecay), to_front(gc), to_front(attn_intra))
    state0 = jnp.zeros((B, H, Dk, Dv), dtype=jnp.float32)
    _, o = lax.scan(step, state0, xs)
    return jnp.moveaxis(o, 0, 2).reshape(B, H, S, Dv)


def peer_ffn(h, w_q, keys1, keys2, u_tab, v_tab):
    B, S, D = h.shape
    q = jnp.einsum('bsd,de->bse', h, w_q).astype(jnp.float32).reshape(B, S, PEER_HEADS, PEER_D_KEY)
    s1 = jnp.einsum('bshd,hnd->bshn', q[..., :PEER_HALF], keys1.astype(jnp.float32))
    s2 = jnp.einsum('bshd,hnd->bshn', q[..., PEER_HALF:], keys2.astype(jnp.float32))
    v1, i1 = lax.top_k(s1, PEER_TOPK)
    v2, i2 = lax.top_k(s2, PEER_TOPK)
    cand = (v1[..., :, None] + v2[..., None, :]).reshape(B, S, PEER_HEADS, PEER_TOPK * PEER_TOPK)
    vals, c = lax.top_k(cand, PEER_TOPK)
    e1 = jnp.take_along_axis(i1, c // PEER_TOPK, axis=-1)
    e2 = jnp.take_along_axis(i2, c % PEER_TOPK, axis=-1)
    expert = e1 * PEER_N_KEYS + e2
    gate = jax.nn.softmax(vals, axis=-1)
    n_tok = B * S
    nb = n_tok // PEER_TOKEN_BLOCK
    hk = PEER_HEADS * PEER_TOPK
    hx = h.reshape(nb, PEER_TOKEN_BLOCK, D)
    ex = expert.reshape(nb, PEER_TOKEN_BLOCK, hk)
    gx = gate.reshape(nb, PEER_TOKEN_BLOCK, hk).astype(h.dtype)

    def block(args):
        hb, eb, gb = args
        u = u_tab[eb]
        act = jax.nn.gelu(jnp.einsum('td,ted->te', hb, u), approximate=False)
        v = v_tab[eb]
        return jnp.einsum('te,ted->td', gb * act, v)

    out = lax.map(block, (hx, ex, gx))
    return out.reshape(B, S, D).astype(h.dtype)


def setup_inputs(seed: int = 0) -> dict:
    key = jax.random.key(seed)
    ks = jax.random.split(key, 20)
    f32 = jnp.float32
    nrm = lambda k, shape, scale: jax.random.normal(k, shape, f32) * scale
    gain = lambda k, shape: 1.0 + 0.02 * jax.random.normal(k, shape, f32)
    dt = jnp.exp(jax.random.uniform(ks[7], (DEPTH, GDN_HEADS), f32, np.log(1e-3), np.log(1e-1)))
    return {
        "x": jax.random.normal(ks[0], (BATCH, SEQ, D_MODEL), f32),
        "mix_norm_w": gain(ks[1], (DEPTH, D_MODEL)),
        "w_in": nrm(ks[2], (DEPTH, D_MODEL, IN_WIDTH), D_MODEL ** -0.5),
        "sb_q_norm_w": gain(ks[3], (DEPTH, SB_HEAD_DIM)),
        "sb_k_norm_w": gain(ks[4], (DEPTH, SB_HEAD_DIM)),
        "gdn_conv_w": nrm(ks[5], (DEPTH, GDN_CONV, 3 * GDN_WIDTH), GDN_CONV ** -0.5),
        "gdn_a_log": jnp.log(jax.random.uniform(ks[6], (DEPTH, GDN_HEADS), f32, 1.0, 16.0)),
        "gdn_dt_bias": dt + jnp.log(-jnp.expm1(-dt)),
        "gdn_out_norm_w": gain(ks[8], (DEPTH, GDN_HEAD_DIM)),
        "w_branch_sb": nrm(ks[9], (DEPTH, SB_WIDTH, D_MODEL), SB_WIDTH ** -0.5),
        "w_branch_gdn": nrm(ks[10], (DEPTH, GDN_WIDTH, D_MODEL), GDN_WIDTH ** -0.5),
        "w_out": nrm(ks[11], (DEPTH, D_MODEL, D_MODEL), D_MODEL ** -0.5),
        "ffn_norm_w": gain(ks[12], (DEPTH, D_MODEL)),
        "peer_w_q": nrm(ks[13], (DEPTH, D_MODEL, PEER_HEADS * PEER_D_KEY), D_MODEL ** -0.5),
        "peer_keys1": nrm(ks[14], (DEPTH, PEER_HEADS, PEER_N_KEYS, PEER_HALF), PEER_HALF ** -0.5),
        "peer_keys2": nrm(ks[15], (DEPTH, PEER_HEADS, PEER_N_KEYS, PEER_HALF), PEER_HALF ** -0.5),
        "peer_u": nrm(ks[16], (DEPTH, PEER_N_EXPERTS, D_MODEL), D_MODEL ** -0.5),
        "peer_v": nrm(ks[17], (DEPTH, PEER_N_EXPERTS, D_MODEL), PEER_TOPK ** -0.5),
    }


def reference(x, mix_norm_w, w_in, sb_q_norm_w, sb_k_norm_w, gdn_conv_w, gdn_a_log, gdn_dt_bias,
              gdn_out_norm_w, w_branch_sb, w_branch_gdn, w_out, ffn_norm_w, peer_w_q, peer_keys1,
              peer_keys2, peer_u, peer_v):
    B, S, D = x.shape
    f32 = jnp.float32
    sizes = [3 * SB_WIDTH, 3 * GDN_WIDTH, GDN_WIDTH, GDN_HEADS, GDN_HEADS, D_MODEL, D_MODEL]
    offsets = np.cumsum(sizes)[:-1].tolist()
    for l in range(DEPTH):
        h = rms_norm(x, mix_norm_w[l])
        proj = jnp.einsum('bsd,de->bse', h, w_in[l])
        sb_qkv, gdn_qkv, gdn_z, gdn_b, gdn_a, gate_sb, gate_gdn = jnp.split(proj, offsets, axis=-1)

        heads_sb = lambda t: t.reshape(B, S, SB_HEADS, SB_HEAD_DIM).transpose(0, 2, 1, 3)
        q, k, v = [heads_sb(t) for t in jnp.split(sb_qkv, 3, axis=-1)]
        q = rms_norm(q, sb_q_norm_w[l])
        k = rms_norm(k, sb_k_norm_w[l])
        o_sb = stick_breaking_attention(q, k, v).transpose(0, 2, 1, 3).reshape(B, S, SB_WIDTH)

        qkv = jax.nn.silu(causal_depthwise_conv(gdn_qkv, gdn_conv_w[l]))
        heads_gdn = lambda t: t.reshape(B, S, GDN_HEADS, GDN_HEAD_DIM).transpose(0, 2, 1, 3)
        gq, gk, gv = [heads_gdn(t) for t in jnp.split(qkv, 3, axis=-1)]
        gq = l2_normalize(gq)
        gk = l2_normalize(gk)
        gv = gv.astype(f32)
        beta = jax.nn.sigmoid(gdn_b.astype(f32)).transpose(0, 2, 1)
        g = -(jnp.exp(gdn_a_log[l].astype(f32))
              * jax.nn.softplus(gdn_a.astype(f32) + gdn_dt_bias[l].astype(f32))).transpose(0, 2, 1)
        o_gdn = gated_delta_rule_chunked(gq, gk, gv, g, beta).transpose(0, 2, 1, 3)
        z = gdn_z.reshape(B, S, GDN_HEADS, GDN_HEAD_DIM).astype(f32)
        o_gdn = (rms_norm(o_gdn, gdn_out_norm_w[l]) * jax.nn.silu(z)).reshape(B, S, GDN_WIDTH).astype(x.dtype)

        y_sb = jnp.einsum('bse,ed->bsd', o_sb, w_branch_sb[l])
        y_gdn = jnp.einsum('bse,ed->bsd', o_gdn, w_branch_gdn[l])
        merged = jax.nn.sigmoid(gate_sb) * y_sb + jax.nn.sigmoid(gate_gdn) * y_gdn
        x = x + jnp.einsum('bsd,de->bse', merged, w_out[l])

        h2 = rms_norm(x, ffn_norm_w[l])
        x = x + peer_ffn(h2, peer_w_q[l], peer_keys1[l], peer_keys2[l], peer_u[l], peer_v[l])
    return x
```

```python
import numpy as np
from contextlib import ExitStack
import concourse.bass as bass
import concourse.mybir as mybir
from concourse.bass_utils import run_bass_kernel_spmd

F32 = mybir.dt.float32
F32R = mybir.dt.float32r
BF16 = mybir.dt.bfloat16
I32 = mybir.dt.int32
U32 = mybir.dt.uint32
AF = mybir.ActivationFunctionType
ALU = mybir.AluOpType
AX = mybir.AxisListType

D = 1024
NH = 8
DH = 128
IN_W = 9232
EPS = 1e-6
OFF_SBQ, OFF_SBK, OFF_SBV = 0, 1024, 2048
OFF_GQ, OFF_GK, OFF_GV = 3072, 4096, 5120
OFF_GZ = 6144
OFF_GB = 7168
OFF_GA = 7176
OFF_GSB = 7184
OFF_GGDN = 8208
NEXP = 16384
NEG = -30000.0


class Tok:
    __slots__ = ("w", "r", "sc", "excl")

    def __init__(self, excl=False):
        self.w = None
        self.r = {}
        self.sc = None
        self.excl = excl


class SemCtr:
    LIMIT = 30000

    def __init__(self, prog, name):
        self.prog = prog
        self.name = name
        self.n = 0
        self.sem = None
        self.k = 0

    def next(self, inc):
        if self.sem is None or self.n + inc > self.LIMIT:
            self.sem = self.prog.es.enter_context(self.prog.nc.semaphore(f"{self.name}_{self.k}"))
            self.prog.nsem += 1
            self.k += 1
            self.n = 0
            self.prog.allsems.append(self)
        self.n += inc
        return (self.sem, self.n, self.name)

    def cur(self):
        if self.sem is None or self.n == 0:
            return None
        return (self.sem, self.n, self.name)


class Prog:
    def __init__(self, nc, es):
        self.nc = nc
        self.es = es
        self.engs = {"pe": nc.tensor, "act": nc.scalar, "dve": nc.vector, "pool": nc.gpsimd, "sp": nc.sync}
        self.nsem = 0
        self.allsems = []
        self.ctr = {k: SemCtr(self, "c" + k) for k in self.engs}
        self.seen = {k: {} for k in self.engs}
        self.ninst = 0
        self.dsems = []
        self.dead = False
        self.defq = None

    def _wait(self, e, tk):
        sem, val, _ = tk
        d = self.seen[e]
        key = id(sem)
        if d.get(key, 0) >= val:
            return
        d[key] = val
        self.engs[e].wait_ge(sem, val)
        self.ninst += 1

    def _deps(self, e, reads, writes):
        own = "c" + e
        for t in reads:
            if t.w is not None:
                if not (e == "pe" and t.w[2] == own):
                    self._wait(e, t.w)
            if t.excl:
                for tk in t.r.values():
                    if tk[2] != own:
                        self._wait(e, tk)
        for t in writes:
            if t.w is not None:
                if not (e == "pe" and t.w[2] == own):
                    self._wait(e, t.w)
            for tk in t.r.values():
                if tk[2] == own and e == "pe":
                    continue
                self._wait(e, tk)

    def _record(self, tk, reads, writes):
        for t in reads:
            t.r[id(tk[0])] = tk
        for t in writes:
            t.w = tk
            t.r = {}

    def op(self, e, fn, reads=(), writes=()):
        if self.dead:
            return None
        if self.defq is not None:
            self.defq.append(lambda: self.op(e, fn, reads, writes))
            return None
        self._deps(e, reads, writes)
        inst = fn(self.engs[e])
        tk = self.ctr[e].next(1)
        inst.then_inc(tk[0], 1)
        self.ninst += 1
        self._record(tk, reads, writes)
        return tk

    def dma(self, q, out, in_, reads=(), writes=(), sc=None, fn=None):
        if self.dead:
            return None
        if self.defq is not None:
            self.defq.append(lambda: self.dma(q, out, in_, reads, writes, sc, fn))
            return None
        self._deps(q, reads, writes)
        if sc is None:
            t0 = writes[0] if writes else reads[0]
            if t0.sc is None:
                t0.sc = SemCtr(self, "d%d" % len(self.dsems))
                self.dsems.append(t0.sc)
            sc = t0.sc
        if fn is None:
            inst = self.engs[q].dma_start(out=out, in_=in_)
        else:
            inst = fn(self.engs[q])
        tk = sc.next(16)
        inst.then_inc(tk[0], 16)
        self.ninst += 1
        self._record(tk, reads, writes)
        return tk

    def barrier(self, engines=None):
        toks = []
        seen = set()
        for sc in self.allsems:
            c = sc.cur()
            if c is not None and id(c[0]) not in seen:
                seen.add(id(c[0]))
                toks.append(c)
        for e in engines or self.engs:
            for tk in toks:
                if tk[2] == "c" + e:
                    continue
                self._wait(e, tk)


class Ctx:
    pass


def peer_phase(nc, P, tk, sb, ps, psb, pst, pstr, t_pstr, ident_b, ident_f, t_const, TOK,
               x1_d, out_d, ffnw_d, wqb_d, keysT_d, pu_d, pv_d, mm, act, acopy, vcopy, tt, ts, stt, NTT=None, dbg=None):
    NTT = NTT or TOK // 128
    NU = 20
    ND = 4
    G = 4
    with ExitStack() as ph:
        wq = sb("wq", [128, 8, 2048], BF16, ph)
        keysT = sb("keysT_s", [128, 16 * 128], F32, ph)
        ffnw_b = sb("ffnw_b", [128, D], F32, ph)
        iota_i = sb("iota_i", [128, 16], I32, ph)
        iota16 = sb("iota16", [128, 16], F32, ph)
        P.dma("sp", wq[:], wqb_d.rearrange("(c p) n -> p c n", p=128), writes=[tk["wq"]])
        P.dma("sp", keysT[:], keysT_d[:, :], writes=[tk["keysT"]])
        P.dma("sp", ffnw_b[:], ffnw_d[0:1, :].partition_broadcast(128), writes=[tk["ffnw"]])
        P.op("pool", lambda g: g.iota(iota_i[:], pattern=[[1, 16]], base=0, channel_multiplier=0), writes=[tk["iota"]])
        vcopy("pool", iota16[:], iota_i[:], [tk["iota"]], [tk["iota"]])

        acc = [sb(f"acc{i}", [128, D], F32, ph) for i in range(2)]
        h2 = sb("h2", [128, D], F32, ph)
        h2b = [sb(f"h2b{i}", [128, D], BF16, ph) for i in range(2)]
        junkb = sb("junkb", [128, D], BF16, ph)
        junkc = sb("junkc", [128, D], BF16, ph)
        prod = [sb(f"prod{i}", [128, D], BF16, ph) for i in range(3)]
        diag = [sb(f"diag{i}", [128, 128], BF16, ph) for i in range(ND)]
        h2T = sb("h2T", [128, 8, 128], BF16, ph)
        junk = sb("pjunk", [128, D], F32, ph)
        pss = sb("pss", [128, 1], F32, ph)
        qTs = sb("qTs", [128, 16 * 128], F32, ph)
        sc = sb("sc", [128, 16 * 128], F32, ph)
        sc2 = sb("sc2", [128, 128], F32, ph)
        v16 = sb("v16", [128, 256], F32, ph)
        i16u = sb("i16u", [128, 256], U32, ph)
        i16f = sb("i16f", [128, 256], F32, ph)
        cand = sb("cand", [128, 256], F32, ph)
        cand2 = sb("cand2", [128, 256], F32, ph)
        oh = sb("oh", [128, 256], F32, ph)
        c16 = sb("c16", [128, 16], F32, ph)
        ci16u = sb("ci16u", [128, 16], U32, ph)
        hlu = sb("hlu", [128, 32], U32, ph)
        hlf = sb("hlf", [128, 32], F32, ph)
        e12 = sb("e12", [128, 32], F32, ph)
        e16 = sb("e16", [128, 16], F32, ph)
        sml = sb("sml", [128, 4], F32, ph)
        c16a = sb("c16a", [128, 128], F32, ph)
        e128 = sb("e128", [128, 128], F32, ph)
        sm8 = sb("sm8", [128, 16], F32, ph)
        gate = [sb(f"gate{i}", [128, 128], F32, ph) for i in range(2)]
        esel = sb("esel", [128, 128], F32, ph)
        eidx = [sb(f"eidx{i}", [128, 128], I32, ph) for i in range(2)]
        pre = sb("pre", [128, 128], F32, ph)
        coef = sb("coef", [128, 128], F32, ph)
        ubuf = [sb(f"ubuf{i}", [128, 2 * D], BF16, ph) for i in range(NU)]
        banks_a = [0, 1]
        banks_b = [2, 3, 4]
        sidx = [0, 0]

        def bc3(tile, row, off, inner_first):
            if inner_first:
                return bass.AP(tile, off, [[row, 128], [1, 16], [0, 16]])
            return bass.AP(tile, off, [[row, 128], [0, 16], [1, 16]])

        breg = nc.gpsimd.alloc_register("bchk")
        nc.gpsimd.reg_mov(breg, NEXP - 1)
        gi = [0]

        def sel(tile_i):
            a = tile_i % 2
            r0 = tile_i * 128
            A = acc[a]
            P.dma("sp", A[:], x1_d[r0:r0 + 128, :], writes=[tk["acc", a]])
            P.op("dve", lambda v: v.scalar_tensor_tensor(out=junk[:], in0=A[:], scalar=1.0, in1=A[:], op0=ALU.mult, op1=ALU.mult,
                                                         accum_out=pss[:]), reads=[tk["acc", a]], writes=[tk["pjunk"], tk["pss"]])
            act(pss[:], pss[:], AF.Ln, [tk["pss"]], [tk["pss"]], scale=1.0 / D, bias=EPS)
            act(pss[:], pss[:], AF.Exp, [tk["pss"]], [tk["pss"]], scale=-0.5)
            stt("dve", h2[:], A[:], pss[:, 0:1], ffnw_b[:], ALU.mult, ALU.mult, [tk["acc", a], tk["pss"], tk["ffnw"]], [tk["h2"]])
            vcopy("pool", h2b[a][:], h2[:], [tk["h2"]], [tk["h2b", a]])
            for c in range(8):
                P.op("pe", lambda pe, c=c: pe.transpose(out=pstr[:, c, :], in_=h2b[a][:, c * 128:(c + 1) * 128], identity=ident_b[:]),
                     reads=[tk["h2b", a], t_const], writes=[t_pstr])
            vcopy("dve", h2T[:], pstr[:, :, :], [t_pstr], [tk["h2T"]])
            for jg in range(4):
                bk = banks_a[sidx[0] % len(banks_a)]
                sidx[0] += 1
                for jj in range(4):
                    j = jg * 4 + jj
                    for c in range(8):
                        mm(psb[bk][:, jj * 128:(jj + 1) * 128], wq[:, c, j * 128:(j + 1) * 128], h2T[:, c, :],
                           [tk["wq"], tk["h2T"]], pst[bk], start=(c == 0), stop=(c == 7))
                acopy(qTs[:, jg * 512:(jg + 1) * 512], psb[bk][:], [pst[bk]], [tk["qTs", jg]])
            for jg in range(4):
                bk = banks_b[sidx[1] % len(banks_b)]
                sidx[1] += 1
                for jj in range(4):
                    j = jg * 4 + jj
                    mm(psb[bk][:, jj * 128:(jj + 1) * 128], qTs[:, j * 128:(j + 1) * 128], keysT[:, j * 128:(j + 1) * 128],
                       [tk["qTs", jg], tk["keysT"]], pst[bk])
                acopy(sc[:, jg * 512:(jg + 1) * 512], psb[bk][:], [pst[bk]], [tk["sc", jg]])
            for j in range(16):
                scj = sc[:, j * 128:(j + 1) * 128]
                va = v16[:, j * 16:j * 16 + 8]
                vb_ = v16[:, j * 16 + 8:j * 16 + 16]
                P.op("dve", lambda v, va=va, scj=scj: v.max(out=va, in_=scj), reads=[tk["sc", j // 4]], writes=[tk["v16"]])
                P.op("dve", lambda v, va=va, scj=scj, j=j: v.max_index(out=i16u[:, j * 16:j * 16 + 8], in_max=va, in_values=scj),
                     reads=[tk["sc", j // 4], tk["v16"]], writes=[tk["i16u"]])
                P.op("dve", lambda v, va=va, scj=scj: v.match_replace(out=sc2[:], in_to_replace=va, in_values=scj, imm_value=-1e30),
                     reads=[tk["sc", j // 4], tk["v16"]], writes=[tk["sc2"]])
                P.op("dve", lambda v, vb_=vb_: v.max(out=vb_, in_=sc2[:]), reads=[tk["sc2"]], writes=[tk["v16"]])
                P.op("dve", lambda v, vb_=vb_, j=j: v.max_index(out=i16u[:, j * 16 + 8:j * 16 + 16], in_max=vb_, in_values=sc2[:]),
                     reads=[tk["sc2"], tk["v16"]], writes=[tk["i16u"]])
            vcopy("dve", i16f[:], i16u[:], [tk["i16u"]], [tk["i16f"]])
            for h in range(8):
                o1_, o2_ = (2 * h) * 16, (2 * h + 1) * 16
                c3 = cand[:].rearrange("p (i j) -> p i j", j=16)
                tt("dve", c3, bc3(v16, 256, o1_, True), bc3(v16, 256, o2_, False), ALU.add, [tk["v16"]], [tk["cand"]])
                P.op("dve", lambda v: v.max(out=c16[:, 0:8], in_=cand[:]), reads=[tk["cand"]], writes=[tk["c16"]])
                P.op("dve", lambda v: v.max_index(out=ci16u[:, 0:8], in_max=c16[:, 0:8], in_values=cand[:]),
                     reads=[tk["cand"], tk["c16"]], writes=[tk["ci16u"]])
                P.op("dve", lambda v: v.match_replace(out=cand2[:], in_to_replace=c16[:, 0:8], in_values=cand[:], imm_value=-1e30),
                     reads=[tk["cand"], tk["c16"]], writes=[tk["cand2"]])
                P.op("dve", lambda v: v.max(out=c16[:, 8:16], in_=cand2[:]), reads=[tk["cand2"]], writes=[tk["c16"]])
                P.op("dve", lambda v: v.max_index(out=ci16u[:, 8:16], in_max=c16[:, 8:16], in_values=cand2[:]),
                     reads=[tk["cand2"], tk["c16"]], writes=[tk["ci16u"]])
                vcopy("dve", c16a[:, h * 16:(h + 1) * 16], c16[:], [tk["c16"]], [tk["c16a"]])
                P.op("dve", lambda v: v.tensor_single_scalar(out=hlu[:, 0:16], in_=ci16u[:], scalar=4, op=ALU.logical_shift_right),
                     reads=[tk["ci16u"]], writes=[tk["hlu"]])
                P.op("dve", lambda v: v.tensor_single_scalar(out=hlu[:, 16:32], in_=ci16u[:], scalar=15, op=ALU.bitwise_and),
                     reads=[tk["ci16u"]], writes=[tk["hlu"]])
                vcopy("dve", hlf[:], hlu[:], [tk["hlu"]], [tk["hlf"]])
                o3 = oh[:].rearrange("p (k i) -> p k i", i=16)
                for half, off in ((0, o1_), (1, o2_)):
                    tt("dve", o3, bc3(iota16, 16, 0, False), bc3(hlf, 32, half * 16, True), ALU.is_equal,
                       [tk["iota"], tk["hlf"]], [tk["oh"]])
                    tt("dve", o3, o3, bc3(i16f, 256, off, False), ALU.mult, [tk["oh"], tk["i16f"]], [tk["oh"]])
                    P.op("dve", lambda v, half=half: v.tensor_reduce(out=e12[:, half * 16:(half + 1) * 16], in_=o3, axis=AX.X, op=ALU.add),
                         reads=[tk["oh"]], writes=[tk["e12"]])
                stt("dve", esel[:, h * 16:(h + 1) * 16], e12[:, 0:16], 128.0, e12[:, 16:32], ALU.mult, ALU.add,
                    [tk["e12"]], [tk["esel"]])
            c3a = c16a[:].rearrange("p (h k) -> p h k", k=16)
            e3a = e128[:].rearrange("p (h k) -> p h k", k=16)
            tt("dve", e3a, c3a, bass.AP(c16a, 0, [[128, 128], [16, 8], [0, 16]]), ALU.subtract, [tk["c16a"]], [tk["e128"]])
            act(e128[:], e128[:], AF.Exp, [tk["e128"]], [tk["e128"]])
            P.op("dve", lambda v: v.tensor_reduce(out=sm8[:, 0:8], in_=e3a, axis=AX.X, op=ALU.add), reads=[tk["e128"]], writes=[tk["sm8"]])
            P.op("dve", lambda v: v.reciprocal(out=sm8[:, 8:16], in_=sm8[:, 0:8]), reads=[tk["sm8"]], writes=[tk["sm8"]])
            tt("dve", gate[a][:].rearrange("p (h k) -> p h k", k=16), e3a, bass.AP(sm8, 8, [[16, 128], [1, 8], [0, 16]]), ALU.mult,
               [tk["e128"], tk["sm8"]], [tk["gate", a]])
            ei = eidx[a]
            vcopy("dve", ei[:], esel[:], [tk["esel"]], [tk["eidx", a]])
            if dbg is not None and tile_i == 0:
                P.dma("sp", dbg["esel"][:, :], esel[:], reads=[tk["esel"]])
                P.dma("sp", dbg["gate"][:, :], gate[a][:], reads=[tk["gate", a]])
                P.dma("sp", dbg["v16"][:, :], v16[:], reads=[tk["v16"]])
                P.dma("sp", dbg["i16f"][:, :], i16f[:], reads=[tk["i16f"]])

        def record(tile_i):
            P.defq = []
            sel(tile_i)
            q = P.defq
            P.defq = None
            return q

        def drain(q, n):
            while q and n > 0:
                q.pop(0)()
                n -= 1

        drain(record(0), 1 << 30)
        for tile_i in range(NTT):
            a = tile_i % 2
            r0 = tile_i * 128
            A = acc[a]
            ei = eidx[a]
            nq = record(tile_i + 1) if tile_i + 1 < NTT else []
            if dbg is not None and dbg.get("nogather"):
                continue
            slot_buf = {}

            def group_math(grp):
                cols = slice(grp * G, (grp + 1) * G)
                act(coef[:, cols], pre[:, cols], AF.Gelu, [tk["pre", grp]], [tk["coef", grp]])
                tt("dve", coef[:, cols], coef[:, cols], gate[a][:, cols], ALU.mult, [tk["coef", grp], tk["gate", a]], [tk["coef", grp]])
                for s in range(grp * G, (grp + 1) * G):
                    if dbg is not None and dbg.get("nodot"):
                        break
                    k = s % ND
                    ub = slot_buf[s]
                    act(diag[k][:], ident_b[:], AF.Identity, [tk["coef", grp], t_const], [tk["diag", k]], scale=coef[:, s:s + 1])
                    for n in range(2):
                        mm(psb[5 + n][:], diag[k][:], ubuf[ub][:, D + n * 512:D + (n + 1) * 512], [tk["diag", k], tk["ubuf", ub]],
                           pst[5 + n], start=(s == 0), stop=(s == 127))

            for grp in range(128 // G):
                for s in range(grp * G, (grp + 1) * G):
                    ub = gi[0] % NU
                    gi[0] += 1
                    slot_buf[s] = ub
                    P.dma("pool", None, None, reads=[tk["eidx", a]], writes=[tk["ubuf", ub]],
                          fn=lambda g, ub=ub, s=s, ei=ei: g.indirect_dma_start(
                              out=ubuf[ub][:], out_offset=None, in_=pu_d[:, :],
                              in_offset=bass.IndirectOffsetOnAxis(ap=ei[:, s:s + 1], axis=0),
                              bounds_check=breg, oob_is_err=False))
                    if dbg is not None and dbg.get("nodot"):
                        vcopy("dve", pre[:, s:s + 1], ubuf[ub][:, 0:1], [tk["ubuf", ub]], [tk["pre", grp]])
                    elif s % 4 != 3:
                        pk = s % 3
                        tt("dve", prod[pk][:], ubuf[ub][:, 0:D], h2b[a][:], ALU.mult, [tk["ubuf", ub], tk["h2b", a]], [tk["prod", pk]])
                        act(junkb[:], prod[pk][:], AF.Identity, [tk["prod", pk]], [tk["junkb"], tk["pre", grp]], accum_out=pre[:, s:s + 1])
                    else:
                        P.op("dve", lambda v, ub=ub, s=s, a=a: v.scalar_tensor_tensor(
                            out=junkc[:], in0=ubuf[ub][:, 0:D], scalar=1.0, in1=h2b[a][:], op0=ALU.mult, op1=ALU.mult,
                            accum_out=pre[:, s:s + 1]), reads=[tk["ubuf", ub], tk["h2b", a]], writes=[tk["junkc"], tk["pre", grp]])
                    drain(nq, 3)
                if grp > 0:
                    group_math(grp - 1)
            group_math(128 // G - 1)
            for n in range(2):
                tt("dve", A[:, n * 512:(n + 1) * 512], A[:, n * 512:(n + 1) * 512], psb[5 + n][:], ALU.add,
                   [tk["acc", a], pst[5 + n]], [tk["acc", a]])
            P.dma("sp", out_d[r0:r0 + 128, :], A[:], reads=[tk["acc", a]])
            drain(nq, 1 << 30)
        P.barrier()


class _Stop(Exception):
    pass


def build(NB=4, S=2048, stage="full", skip=(), stop=0):
    nc = bass.Bass("TRN2", target_bir_lowering=False)
    NT = S // 128
    NG = S // 512
    TOK = NB * S
    from collections import defaultdict

    def din(name, shape, dt=F32):
        return nc.dram_tensor(name, shape, dt, kind="ExternalInput").ap()

    x_d = din("x", [TOK, D])
    mixw_d = din("mix_norm_w", [1, D])
    ffnw_d = din("ffn_norm_w", [1, D])
    win_d = din("w_in", [D, IN_W])
    sbqw_d = din("sb_q_norm_w", [DH, 1])
    sbkw_d = din("sb_k_norm_w", [DH, 1])
    convw_d = din("convw", [128, 96])
    alog_d = din("a_log_rep", [1, NT * 8])
    dtb_d = din("dtb_rep", [1, NT * 8])
    gnw_d = din("gdn_out_norm_w", [1, DH])
    wbsb_d = din("w_branch_sb", [D, D])
    wbgdn_d = din("w_branch_gdn", [D, D])
    wout_d = din("w_out", [D, D])
    wq_d = din("peer_w_q", [D, 2048])
    keysT_d = din("keysT", [128, 16 * 128])
    pu_d = din("peer_u", [NEXP, D])
    pv_d = din("peer_v", [NEXP, D])
    if stage in ("sb", "gdn"):
        dbg_d = nc.dram_tensor("dbg", [NB, D, S], F32, kind="ExternalOutput").ap()
    else:
        out_d = nc.dram_tensor("out", [TOK, D], F32, kind="ExternalOutput").ap()

    winb_d = nc.dram_tensor("w_in_b", [D, IN_W], BF16, kind="Internal").ap()
    wbsbb_d = nc.dram_tensor("wbsb_b", [D, D], BF16, kind="Internal").ap()
    wbgdnb_d = nc.dram_tensor("wbgdn_b", [D, D], BF16, kind="Internal").ap()
    woutb_d = nc.dram_tensor("wout_b", [D, D], BF16, kind="Internal").ap()
    wqb_d = nc.dram_tensor("wq_b", [D, 2048], BF16, kind="Internal").ap()
    x1_d = nc.dram_tensor("x1_s", [TOK, D], F32, kind="Internal").ap()
    puvb_d = nc.dram_tensor("puv_b", [NEXP, 2 * D], BF16, kind="Internal").ap()

    es = ExitStack()
    with es:
        P = Prog(nc, es)
        tk = defaultdict(Tok)

        uid = [0]

        def sb(name, shape, dt, st=es):
            uid[0] += 1
            return st.enter_context(nc.sbuf_tensor(f"{name}_{uid[0]}", shape, dt))

        def ps(name, shape, dt=F32, st=es):
            return st.enter_context(nc.psum_tensor(name, shape, dt))

        def mm(out, lhsT, rhs, reads, wtok, start=True, stop=True):
            P.op("pe", lambda pe: pe.matmul(out, lhsT=lhsT, rhs=rhs, start=start, stop=stop),
                 reads=reads, writes=[wtok])

        def act(out, in_, func, reads, writes, **kw):
            P.op("act", lambda a: a.activation(out=out, in_=in_, func=func, **kw), reads=reads, writes=writes)

        def acopy(out, in_, reads, writes):
            P.op("act", lambda a: a.copy(out=out, in_=in_), reads=reads, writes=writes)

        def vcopy(e, out, in_, reads, writes):
            P.op(e, lambda v: v.tensor_copy(out=out, in_=in_), reads=reads, writes=writes)

        def tt(e, out, in0, in1, op, reads, writes):
            P.op(e, lambda v: v.tensor_tensor(out=out, in0=in0, in1=in1, op=op), reads=reads, writes=writes)

        def ts(e, out, in0, s1, s2, op0, op1, reads, writes):
            if s2 is None:
                P.op(e, lambda v: v.tensor_scalar(out=out, in0=in0, scalar1=s1, scalar2=None, op0=op0),
                     reads=reads, writes=writes)
            else:
                P.op(e, lambda v: v.tensor_scalar(out=out, in0=in0, scalar1=s1, scalar2=s2, op0=op0, op1=op1),
                     reads=reads, writes=writes)

        def stt(e, out, in0, scalar, in1, op0, op1, reads, writes):
            P.op(e, lambda v: v.scalar_tensor_tensor(out=out, in0=in0, scalar=scalar, in1=in1, op0=op0, op1=op1),
                 reads=reads, writes=writes)

        ident_f = sb("ident_f", [128, 128], F32)
        ident_b = sb("ident_b", [128, 128], BF16)
        ones_f = sb("ones_f", [128, 128], F32)
        ustrict = sb("ustrict", [128, 128], F32)
        tri_incl = sb("tri_incl", [128, 128], F32)
        negstrict = sb("negstrict", [128, 128], F32)
        negtriu = sb("negtriu", [128, 128], F32)
        t_const = Tok()

        def amask(t, pattern, cm, op, fill, base=0, val=1.0):
            P.op("pool", lambda g: g.memset(t, val), writes=[t_const])
            P.op("pool", lambda g: g.affine_select(out=t, in_=t, pattern=pattern, compare_op=op, fill=fill,
                                                    base=base, channel_multiplier=cm),
                 reads=[t_const], writes=[t_const])

        P.op("pool", lambda g: g.memset(ones_f[:], 1.0), writes=[t_const])
        amask(ident_f[:], [[-1, 128]], 1, ALU.is_equal, 0.0)
        vcopy("pool", ident_b[:], ident_f[:], [t_const], [t_const])
        amask(ustrict[:], [[-1, 128]], 1, ALU.is_gt, 0.0)
        amask(tri_incl[:], [[1, 128]], -1, ALU.is_ge, 0.0)
        amask(negstrict[:], [[-1, 128]], 1, ALU.is_gt, NEG, val=0.0)
        amask(negtriu[:], [[1, 128]], -1, ALU.is_ge, NEG, val=0.0)

        mixw_b = sb("mixw_b", [128, D], F32)
        sbqw = sb("sbqw", [128, 1], F32)
        sbkw = sb("sbkw", [128, 1], F32)
        convw = sb("convw_s", [128, 96], F32)
        nexpa = sb("nexpa", [128, NT * 8], F32)
        dtb = sb("dtb", [128, NT * 8], F32)
        gnw_b = sb("gnw_b", [128, DH], F32)
        t_mixw = Tok()
        t_sbw = Tok()
        t_gw = Tok()
        P.dma("sp", mixw_b[:], mixw_d[0:1, :].partition_broadcast(128), writes=[t_mixw])
        P.dma("sp", sbqw[:], sbqw_d[:, :], writes=[t_sbw])
        P.dma("sp", sbkw[:], sbkw_d[:, :], writes=[tk["sbkw"]])
        P.dma("sp", convw[:], convw_d[:, :], writes=[tk["convw"]])
        P.dma("sp", nexpa[:], alog_d[0:1, :].partition_broadcast(128), writes=[tk["nexpa"]])
        P.dma("sp", dtb[:], dtb_d[0:1, :].partition_broadcast(128), writes=[tk["dtb"]])
        P.dma("sp", gnw_b[:], gnw_d[0:1, :].partition_broadcast(128), writes=[tk["gnw"]])
        ts("dve", sbqw[:], sbqw[:], float(DH) ** -0.5, None, ALU.mult, None, [t_sbw], [t_sbw])
        act(nexpa[:], nexpa[:], AF.Exp, [tk["nexpa"]], [tk["nexpa"]])
        ts("dve", nexpa[:], nexpa[:], -1.0, None, ALU.mult, None, [tk["nexpa"]], [tk["nexpa"]])

        psb = [ps(f"psb{i}", [128, 512], F32) for i in range(7)]
        pst = [Tok(excl=True) for _ in range(7)]
        pstr = ps("pstr", [128, 8, 128], BF16)
        t_pstr = Tok(excl=True)

        with ExitStack() as ph:
            CW = 2308
            stg = [sb(f"wstg{i}", [128, CW], F32, ph) for i in range(3)]
            stb = [sb(f"wstb{i}", [128, CW], BF16, ph) for i in range(3)]
            tg = [Tok() for _ in range(3)]
            tb = [Tok() for _ in range(3)]
            ceng = ["dve", "act", "dve"]
            it = 0
            jobs = []
            for c in range(8):
                rs_ = slice(c * 128, (c + 1) * 128)
                for j in range(IN_W // CW):
                    jobs.append((win_d[rs_, j * CW:(j + 1) * CW], winb_d[rs_, j * CW:(j + 1) * CW], CW, None))
                for (s_, d_) in ((wbsb_d, wbsbb_d), (wbgdn_d, wbgdnb_d), (wout_d, woutb_d)):
                    jobs.append((s_[rs_, :], d_[rs_, :], 1024, None))
                jobs.append((wq_d[rs_, :], wqb_d[rs_, :], 2048, None))
            if stage in ("full", "peer"):
                for (s_, c0_) in ((pu_d, 0), (pv_d, D)):
                    for r in range(NEXP // 256):
                        jobs.append((s_[r * 256:(r + 1) * 256, :].rearrange("(t p) d -> p t d", p=128),
                                     puvb_d[r * 256:(r + 1) * 256, c0_:c0_ + D].rearrange("(t p) d -> p t d", p=128), 2048, 2))
            for (s_, d_, w, t3) in jobs:
                i = it % 3
                sv, bv = stg[i][:, 0:w], stb[i][:, 0:w]
                if t3:
                    sv3 = sv.rearrange("p (t d) -> p t d", t=t3)
                    bv3 = bv.rearrange("p (t d) -> p t d", t=t3)
                else:
                    sv3, bv3 = sv, bv
                P.dma("sp", sv3, s_, writes=[tg[i]])
                e = ceng[it % 3]
                if e == "act":
                    acopy(bv, sv, [tg[i]], [tb[i]])
                else:
                    vcopy(e, bv, sv, [tg[i]], [tb[i]])
                P.dma("act", d_, bv3, reads=[tb[i]])
                it += 1
            P.barrier()

        with ExitStack() as mx:
            hT = sb("hT", [128, 8, S], BF16, mx)
            t_hT = [Tok() for _ in range(NT)]
            osbT = sb("osbT", [128, NH, S], BF16, mx)
            t_osb = [[Tok() for _ in range(NG)] for _ in range(NH)]
            ogdnT = sb("ogdnT", [128, NH, S], BF16, mx)
            t_ogdn = [[Tok() for _ in range(NT)] for _ in range(NH)]

            for b in range(NB if stage != "peer" else 0):
                with ExitStack() as ph:
                    xin = [sb(f"xin{i}", [128, D], F32, ph) for i in range(2)]
                    xn = [sb(f"xn{i}", [128, D], BF16, ph) for i in range(2)]
                    junk = sb("junk", [128, D], F32, ph)
                    ss = [sb(f"ss{i}", [128, 1], F32, ph) for i in range(2)]
                    for t in range(NT):
                        i = t % 2
                        r0 = b * S + t * 128
                        P.dma("sp", xin[i][:], x_d[r0:r0 + 128, :], writes=[tk["xin", i]])
                        act(junk[:], xin[i][:], AF.Square, [tk["xin", i]], [tk["junk"], tk["ss", i]], accum_out=ss[i][:])
                        act(ss[i][:], ss[i][:], AF.Ln, [tk["ss", i]], [tk["ss", i]], scale=1.0 / D, bias=EPS)
                        act(ss[i][:], ss[i][:], AF.Exp, [tk["ss", i]], [tk["ss", i]], scale=-0.5)
                        stt("dve", xn[i][:], xin[i][:], ss[i][:, 0:1], mixw_b[:], ALU.mult, ALU.mult,
                            [tk["xin", i], tk["ss", i], t_mixw], [tk["xn", i]])
                        for c in range(8):
                            P.op("pe", lambda pe, i=i, c=c: pe.transpose(out=pstr[:, c, :], in_=xn[i][:, c * 128:(c + 1) * 128],
                                                                         identity=ident_b[:]),
                                 reads=[tk["xn", i], t_const], writes=[t_pstr])
                        acopy(hT[:, :, t * 128:(t + 1) * 128], pstr[:, :, :], [t_pstr], [t_hT[t]])
                    P.barrier()

                if "sb" not in skip:
                  with ExitStack() as ph:
                    m01 = [sb(f"m01_{d}", [128, 512], F32, ph) for d in range(4)]
                    mneg = [sb(f"mneg_{d}", [128, 512], F32, ph) for d in range(4)]
                    t_msk = Tok()
                    for d in range(4):
                        P.op("pool", lambda g, d=d: g.memset(m01[d][:], 1.0), writes=[t_msk])
                        P.op("pool", lambda g, d=d: g.affine_select(out=m01[d][:], in_=m01[d][:], pattern=[[1, 512]],
                                                                     compare_op=ALU.is_gt, fill=0.0, base=-128 * d,
                                                                     channel_multiplier=-1), reads=[t_msk], writes=[t_msk])
                        ts("pool", mneg[d][:], m01[d][:], -1.0, -NEG, ALU.add, ALU.mult, [t_msk], [t_msk])
                    ustrict_r = sb("ustrict_r", [128, 128], F32R, ph)
                    ones_r = sb("ones_r", [128, 128], F32R, ph)
                    vcopy("pool", ustrict_r[:], ustrict[:], [t_const, t_msk], [t_msk])
                    vcopy("pool", ones_r[:], ones_f[:], [t_const, t_msk], [t_msk])
                    wsb = [sb(f"wsb{i}", [128, 8, 384], BF16, ph) for i in range(2)]
                    t_wsb = [Tok() for _ in range(2)]
                    qT = sb("qT", [128, S], BF16, ph)
                    kT = sb("kT", [128, S], BF16, ph)
                    t_qT = [Tok() for _ in range(NG)]
                    t_kT = [Tok() for _ in range(NG)]
                    vtm = sb("vtm", [128, NT, 128], BF16, ph)
                    t_v = [Tok() for _ in range(NT)]
                    sqb = [sb(f"sqb{i}", [128, 512], F32, ph) for i in range(2)]
                    t_sqb = [Tok() for _ in range(2)]
                    rsb = [sb(f"rsb{i}", [128, 512], F32, ph) for i in range(2)]
                    t_rsb = [Tok() for _ in range(2)]
                    NR = 5
                    eb = [sb(f"eb{i}", [128, 512], F32, ph) for i in range(NR)]
                    spb = [sb(f"spb{i}", [128, 512], F32R, ph) for i in range(NR)]
                    t1b = [sb(f"t1b{i}", [128, 512], F32, ph) for i in range(NR)]
                    aTb = [sb(f"aTb{i}", [128, 512], BF16, ph) for i in range(NR)]
                    t_eb = [Tok() for _ in range(NR)]
                    t_spb = [Tok() for _ in range(NR)]
                    t_t1b = [Tok() for _ in range(NR)]
                    t_aTb = [Tok() for _ in range(NR)]
                    lacc = [sb(f"lacc{i}", [128, 512], F32R, ph) for i in range(3)]
                    t_lacc = [Tok() for _ in range(3)]
                    for h in range(NH):
                        wi = h % 2
                        for j, off in enumerate((OFF_SBQ, OFF_SBK, OFF_SBV)):
                            src = winb_d[:, off + h * 128: off + (h + 1) * 128].rearrange("(c p) n -> p c n", p=128)
                            P.dma("sp", wsb[wi][:, :, j * 128:(j + 1) * 128], src, writes=[t_wsb[wi]])
                        for which, dst, tdst, wcol, twc in ((0, qT, t_qT, sbqw, t_sbw), (1, kT, t_kT, sbkw, tk["sbkw"])):
                            for g in range(NG):
                                bk = (which * NG + g) % 2
                                for c in range(8):
                                    mm(psb[bk][:], wsb[wi][:, c, which * 128:(which + 1) * 128],
                                       hT[:, c, g * 512:(g + 1) * 512], [t_wsb[wi]] + t_hT[g * 4:(g + 1) * 4], pst[bk],
                                       start=(c == 0), stop=(c == 7))
                                act(sqb[bk][:], psb[bk][:], AF.Square, [pst[bk]], [t_sqb[bk]])
                                mm(psb[2 + bk][:], ones_f[:], sqb[bk][:], [t_sqb[bk], t_const], pst[2 + bk])
                                act(rsb[bk][:], psb[2 + bk][:], AF.Ln, [pst[2 + bk]], [t_rsb[bk]], scale=1.0 / DH, bias=EPS)
                                act(rsb[bk][:], rsb[bk][:], AF.Exp, [t_rsb[bk]], [t_rsb[bk]], scale=-0.5)
                                stt("dve", dst[:, g * 512:(g + 1) * 512], psb[bk][:], wcol[:, 0:1], rsb[bk][:],
                                    ALU.mult, ALU.mult, [pst[bk], t_rsb[bk], twc], [tdst[g]])
                        for t in range(NT):
                            bk = 2 + (t % 2)
                            for c in range(8):
                                mm(psb[bk][:, 0:128], hT[:, c, t * 128:(t + 1) * 128], wsb[wi][:, c, 256:384],
                                   [t_wsb[wi], t_hT[t]], pst[bk], start=(c == 0), stop=(c == 7))
                            acopy(vtm[:, t, :], psb[bk][:, 0:128], [pst[bk]], [t_v[t]])
                        steps = []
                        for g in range(NG):
                            nkb = 4 * g + 4
                            for kb in range(nkb - 1, -1, -1):
                                steps.append((g, kb, kb == nkb - 1, kb == 0))

                        NS = len(steps)
                        sbanks = (0, 1, 6)

                        def stA1(i):
                            g, kb, first, last = steps[i]
                            r = i % NR
                            sbk = sbanks[i % 3]
                            q0 = g * 512
                            mm(psb[sbk][:], kT[:, kb * 128:(kb + 1) * 128], qT[:, q0:q0 + 512],
                               [t_kT[kb // 4], t_qT[g]], pst[sbk])
                            act(eb[r][:], psb[sbk][:], AF.Exp, [pst[sbk]], [t_eb[r]])
                            act(spb[r][:], eb[r][:], AF.Ln, [t_eb[r]], [t_spb[r]], bias=1.0)

                        def stA2(i):
                            g, kb, first, last = steps[i]
                            r = i % NR
                            sbk = sbanks[i % 3]
                            tt("dve", t1b[r][:], psb[sbk][:], spb[r][:], ALU.subtract, [pst[sbk], t_spb[r]], [t_t1b[r]])
                            if kb >= 4 * g:
                                tt("pool", spb[r][:], spb[r][:], m01[kb - 4 * g][:], ALU.mult, [t_spb[r], t_msk], [t_spb[r]])
                            if not last:
                                lo, ln_ = i % 3, (i + 1) % 3
                                if first:
                                    vcopy("pool", lacc[ln_][:], spb[r][:], [t_spb[r]], [t_lacc[ln_]])
                                else:
                                    tt("pool", lacc[ln_][:], lacc[lo][:], spb[r][:], ALU.add, [t_spb[r], t_lacc[lo]], [t_lacc[ln_]])

                        def stB(i):
                            g, kb, first, last = steps[i]
                            r = i % NR
                            ubk = 2 + (i % 2)
                            mm(psb[ubk][:], ustrict_r[:], spb[r][:], [t_spb[r], t_msk], pst[ubk], start=True, stop=first)
                            if not first:
                                mm(psb[ubk][:], ones_r[:], lacc[i % 3][:], [t_lacc[i % 3], t_msk], pst[ubk], start=False, stop=True)
                            tt("dve", t1b[r][:], t1b[r][:], psb[ubk][:], ALU.subtract, [t_t1b[r], pst[ubk]], [t_t1b[r]])
                            if kb >= 4 * g:
                                tt("pool", t1b[r][:], t1b[r][:], mneg[kb - 4 * g][:], ALU.add, [t_t1b[r], t_msk], [t_t1b[r]])

                        def stC(i):
                            r = i % NR
                            act(aTb[r][:], t1b[r][:], AF.Exp, [t_t1b[r]], [t_aTb[r]])

                        def stD(i):
                            g, kb, first, last = steps[i]
                            r = i % NR
                            ob = 4 + (g % 2)
                            mm(psb[ob][:], vtm[:, kb, :], aTb[r][:], [t_aTb[r], t_v[kb]], pst[ob], start=first, stop=last)
                            if last:
                                acopy(osbT[:, h, g * 512:(g + 1) * 512], psb[ob][:], [pst[ob]], [t_osb[h][g]])

                        for n in range(-3, NS + 1):
                            for fn_, off in ((stA1, 3), (stA2, 2), (stB, 1), (stC, 0), (stD, -1)):
                                i = n + off
                                if 0 <= i < NS:
                                    fn_(i)
                    P.barrier()

                if "gdn" not in skip:
                  with ExitStack() as ph:
                    wab = sb("wab", [128, 8, 16], BF16, ph)
                    ab_sb = sb("ab_sb", [128, NT * 16], F32, ph)
                    sm = {n: sb("gs_" + n, [128, NT * 8], F32, ph) for n in ("tmp", "beta", "g", "gc", "ngc", "egc", "bg")}
                    t_gsm = Tok()
                    wg = sb("wg", [128, 8, 512], BF16, ph)
                    cst = sb("cst", [128, 3 + S], F32, ph)
                    cvo = sb("cvo", [128, S], F32, ph)
                    QT = sb("QT", [128, S], F32, ph)
                    KT = sb("KT", [128, S], F32, ph)
                    Ktm = sb("Ktm", [128, NT, 128], F32, ph)
                    Vtm = sb("Vtm", [128, NT, 128], F32, ph)
                    zs = sb("zs", [128, NT, 128], F32, ph)
                    sq5 = sb("sq5", [128, 512], F32, ph)
                    rs5 = sb("rs5", [128, 512], F32, ph)
                    ztmp = sb("ztmp", [128, 128], F32, ph)
                    Sst = sb("Sst", [128, 128], F32, ph)
                    junk1 = sb("junk1", [128, 128], F32, ph)
                    names = ("dg", "tmpa", "tmpb", "dec", "decT", "M0", "M1", "MT0", "MT1", "PT", "attnT", "Vb", "Kbg",
                             "kw", "vcorr", "kcdT", "vnew", "o1", "o")
                    cb = {n: [sb(f"c_{n}{u}", [128, 128], F32, ph) for u in range(4 if n in ("attnT", "kw", "vcorr", "kcdT") else 2)]
                          for n in names}
                    og = [sb(f"c_og{u}", [128, 128], BF16, ph) for u in range(2)]
                    col = {n: [sb(f"k_{n}{u}", [128, 1], F32, ph) for u in range(4)] for n in ("glc", "egl", "ew", "oss")}
                    sbanks_g = ((0, 1), (2, 3), (4, 5, 6))
                    sidx = [0, 0, 0]

                    def pslot(st=2):
                        bi = sbanks_g[st][sidx[st] % len(sbanks_g[st])]
                        sidx[st] += 1
                        return psb[bi][:, 0:128], pst[bi]

                    def ck(k):
                        if stop == k:
                            P.dead = True

                    P.op("pool", lambda g: g.memset(cst[:, 0:3], 0.0), writes=[tk["cst"]])
                    P.dma("sp", wab[:], winb_d[:, OFF_GB:OFF_GB + 16].rearrange("(c p) n -> p c n", p=128), writes=[tk["wab"]])
                    for t in range(NT):
                        for c in range(8):
                            mm(psb[6][:, t * 16:(t + 1) * 16], hT[:, c, t * 128:(t + 1) * 128], wab[:, c, :],
                               [tk["wab"], t_hT[t]], pst[6], start=(c == 0), stop=(c == 7))
                    acopy(ab_sb[:], psb[6][:, 0:NT * 16], [pst[6]], [tk["ab"]])
                    ab3 = ab_sb[:].rearrange("p (t k) -> p t k", k=16)
                    v3 = lambda n: sm[n][:].rearrange("p (t k) -> p t k", k=8)
                    act(v3("tmp"), ab3[:, :, 0:8], AF.Exp, [tk["ab"]], [t_gsm], scale=-1.0)
                    ts("dve", sm["tmp"][:], sm["tmp"][:], 1.0, None, ALU.add, None, [t_gsm], [t_gsm])
                    P.op("dve", lambda v: v.reciprocal(out=sm["beta"][:], in_=sm["tmp"][:]), reads=[t_gsm], writes=[t_gsm])
                    tt("dve", v3("g"), ab3[:, :, 8:16], dtb[:].rearrange("p (t k) -> p t k", k=8), ALU.add,
                       [tk["ab"], tk["dtb"]], [t_gsm])
                    act(sm["g"][:], sm["g"][:], AF.Exp, [t_gsm], [t_gsm])
                    act(sm["g"][:], sm["g"][:], AF.Ln, [t_gsm], [t_gsm], bias=1.0)
                    tt("dve", sm["g"][:], sm["g"][:], nexpa[:], ALU.mult, [t_gsm, tk["nexpa"]], [t_gsm])
                    for t in range(NT):
                        mm(psb[6][:, 256 + t * 8:256 + (t + 1) * 8], tri_incl[:], sm["g"][:, t * 8:(t + 1) * 8],
                           [t_gsm, t_const], pst[6])
                    acopy(sm["gc"][:], psb[6][:, 256:256 + NT * 8], [pst[6]], [t_gsm])
                    ts("dve", sm["ngc"][:], sm["gc"][:], -1.0, None, ALU.mult, None, [t_gsm], [t_gsm])
                    act(sm["egc"][:], sm["gc"][:], AF.Exp, [t_gsm], [t_gsm])
                    tt("dve", sm["bg"][:], sm["beta"][:], sm["egc"][:], ALU.mult, [t_gsm], [t_gsm])

                    ck(1)
                    for h in range(NH):
                        for j, off in enumerate((OFF_GQ, OFF_GK, OFF_GV, OFF_GZ)):
                            src = winb_d[:, off + h * 128: off + (h + 1) * 128].rearrange("(c p) n -> p c n", p=128)
                            P.dma("sp", wg[:, :, j * 128:(j + 1) * 128], src, writes=[tk["wg"]])
                        for t in range(NT):
                            for c in range(8):
                                mm(psb[6][:, 0:128], hT[:, c, t * 128:(t + 1) * 128], wg[:, c, 384:512],
                                   [tk["wg"], t_hT[t]], pst[6], start=(c == 0), stop=(c == 7))
                            act(ztmp[:], psb[6][:, 0:128], AF.Silu, [pst[6]], [tk["ztmp"]])
                            tt("pool", zs[:, t, :], ztmp[:], gnw_b[:], ALU.mult, [tk["ztmp"], tk["gnw"]], [tk["zs", t]])
                        ck(2)
                        for which in range(3):
                            for g in range(NG):
                                bk = 5 + (g % 2)
                                for c in range(8):
                                    mm(psb[bk][:], wg[:, c, which * 128:(which + 1) * 128], hT[:, c, g * 512:(g + 1) * 512],
                                       [tk["wg"]] + t_hT[g * 4:(g + 1) * 4], pst[bk], start=(c == 0), stop=(c == 7))
                                acopy(cst[:, 3 + g * 512:3 + (g + 1) * 512], psb[bk][:], [pst[bk]], [tk["cst"]])
                            wc = lambda i: convw[:, (which * 8 + h) * 4 + i:(which * 8 + h) * 4 + i + 1]
                            ts("dve", cvo[:], cst[:, 3:3 + S], wc(3), None, ALU.mult, None, [tk["cst"], tk["convw"]], [tk["cvo"]])
                            for i in range(3):
                                stt("dve", cvo[:], cst[:, i:i + S], wc(i), cvo[:], ALU.mult, ALU.add,
                                    [tk["cst"], tk["convw"], tk["cvo"]], [tk["cvo"]])
                            act(cvo[:], cvo[:], AF.Silu, [tk["cvo"]], [tk["cvo"]])
                            if which < 2:
                                dst = QT if which == 0 else KT
                                for g in range(NG):
                                    gs = slice(g * 512, (g + 1) * 512)
                                    act(sq5[:], cvo[:, gs], AF.Square, [tk["cvo"]], [tk["sq5"]])
                                    mm(psb[5][:], ones_f[:], sq5[:], [tk["sq5"], t_const], pst[5])
                                    act(rs5[:], psb[5][:], AF.Ln, [pst[5]], [tk["rs5"]], bias=EPS)
                                    act(rs5[:], rs5[:], AF.Exp, [tk["rs5"]], [tk["rs5"]], scale=-0.5)
                                    if which == 0:
                                        stt("dve", dst[:, gs], cvo[:, gs], float(DH) ** -0.5, rs5[:], ALU.mult, ALU.mult,
                                            [tk["cvo"], tk["rs5"]], [tk["QT", g]])
                                    else:
                                        tt("dve", dst[:, gs], cvo[:, gs], rs5[:], ALU.mult, [tk["cvo"], tk["rs5"]], [tk["KT", g]])
                            else:
                                for t in range(NT):
                                    sl, ts_ = pslot()
                                    P.op("pe", lambda pe, sl=sl, t=t: pe.transpose(out=sl, in_=cvo[:, t * 128:(t + 1) * 128],
                                                                                 identity=ident_f[:]),
                                         reads=[tk["cvo"], t_const], writes=[ts_])
                                    acopy(Vtm[:, t, :], sl, [ts_], [tk["Vtm", t]])
                        ck(3)
                        for t in range(NT):
                            sl, ts_ = pslot()
                            P.op("pe", lambda pe, sl=sl, t=t: pe.transpose(out=sl, in_=KT[:, t * 128:(t + 1) * 128],
                                                                         identity=ident_f[:]),
                                 reads=[tk["KT", t // 4], t_const], writes=[ts_])
                            vcopy("dve", Ktm[:, t, :], sl, [ts_], [tk["Ktm", t]])
                        ck(4)
                        P.op("pool", lambda g: g.memset(Sst[:], 0.0), writes=[tk["S"]])

                        def chunk_env(t):
                            ci = t * 8 + h
                            e = dict(cs=slice(t * 128, (t + 1) * 128),
                                     gcc=sm["gc"][:, ci:ci + 1], ngcc=sm["ngc"][:, ci:ci + 1], betac=sm["beta"][:, ci:ci + 1],
                                     egcc=sm["egc"][:, ci:ci + 1], bgc=sm["bg"][:, ci:ci + 1],
                                     tQ=tk["QT", t // 4], tK=tk["KT", t // 4])
                            return e

                        HAND = ("attnT", "kw", "vcorr", "kcdT")

                        def tcomp(t):
                            e = chunk_env(t)
                            cs, gcc, ngcc, betac, bgc, tQ, tK = e["cs"], e["gcc"], e["ngcc"], e["betac"], e["bgc"], e["tQ"], e["tK"]
                            u = t % 2
                            u4 = t % 4
                            st = t % 2
                            B = lambda n: cb[n][u4 if n in HAND else u][:]
                            T = lambda n: tk[n, u4 if n in HAND else u]
                            egl_c, ew_c, glc_c = col["egl"][u4], col["ew"][u], col["glc"][u]
                            ts("dve", B("dg"), ident_f[:], gcc, None, ALU.mult, None, [t_gsm, t_const], [T("dg")])
                            gr, t_gr = pslot(st)
                            mm(gr, ones_f[:], B("dg"), [T("dg"), t_const], t_gr)
                            acopy(glc_c[:], gr[:, 127:128], [t_gr], [T("glc")])
                            act(egl_c[:], glc_c[:], AF.Exp, [T("glc")], [tk["egl", u4]])
                            act(ew_c[:], ngcc, AF.Exp, [T("glc"), t_gsm], [T("ew")], bias=glc_c[:, 0:1])
                            stt("dve", B("tmpa"), gr, -1.0, negstrict[:], ALU.mult, ALU.add, [t_gr, t_const], [T("tmpa")])
                            act(B("dec"), B("tmpa"), AF.Exp, [T("tmpa"), t_gsm], [T("dec")], bias=gcc)
                            tt("dve", B("tmpb"), gr, negtriu[:], ALU.add, [t_gr, t_const], [T("tmpb")])
                            act(B("decT"), B("tmpb"), AF.Exp, [T("tmpb"), t_gsm], [T("decT")], bias=ngcc)
                            kk, t_kk = pslot(st)
                            mm(kk, KT[:, cs], KT[:, cs], [tK], t_kk)
                            stt("dve", B("M0"), kk, betac, B("dec"), ALU.mult, ALU.mult, [t_kk, T("dec"), t_gsm], [T("M0")])
                            qk, t_qk = pslot(st)
                            mm(qk, KT[:, cs], QT[:, cs], [tK, tQ], t_qk)
                            tt("dve", B("attnT"), qk, B("decT"), ALU.mult, [t_qk, T("decT")], [T("attnT")])
                            lt, t_lt = pslot(st)
                            P.op("pe", lambda pe, lt=lt, u=u: pe.transpose(out=lt, in_=cb["M0"][u][:], identity=ident_f[:]),
                                 reads=[T("M0"), t_const], writes=[t_lt])
                            acopy(B("MT0"), lt, [t_lt], [T("MT0")])
                            tt("dve", B("PT"), ident_f[:], lt, ALU.subtract, [t_lt, t_const], [T("PT")])
                            cur = 0
                            for k in range(6):
                                nx = 1 - cur
                                Mc, MTc, Mn, MTn = f"M{cur}", f"MT{cur}", f"M{nx}", f"MT{nx}"
                                m2, t_m2 = pslot(st)
                                mm(m2, B(MTc), B(Mc), [T(MTc), T(Mc)], t_m2)
                                if k < 5:
                                    m2t, t_m2t = pslot(st)
                                    mm(m2t, B(Mc), B(MTc), [T(MTc), T(Mc)], t_m2t)
                                acopy(B(Mn), m2, [t_m2], [T(Mn)])
                                if k < 5:
                                    vcopy("dve", B(MTn), m2t, [t_m2t], [T(MTn)])
                                pp, t_pp = pslot(st)
                                mm(pp, B(Mn), B("PT"), [T(Mn), T("PT")], t_pp)
                                tt("dve", B("PT"), B("PT"), pp, ALU.add, [T("PT"), t_pp], [T("PT")])
                                cur = nx
                            ts("pool", B("Vb"), Vtm[:, t, :], betac, None, ALU.mult, None, [tk["Vtm", t], t_gsm], [T("Vb")])
                            ts("pool", B("Kbg"), Ktm[:, t, :], bgc, None, ALU.mult, None, [tk["Ktm", t], t_gsm], [T("Kbg")])
                            ts("pool", B("kw"), Ktm[:, t, :], ew_c[:, 0:1], None, ALU.mult, None,
                               [tk["Ktm", t], T("ew")], [T("kw")])
                            vc, t_vc = pslot(st)
                            mm(vc, B("PT"), B("Vb"), [T("PT"), T("Vb")], t_vc)
                            acopy(B("vcorr"), vc, [t_vc], [T("vcorr")])
                            kc, t_kc = pslot(st)
                            mm(kc, B("Kbg"), B("PT"), [T("PT"), T("Kbg")], t_kc)
                            vcopy("dve", B("kcdT"), kc, [t_kc], [T("kcdT")])

                        def rec(t):
                            e = chunk_env(t)
                            cs, egcc, tQ = e["cs"], e["egcc"], e["tQ"]
                            u = t % 2
                            u4 = t % 4
                            B = lambda n: cb[n][u4 if n in HAND else u][:]
                            T = lambda n: tk[n, u4 if n in HAND else u]
                            vn, t_vn = pslot(2)
                            mm(vn, B("kcdT"), Sst[:], [T("kcdT"), tk["S"]], t_vn)
                            tt("dve", B("vnew"), B("vcorr"), vn, ALU.subtract, [T("vcorr"), t_vn], [T("vnew")])
                            o1p, t_o1p = pslot(2)
                            mm(o1p, QT[:, cs], Sst[:], [tQ, tk["S"]], t_o1p)
                            act(B("o1"), o1p, AF.Identity, [t_o1p, t_gsm], [T("o1")], scale=egcc)
                            sup, t_sup = pslot(2)
                            mm(sup, B("kw"), B("vnew"), [T("kw"), T("vnew")], t_sup)
                            stt("dve", Sst[:], Sst[:], col["egl"][u4][:, 0:1], sup, ALU.mult, ALU.add,
                                [tk["S"], tk["egl", u4], t_sup], [tk["S"]])
                            o2p, t_o2p = pslot(2)
                            mm(o2p, B("attnT"), B("vnew"), [T("attnT"), T("vnew")], t_o2p)
                            tt("dve", B("o"), B("o1"), o2p, ALU.add, [T("o1"), t_o2p], [T("o")])
                            act(junk1[:], B("o"), AF.Square, [T("o")], [tk["junk1"], T("oss")], accum_out=col["oss"][u][:])
                            act(col["oss"][u][:], col["oss"][u][:], AF.Ln, [T("oss")], [T("oss")], scale=1.0 / DH, bias=EPS)
                            act(col["oss"][u][:], col["oss"][u][:], AF.Exp, [T("oss")], [T("oss")], scale=-0.5)
                            stt("dve", og[u][:], B("o"), col["oss"][u][:, 0:1], zs[:, t, :], ALU.mult, ALU.mult,
                                [T("o"), T("oss"), tk["zs", t]], [T("og")])
                            P.op("pe", lambda pe, u=u: pe.transpose(out=pstr[:, u, :], in_=og[u][:], identity=ident_b[:]),
                                 reads=[T("og"), t_const], writes=[t_pstr])
                            acopy(ogdnT[:, h, cs], pstr[:, u, :], [t_pstr], [t_ogdn[h][t]])

                        def record(fn_, *a_):
                            P.defq = []
                            fn_(*a_)
                            q_ = P.defq
                            P.defq = None
                            return q_

                        def zipdrain(qs, ws):
                            while any(qs):
                                for q_, w_ in zip(qs, ws):
                                    for _ in range(w_):
                                        if q_:
                                            q_.pop(0)()

                        zipdrain([record(tcomp, 0), record(tcomp, 1)], [1, 1])
                        for p_ in range(NT // 2):
                            qa = record(tcomp, 2 * p_ + 2) if 2 * p_ + 2 < NT else []
                            qb = record(tcomp, 2 * p_ + 3) if 2 * p_ + 3 < NT else []
                            qc = record(rec, 2 * p_) + record(rec, 2 * p_ + 1)
                            zipdrain([qa, qb, qc], [3, 3, 2])
                    P.dead = False
                    P.barrier()

                if stage in ("sb", "gdn"):
                    with ExitStack() as ph:
                        dbgo = sb("dbgo", [128, 512], F32, ph)
                        src_t = osbT if stage == "sb" else ogdnT
                        for h in range(NH):
                            for g in range(NG):
                                rd = [t_osb[h][g]] if stage == "sb" else t_ogdn[h][g * 4:(g + 1) * 4]
                                vcopy("dve", dbgo[:], src_t[:, h, g * 512:(g + 1) * 512], rd, [tk["dbgo"]])
                                P.dma("sp", dbg_d[b, h * 128:(h + 1) * 128, g * 512:(g + 1) * 512], dbgo[:], reads=[tk["dbgo"]])
                        P.barrier()
                    continue

                with ExitStack() as ph:
                    wout = sb("wout", [128, 8, D], BF16, ph)
                    P.dma("sp", wout[:], woutb_d.rearrange("(c p) n -> p c n", p=128), writes=[tk["wout"]])
                    wbs = [sb(f"wbs{i}", [128, 8, 128], BF16, ph) for i in range(2)]
                    wbg = [sb(f"wbg{i}", [128, 8, 128], BF16, ph) for i in range(2)]
                    wgt = [sb(f"wgt{i}", [128, 8, 256], BF16, ph) for i in range(2)]
                    mT = [sb(f"mT{i}", [128, 8, 512], BF16, ph) for i in range(2)]
                    e1 = [sb(f"e1_{i}", [128, 512], F32, ph) for i in range(2)]
                    e2 = [sb(f"e2_{i}", [128, 512], F32, ph) for i in range(2)]
                    xr = [sb(f"xr{i}", [128, D], F32, ph) for i in range(2)]
                    x1t = [sb(f"x1t{i}", [128, D], F32, ph) for i in range(2)]
                    it = 0
                    for g in range(NG):
                        gs = slice(g * 512, (g + 1) * 512)
                        gi = g % 2
                        for m in range(8):
                            w = it % 2
                            it += 1
                            ms = slice(m * 128, (m + 1) * 128)
                            P.dma("sp", wbs[w][:], wbsbb_d[:, ms].rearrange("(c p) n -> p c n", p=128), writes=[tk["wbs", w]])
                            P.dma("sp", wbg[w][:], wbgdnb_d[:, ms].rearrange("(c p) n -> p c n", p=128), writes=[tk["wbg", w]])
                            P.dma("sp", wgt[w][:, :, 0:128],
                                  winb_d[:, OFF_GSB + m * 128:OFF_GSB + (m + 1) * 128].rearrange("(c p) n -> p c n", p=128),
                                  writes=[tk["wgt", w]])
                            P.dma("sp", wgt[w][:, :, 128:256],
                                  winb_d[:, OFF_GGDN + m * 128:OFF_GGDN + (m + 1) * 128].rearrange("(c p) n -> p c n", p=128),
                                  writes=[tk["wgt", w]])
                            for c in range(8):
                                mm(psb[0][:], wbs[w][:, c, :], osbT[:, c, gs], [tk["wbs", w], t_osb[c][g]], pst[0],
                                   start=(c == 0), stop=(c == 7))
                            for c in range(8):
                                mm(psb[1][:], wbg[w][:, c, :], ogdnT[:, c, gs], [tk["wbg", w]] + t_ogdn[c][g * 4:(g + 1) * 4],
                                   pst[1], start=(c == 0), stop=(c == 7))
                            for c in range(8):
                                mm(psb[2][:], wgt[w][:, c, 0:128], hT[:, c, gs], [tk["wgt", w]] + t_hT[g * 4:(g + 1) * 4],
                                   pst[2], start=(c == 0), stop=(c == 7))
                            for c in range(8):
                                mm(psb[3][:], wgt[w][:, c, 128:256], hT[:, c, gs], [tk["wgt", w]] + t_hT[g * 4:(g + 1) * 4],
                                   pst[3], start=(c == 0), stop=(c == 7))
                            act(e1[w][:], psb[2][:], AF.Sigmoid, [pst[2]], [tk["e1", w]])
                            act(e2[w][:], psb[3][:], AF.Sigmoid, [pst[3]], [tk["e2", w]])
                            tt("dve", e1[w][:], e1[w][:], psb[0][:], ALU.mult, [tk["e1", w], pst[0]], [tk["e1", w]])
                            tt("dve", e2[w][:], e2[w][:], psb[1][:], ALU.mult, [tk["e2", w], pst[1]], [tk["e2", w]])
                            tt("pool", mT[gi][:, m, :], e1[w][:], e2[w][:], ALU.add, [tk["e1", w], tk["e2", w]], [tk["mT", gi]])
                        for tt_ in range(4):
                            t = g * 4 + tt_
                            xi = t % 2
                            r0 = b * S + t * 128
                            P.dma("sp", xr[xi][:], x_d[r0:r0 + 128, :], writes=[tk["xr", xi]])
                            for n in range(2):
                                bk = 4 + n
                                for m in range(8):
                                    mm(psb[bk][:], mT[gi][:, m, tt_ * 128:(tt_ + 1) * 128], wout[:, m, n * 512:(n + 1) * 512],
                                       [tk["mT", gi], tk["wout"]], pst[bk], start=(m == 0), stop=(m == 7))
                                tt("dve", x1t[xi][:, n * 512:(n + 1) * 512], xr[xi][:, n * 512:(n + 1) * 512], psb[bk][:], ALU.add,
                                   [tk["xr", xi], pst[bk]], [tk["x1t", xi]])
                            dst = out_d if stage == "mix" else x1_d
                            P.dma("sp", dst[r0:r0 + 128, :], x1t[xi][:], reads=[tk["x1t", xi]])
                    P.barrier()
            P.barrier()
        P.barrier()

        if stage == "peer":
            dbg = {n: nc.dram_tensor("dbg_" + n, [128, w], F32, kind="ExternalOutput").ap()
                   for n, w in (("esel", 128), ("gate", 128), ("v16", 256), ("i16f", 256))}
            dbg["nogather"] = "gather" in skip
            dbg["nodot"] = "nodot" in skip
            peer_phase(nc, P, tk, sb, ps, psb, pst, pstr, t_pstr, ident_b, ident_f, t_const, TOK,
                       x_d, out_d, ffnw_d, wqb_d, keysT_d, puvb_d, None, mm, act, acopy, vcopy, tt, ts, stt,
                       NTT=([int(k[3:]) for k in skip if k.startswith("ntt")] or [2])[0], dbg=dbg)
            P.barrier()
        if stage == "full":
            peer_phase(nc, P, tk, sb, ps, psb, pst, pstr, t_pstr, ident_b, ident_f, t_const, TOK,
                       x1_d, out_d, ffnw_d, wqb_d, keysT_d, puvb_d, None, mm, act, acopy, vcopy, tt, ts, stt)
            P.barrier()
        print("ninst", P.ninst, "nsem", P.nsem)
    return nc


def _prep_shared(inputs, NT):
    f = lambda k: np.ascontiguousarray(np.asarray(inputs[k], dtype=np.float32))
    cw = f("gdn_conv_w")[0]
    convw = cw.reshape(4, 3, 8, 128).transpose(3, 1, 2, 0).reshape(128, 96)
    k1 = f("peer_keys1")[0]
    k2 = f("peer_keys2")[0]
    keysT = np.stack([k1, k2], axis=1)
    keysT = keysT.transpose(3, 0, 1, 2).reshape(128, 16 * 128)
    return {
        "mix_norm_w": f("mix_norm_w").reshape(1, D),
        "ffn_norm_w": f("ffn_norm_w").reshape(1, D),
        "w_in": f("w_in")[0],
        "sb_q_norm_w": f("sb_q_norm_w").reshape(DH, 1),
        "sb_k_norm_w": f("sb_k_norm_w").reshape(DH, 1),
        "convw": np.ascontiguousarray(convw),
        "a_log_rep": np.ascontiguousarray(np.tile(f("gdn_a_log").reshape(1, 8), (1, NT))),
        "dtb_rep": np.ascontiguousarray(np.tile(f("gdn_dt_bias").reshape(1, 8), (1, NT))),
        "gdn_out_norm_w": f("gdn_out_norm_w").reshape(1, DH),
        "w_branch_sb": f("w_branch_sb")[0],
        "w_branch_gdn": f("w_branch_gdn")[0],
        "w_out": f("w_out")[0],
        "peer_w_q": f("peer_w_q")[0],
        "keysT": np.ascontiguousarray(keysT),
        "peer_u": f("peer_u")[0],
        "peer_v": f("peer_v")[0],
    }


def kernel(**inputs):
    x = np.asarray(inputs["x"], dtype=np.float32)
    B, S, _ = x.shape
    n = 8
    NB = B // n
    nc = build(NB=NB, S=S, stage="full")
    shared = _prep_shared(inputs, S // 128)
    in_maps = []
    for i in range(n):
        m = dict(shared)
        m["x"] = np.ascontiguousarray(x[i * NB:(i + 1) * NB].reshape(NB * S, D))
        in_maps.append(m)
    res = run_bass_kernel_spmd(nc, in_maps, core_ids=list(range(n)))
    outs = [np.asarray(r["out"]).reshape(NB, S, D) for r in res.results]
    return np.concatenate(outs, axis=0).astype(np.float32)
```

```python
import numpy as np
from contextlib import ExitStack
import concourse.bass as bass
import concourse.mybir as mybir
from concourse.bass_utils import run_bass_kernel_spmd

F32 = mybir.dt.float32
F32R = mybir.dt.float32r
BF16 = mybir.dt.bfloat16
I32 = mybir.dt.int32
U32 = mybir.dt.uint32
AF = mybir.ActivationFunctionType
ALU = mybir.AluOpType
AX = mybir.AxisListType

D = 1024
NH = 8
DH = 128
IN_W = 9232
EPS = 1e-6
OFF_SBQ, OFF_SBK, OFF_SBV = 0, 1024, 2048
OFF_GQ, OFF_GK, OFF_GV = 3072, 4096, 5120
OFF_GZ = 6144
OFF_GB = 7168
OFF_GA = 7176
OFF_GSB = 7184
OFF_GGDN = 8208
NEXP = 16384
NEG = -30000.0


class Tok:
    __slots__ = ("w", "r", "sc", "excl")

    def __init__(self, excl=False):
        self.w = None
        self.r = {}
        self.sc = None
        self.excl = excl


class SemCtr:
    LIMIT = 30000

    def __init__(self, prog, name):
        self.prog = prog
        self.name = name
        self.n = 0
        self.sem = None
        self.k = 0

    def next(self, inc):
        if self.sem is None or self.n + inc > self.LIMIT:
            self.sem = self.prog.es.enter_context(self.prog.nc.semaphore(f"{self.name}_{self.k}"))
            self.prog.nsem += 1
            self.k += 1
            self.n = 0
            self.prog.allsems.append(self)
        self.n += inc
        return (self.sem, self.n, self.name)

    def cur(self):
        if self.sem is None or self.n == 0:
            return None
        return (self.sem, self.n, self.name)


class Prog:
    def __init__(self, nc, es):
        self.nc = nc
        self.es = es
        self.engs = {"pe": nc.tensor, "act": nc.scalar, "dve": nc.vector, "pool": nc.gpsimd, "sp": nc.sync}
        self.nsem = 0
        self.allsems = []
        self.ctr = {k: SemCtr(self, "c" + k) for k in self.engs}
        self.seen = {k: {} for k in self.engs}
        self.ninst = 0
        self.dsems = []
        self.dead = False
        self.defq = None

    def _wait(self, e, tk):
        sem, val, _ = tk
        d = self.seen[e]
        key = id(sem)
        if d.get(key, 0) >= val:
            return
        d[key] = val
        self.engs[e].wait_ge(sem, val)
        self.ninst += 1

    def _deps(self, e, reads, writes):
        own = "c" + e
        for t in reads:
            if t.w is not None:
                if not (e == "pe" and t.w[2] == own):
                    self._wait(e, t.w)
            if t.excl:
                for tk in t.r.values():
                    if tk[2] != own:
                        self._wait(e, tk)
        for t in writes:
            if t.w is not None:
                if not (e == "pe" and t.w[2] == own):
                    self._wait(e, t.w)
            for tk in t.r.values():
                if tk[2] == own and e == "pe":
                    continue
                self._wait(e, tk)

    def _record(self, tk, reads, writes):
        for t in reads:
            t.r[id(tk[0])] = tk
        for t in writes:
            t.w = tk
            t.r = {}

    def op(self, e, fn, reads=(), writes=()):
        if self.dead:
            return None
        if self.defq is not None:
            self.defq.append(lambda: self.op(e, fn, reads, writes))
            return None
        self._deps(e, reads, writes)
        inst = fn(self.engs[e])
        tk = self.ctr[e].next(1)
        inst.then_inc(tk[0], 1)
        self.ninst += 1
        self._record(tk, reads, writes)
        return tk

    def dma(self, q, out, in_, reads=(), writes=(), sc=None, fn=None):
        if self.dead:
            return None
        if self.defq is not None:
            self.defq.append(lambda: self.dma(q, out, in_, reads, writes, sc, fn))
            return None
        self._deps(q, reads, writes)
        if sc is None:
            t0 = writes[0] if writes else reads[0]
            if t0.sc is None:
                t0.sc = SemCtr(self, "d%d" % len(self.dsems))
                self.dsems.append(t0.sc)
            sc = t0.sc
        if fn is None:
            inst = self.engs[q].dma_start(out=out, in_=in_)
        else:
            inst = fn(self.engs[q])
        tk = sc.next(16)
        inst.then_inc(tk[0], 16)
        self.ninst += 1
        self._record(tk, reads, writes)
        return tk

    def barrier(self, engines=None):
        toks = []
        seen = set()
        for sc in self.allsems:
            c = sc.cur()
            if c is not None and id(c[0]) not in seen:
                seen.add(id(c[0]))
                toks.append(c)
        for e in engines or self.engs:
            for tk in toks:
                if tk[2] == "c" + e:
                    continue
                self._wait(e, tk)


class Ctx:
    pass


def peer_phase(nc, P, tk, sb, ps, psb, pst, pstr, t_pstr, ident_b, ident_f, t_const, TOK,
               x1_d, out_d, ffnw_d, wqb_d, keysT_d, pu_d, pv_d, mm, act, acopy, vcopy, tt, ts, stt, NTT=None, dbg=None):
    NTT = NTT or TOK // 128
    NU = 20
    ND = 4
    G = 4
    with ExitStack() as ph:
        wq = sb("wq", [128, 8, 2048], BF16, ph)
        keysT = sb("keysT_s", [128, 16 * 128], F32, ph)
        ffnw_b = sb("ffnw_b", [128, D], F32, ph)
        iota_i = sb("iota_i", [128, 16], I32, ph)
        iota16 = sb("iota16", [128, 16], F32, ph)
        P.dma("sp", wq[:], wqb_d.rearrange("(c p) n -> p c n", p=128), writes=[tk["wq"]])
        P.dma("sp", keysT[:], keysT_d[:, :], writes=[tk["keysT"]])
        P.dma("sp", ffnw_b[:], ffnw_d[0:1, :].partition_broadcast(128), writes=[tk["ffnw"]])
        P.op("pool", lambda g: g.iota(iota_i[:], pattern=[[1, 16]], base=0, channel_multiplier=0), writes=[tk["iota"]])
        vcopy("pool", iota16[:], iota_i[:], [tk["iota"]], [tk["iota"]])

        acc = [sb(f"acc{i}", [128, D], F32, ph) for i in range(2)]
        h2 = sb("h2", [128, D], F32, ph)
        h2b = [sb(f"h2b{i}", [128, D], BF16, ph) for i in range(2)]
        junkb = sb("junkb", [128, D], BF16, ph)
        junkc = sb("junkc", [128, D], BF16, ph)
        prod = [sb(f"prod{i}", [128, D], BF16, ph) for i in range(3)]
        diag = [sb(f"diag{i}", [128, 128], BF16, ph) for i in range(ND)]
        h2T = sb("h2T", [128, 8, 128], BF16, ph)
        junk = sb("pjunk", [128, D], F32, ph)
        pss = sb("pss", [128, 1], F32, ph)
        qTs = sb("qTs", [128, 16 * 128], F32, ph)
        sc = sb("sc", [128, 16 * 128], F32, ph)
        sc2 = sb("sc2", [128, 128], F32, ph)
        v16 = sb("v16", [128, 256], F32, ph)
        i16u = sb("i16u", [128, 256], U32, ph)
        i16f = sb("i16f", [128, 256], F32, ph)
        cand = sb("cand", [128, 256], F32, ph)
        cand2 = sb("cand2", [128, 256], F32, ph)
        oh = sb("oh", [128, 256], F32, ph)
        c16 = sb("c16", [128, 16], F32, ph)
        ci16u = sb("ci16u", [128, 16], U32, ph)
        hlu = sb("hlu", [128, 32], U32, ph)
        hlf = sb("hlf", [128, 32], F32, ph)
        e12 = sb("e12", [128, 32], F32, ph)
        e16 = sb("e16", [128, 16], F32, ph)
        sml = sb("sml", [128, 4], F32, ph)
        c16a = sb("c16a", [128, 128], F32, ph)
        e128 = sb("e128", [128, 128], F32, ph)
        sm8 = sb("sm8", [128, 16], F32, ph)
        gate = [sb(f"gate{i}", [128, 128], F32, ph) for i in range(2)]
        esel = sb("esel", [128, 128], F32, ph)
        eidx = [sb(f"eidx{i}", [128, 128], I32, ph) for i in range(2)]
        pre = sb("pre", [128, 128], F32, ph)
        coef = sb("coef", [128, 128], F32, ph)
        ubuf = [sb(f"ubuf{i}", [128, 2 * D], BF16, ph) for i in range(NU)]
        banks_a = [0, 1]
        banks_b = [2, 3, 4]
        sidx = [0, 0]

        def bc3(tile, row, off, inner_first):
            if inner_first:
                return bass.AP(tile, off, [[row, 128], [1, 16], [0, 16]])
            return bass.AP(tile, off, [[row, 128], [0, 16], [1, 16]])

        breg = nc.gpsimd.alloc_register("bchk")
        nc.gpsimd.reg_mov(breg, NEXP - 1)
        gi = [0]

        def sel(tile_i):
            a = tile_i % 2
            r0 = tile_i * 128
            A = acc[a]
            P.dma("sp", A[:], x1_d[r0:r0 + 128, :], writes=[tk["acc", a]])
            P.op("dve", lambda v: v.scalar_tensor_tensor(out=junk[:], in0=A[:], scalar=1.0, in1=A[:], op0=ALU.mult, op1=ALU.mult,
                                                         accum_out=pss[:]), reads=[tk["acc", a]], writes=[tk["pjunk"], tk["pss"]])
            act(pss[:], pss[:], AF.Ln, [tk["pss"]], [tk["pss"]], scale=1.0 / D, bias=EPS)
            act(pss[:], pss[:], AF.Exp, [tk["pss"]], [tk["pss"]], scale=-0.5)
            stt("dve", h2[:], A[:], pss[:, 0:1], ffnw_b[:], ALU.mult, ALU.mult, [tk["acc", a], tk["pss"], tk["ffnw"]], [tk["h2"]])
            vcopy("pool", h2b[a][:], h2[:], [tk["h2"]], [tk["h2b", a]])
            for c in range(8):
                P.op("pe", lambda pe, c=c: pe.transpose(out=pstr[:, c, :], in_=h2b[a][:, c * 128:(c + 1) * 128], identity=ident_b[:]),
                     reads=[tk["h2b", a], t_const], writes=[t_pstr])
            vcopy("dve", h2T[:], pstr[:, :, :], [t_pstr], [tk["h2T"]])
            for jg in range(4):
                bk = banks_a[sidx[0] % len(banks_a)]
                sidx[0] += 1
                for jj in range(4):
                    j = jg * 4 + jj
                    for c in range(8):
                        mm(psb[bk][:, jj * 128:(jj + 1) * 128], wq[:, c, j * 128:(j + 1) * 128], h2T[:, c, :],
                           [tk["wq"], tk["h2T"]], pst[bk], start=(c == 0), stop=(c == 7))
                acopy(qTs[:, jg * 512:(jg + 1) * 512], psb[bk][:], [pst[bk]], [tk["qTs", jg]])
            for jg in range(4):
                bk = banks_b[sidx[1] % len(banks_b)]
                sidx[1] += 1
                for jj in range(4):
                    j = jg * 4 + jj
                    mm(psb[bk][:, jj * 128:(jj + 1) * 128], qTs[:, j * 128:(j + 1) * 128], keysT[:, j * 128:(j + 1) * 128],
                       [tk["qTs", jg], tk["keysT"]], pst[bk])
                acopy(sc[:, jg * 512:(jg + 1) * 512], psb[bk][:], [pst[bk]], [tk["sc", jg]])
            for j in range(16):
                scj = sc[:, j * 128:(j + 1) * 128]
                va = v16[:, j * 16:j * 16 + 8]
                vb_ = v16[:, j * 16 + 8:j * 16 + 16]
                P.op("dve", lambda v, va=va, scj=scj: v.max(out=va, in_=scj), reads=[tk["sc", j // 4]], writes=[tk["v16"]])
                P.op("dve", lambda v, va=va, scj=scj, j=j: v.max_index(out=i16u[:, j * 16:j * 16 + 8], in_max=va, in_values=scj),
                     reads=[tk["sc", j // 4], tk["v16"]], writes=[tk["i16u"]])
                P.op("dve", lambda v, va=va, scj=scj: v.match_replace(out=sc2[:], in_to_replace=va, in_values=scj, imm_value=-1e30),
                     reads=[tk["sc", j // 4], tk["v16"]], writes=[tk["sc2"]])
                P.op("dve", lambda v, vb_=vb_: v.max(out=vb_, in_=sc2[:]), reads=[tk["sc2"]], writes=[tk["v16"]])
                P.op("dve", lambda v, vb_=vb_, j=j: v.max_index(out=i16u[:, j * 16 + 8:j * 16 + 16], in_max=vb_, in_values=sc2[:]),
                     reads=[tk["sc2"], tk["v16"]], writes=[tk["i16u"]])
            vcopy("dve", i16f[:], i16u[:], [tk["i16u"]], [tk["i16f"]])
            for h in range(8):
                o1_, o2_ = (2 * h) * 16, (2 * h + 1) * 16
                c3 = cand[:].rearrange("p (i j) -> p i j", j=16)
                tt("dve", c3, bc3(v16, 256, o1_, True), bc3(v16, 256, o2_, False), ALU.add, [tk["v16"]], [tk["cand"]])
                P.op("dve", lambda v: v.max(out=c16[:, 0:8], in_=cand[:]), reads=[tk["cand"]], writes=[tk["c16"]])
                P.op("dve", lambda v: v.max_index(out=ci16u[:, 0:8], in_max=c16[:, 0:8], in_values=cand[:]),
                     reads=[tk["cand"], tk["c16"]], writes=[tk["ci16u"]])
                P.op("dve", lambda v: v.match_replace(out=cand2[:], in_to_replace=c16[:, 0:8], in_values=cand[:], imm_value=-1e30),
                     reads=[tk["cand"], tk["c16"]], writes=[tk["cand2"]])
                P.op("dve", lambda v: v.max(out=c16[:, 8:16], in_=cand2[:]), reads=[tk["cand2"]], writes=[tk["c16"]])
                P.op("dve", lambda v: v.max_index(out=ci16u[:, 8:16], in_max=c16[:, 8:16], in_values=cand2[:]),
                     reads=[tk["cand2"], tk["c16"]], writes=[tk["ci16u"]])
                vcopy("dve", c16a[:, h * 16:(h + 1) * 16], c16[:], [tk["c16"]], [tk["c16a"]])
                P.op("dve", lambda v: v.tensor_single_scalar(out=hlu[:, 0:16], in_=ci16u[:], scalar=4, op=ALU.logical_shift_right),
                     reads=[tk["ci16u"]], writes=[tk["hlu"]])
                P.op("dve", lambda v: v.tensor_single_scalar(out=hlu[:, 16:32], in_=ci16u[:], scalar=15, op=ALU.bitwise_and),
                     reads=[tk["ci16u"]], writes=[tk["hlu"]])
                vcopy("dve", hlf[:], hlu[:], [tk["hlu"]], [tk["hlf"]])
                o3 = oh[:].rearrange("p (k i) -> p k i", i=16)
                for half, off in ((0, o1_), (1, o2_)):
                    tt("dve", o3, bc3(iota16, 16, 0, False), bc3(hlf, 32, half * 16, True), ALU.is_equal,
                       [tk["iota"], tk["hlf"]], [tk["oh"]])
                    tt("dve", o3, o3, bc3(i16f, 256, off, False), ALU.mult, [tk["oh"], tk["i16f"]], [tk["oh"]])
                    P.op("dve", lambda v, half=half: v.tensor_reduce(out=e12[:, half * 16:(half + 1) * 16], in_=o3, axis=AX.X, op=ALU.add),
                         reads=[tk["oh"]], writes=[tk["e12"]])
                stt("dve", esel[:, h * 16:(h + 1) * 16], e12[:, 0:16], 128.0, e12[:, 16:32], ALU.mult, ALU.add,
                    [tk["e12"]], [tk["esel"]])
            c3a = c16a[:].rearrange("p (h k) -> p h k", k=16)
            e3a = e128[:].rearrange("p (h k) -> p h k", k=16)
            tt("dve", e3a, c3a, bass.AP(c16a, 0, [[128, 128], [16, 8], [0, 16]]), ALU.subtract, [tk["c16a"]], [tk["e128"]])
            act(e128[:], e128[:], AF.Exp, [tk["e128"]], [tk["e128"]])
            P.op("dve", lambda v: v.tensor_reduce(out=sm8[:, 0:8], in_=e3a, axis=AX.X, op=ALU.add), reads=[tk["e128"]], writes=[tk["sm8"]])
            P.op("dve", lambda v: v.reciprocal(out=sm8[:, 8:16], in_=sm8[:, 0:8]), reads=[tk["sm8"]], writes=[tk["sm8"]])
            tt("dve", gate[a][:].rearrange("p (h k) -> p h k", k=16), e3a, bass.AP(sm8, 8, [[16, 128], [1, 8], [0, 16]]), ALU.mult,
               [tk["e128"], tk["sm8"]], [tk["gate", a]])
            ei = eidx[a]
            vcopy("dve", ei[:], esel[:], [tk["esel"]], [tk["eidx", a]])
            if dbg is not None and tile_i == 0:
                P.dma("sp", dbg["esel"][:, :], esel[:], reads=[tk["esel"]])
                P.dma("sp", dbg["gate"][:, :], gate[a][:], reads=[tk["gate", a]])
                P.dma("sp", dbg["v16"][:, :], v16[:], reads=[tk["v16"]])
                P.dma("sp", dbg["i16f"][:, :], i16f[:], reads=[tk["i16f"]])

        def record(tile_i):
            P.defq = []
            sel(tile_i)
            q = P.defq
            P.defq = None
            return q

        def drain(q, n):
            while q and n > 0:
                q.pop(0)()
                n -= 1

        drain(record(0), 1 << 30)
        for tile_i in range(NTT):
            a = tile_i % 2
            r0 = tile_i * 128
            A = acc[a]
            ei = eidx[a]
            nq = record(tile_i + 1) if tile_i + 1 < NTT else []
            if dbg is not None and dbg.get("nogather"):
                continue
            slot_buf = {}

            def group_math(grp):
                cols = slice(grp * G, (grp + 1) * G)
                act(coef[:, cols], pre[:, cols], AF.Gelu, [tk["pre", grp]], [tk["coef", grp]])
                tt("dve", coef[:, cols], coef[:, cols], gate[a][:, cols], ALU.mult, [tk["coef", grp], tk["gate", a]], [tk["coef", grp]])
                for s in range(grp * G, (grp + 1) * G):
                    if dbg is not None and dbg.get("nodot"):
                        break
                    k = s % ND
                    ub = slot_buf[s]
                    act(diag[k][:], ident_b[:], AF.Identity, [tk["coef", grp], t_const], [tk["diag", k]], scale=coef[:, s:s + 1])
                    for n in range(2):
                        mm(psb[5 + n][:], diag[k][:], ubuf[ub][:, D + n * 512:D + (n + 1) * 512], [tk["diag", k], tk["ubuf", ub]],
                           pst[5 + n], start=(s == 0), stop=(s == 127))

            for grp in range(128 // G):
                for s in range(grp * G, (grp + 1) * G):
                    ub = gi[0] % NU
                    gi[0] += 1
                    slot_buf[s] = ub
                    P.dma("pool", None, None, reads=[tk["eidx", a]], writes=[tk["ubuf", ub]],
                          fn=lambda g, ub=ub, s=s, ei=ei: g.indirect_dma_start(
                              out=ubuf[ub][:], out_offset=None, in_=pu_d[:, :],
                              in_offset=bass.IndirectOffsetOnAxis(ap=ei[:, s:s + 1], axis=0),
                              bounds_check=breg, oob_is_err=False))
                    if dbg is not None and dbg.get("nodot"):
                        vcopy("dve", pre[:, s:s + 1], ubuf[ub][:, 0:1], [tk["ubuf", ub]], [tk["pre", grp]])
                    elif s % 4 != 3:
                        pk = s % 3
                        tt("dve", prod[pk][:], ubuf[ub][:, 0:D], h2b[a][:], ALU.mult, [tk["ubuf", ub], tk["h2b", a]], [tk["prod", pk]])
                        act(junkb[:], prod[pk][:], AF.Identity, [tk["prod", pk]], [tk["junkb"], tk["pre", grp]], accum_out=pre[:, s:s + 1])
                    else:
                        P.op("dve", lambda v, ub=ub, s=s, a=a: v.scalar_tensor_tensor(
                            out=junkc[:], in0=ubuf[ub][:, 0:D], scalar=1.0, in1=h2b[a][:], op0=ALU.mult, op1=ALU.mult,
                            accum_out=pre[:, s:s + 1]), reads=[tk["ubuf", ub], tk["h2b", a]], writes=[tk["junkc"], tk["pre", grp]])
                    drain(nq, 3)
                if grp > 0:
                    group_math(grp - 1)
            group_math(128 // G - 1)
            for n in range(2):
                tt("dve", A[:, n * 512:(n + 1) * 512], A[:, n * 512:(n + 1) * 512], psb[5 + n][:], ALU.add,
                   [tk["acc", a], pst[5 + n]], [tk["acc", a]])
            P.dma("sp", out_d[r0:r0 + 128, :], A[:], reads=[tk["acc", a]])
            drain(nq, 1 << 30)
        P.barrier()


class _Stop(Exception):
    pass


def build(NB=4, S=2048, stage="full", skip=(), stop=0):
    nc = bass.Bass("TRN2", target_bir_lowering=False)
    NT = S // 128
    NG = S // 512
    TOK = NB * S
    from collections import defaultdict

    def din(name, shape, dt=F32):
        return nc.dram_tensor(name, shape, dt, kind="ExternalInput").ap()

    x_d = din("x", [TOK, D])
    mixw_d = din("mix_norm_w", [1, D])
    ffnw_d = din("ffn_norm_w", [1, D])
    win_d = din("w_in", [D, IN_W])
    sbqw_d = din("sb_q_norm_w", [DH, 1])
    sbkw_d = din("sb_k_norm_w", [DH, 1])
    convw_d = din("convw", [128, 96])
    alog_d = din("a_log_rep", [1, NT * 8])
    dtb_d = din("dtb_rep", [1, NT * 8])
    gnw_d = din("gdn_out_norm_w", [1, DH])
    wbsb_d = din("w_branch_sb", [D, D])
    wbgdn_d = din("w_branch_gdn", [D, D])
    wout_d = din("w_out", [D, D])
    wq_d = din("peer_w_q", [D, 2048])
    keysT_d = din("keysT", [128, 16 * 128])
    pu_d = din("peer_u", [NEXP, D])
    pv_d = din("peer_v", [NEXP, D])
    if stage in ("sb", "gdn"):
        dbg_d = nc.dram_tensor("dbg", [NB, D, S], F32, kind="ExternalOutput").ap()
    else:
        out_d = nc.dram_tensor("out", [TOK, D], F32, kind="ExternalOutput").ap()

    winb_d = nc.dram_tensor("w_in_b", [D, IN_W], BF16, kind="Internal").ap()
    wbsbb_d = nc.dram_tensor("wbsb_b", [D, D], BF16, kind="Internal").ap()
    wbgdnb_d = nc.dram_tensor("wbgdn_b", [D, D], BF16, kind="Internal").ap()
    woutb_d = nc.dram_tensor("wout_b", [D, D], BF16, kind="Internal").ap()
    wqb_d = nc.dram_tensor("wq_b", [D, 2048], BF16, kind="Internal").ap()
    x1_d = nc.dram_tensor("x1_s", [TOK, D], F32, kind="Internal").ap()
    puvb_d = nc.dram_tensor("puv_b", [NEXP, 2 * D], BF16, kind="Internal").ap()

    es = ExitStack()
    with es:
        P = Prog(nc, es)
        tk = defaultdict(Tok)

        uid = [0]

        def sb(name, shape, dt, st=es):
            uid[0] += 1
            return st.enter_context(nc.sbuf_tensor(f"{name}_{uid[0]}", shape, dt))

        def ps(name, shape, dt=F32, st=es):
            return st.enter_context(nc.psum_tensor(name, shape, dt))

        def mm(out, lhsT, rhs, reads, wtok, start=True, stop=True):
            P.op("pe", lambda pe: pe.matmul(out, lhsT=lhsT, rhs=rhs, start=start, stop=stop),
                 reads=reads, writes=[wtok])

        def act(out, in_, func, reads, writes, **kw):
            P.op("act", lambda a: a.activation(out=out, in_=in_, func=func, **kw), reads=reads, writes=writes)

        def acopy(out, in_, reads, writes):
            P.op("act", lambda a: a.copy(out=out, in_=in_), reads=reads, writes=writes)

        def vcopy(e, out, in_, reads, writes):
            P.op(e, lambda v: v.tensor_copy(out=out, in_=in_), reads=reads, writes=writes)

        def tt(e, out, in0, in1, op, reads, writes):
            P.op(e, lambda v: v.tensor_tensor(out=out, in0=in0, in1=in1, op=op), reads=reads, writes=writes)

        def ts(e, out, in0, s1, s2, op0, op1, reads, writes):
            if s2 is None:
                P.op(e, lambda v: v.tensor_scalar(out=out, in0=in0, scalar1=s1, scalar2=None, op0=op0),
                     reads=reads, writes=writes)
            else:
                P.op(e, lambda v: v.tensor_scalar(out=out, in0=in0, scalar1=s1, scalar2=s2, op0=op0, op1=op1),
                     reads=reads, writes=writes)

        def stt(e, out, in0, scalar, in1, op0, op1, reads, writes):
            P.op(e, lambda v: v.scalar_tensor_tensor(out=out, in0=in0, scalar=scalar, in1=in1, op0=op0, op1=op1),
                 reads=reads, writes=writes)

        ident_f = sb("ident_f", [128, 128], F32)
        ident_b = sb("ident_b", [128, 128], BF16)
        ones_f = sb("ones_f", [128, 128], F32)
        ustrict = sb("ustrict", [128, 128], F32)
        tri_incl = sb("tri_incl", [128, 128], F32)
        negstrict = sb("negstrict", [128, 128], F32)
        negtriu = sb("negtriu", [128, 128], F32)
        t_const = Tok()

        def amask(t, pattern, cm, op, fill, base=0, val=1.0):
            P.op("pool", lambda g: g.memset(t, val), writes=[t_const])
            P.op("pool", lambda g: g.affine_select(out=t, in_=t, pattern=pattern, compare_op=op, fill=fill,
                                                    base=base, channel_multiplier=cm),
                 reads=[t_const], writes=[t_const])

        P.op("pool", lambda g: g.memset(ones_f[:], 1.0), writes=[t_const])
        amask(ident_f[:], [[-1, 128]], 1, ALU.is_equal, 0.0)
        vcopy("pool", ident_b[:], ident_f[:], [t_const], [t_const])
        amask(ustrict[:], [[-1, 128]], 1, ALU.is_gt, 0.0)
        amask(tri_incl[:], [[1, 128]], -1, ALU.is_ge, 0.0)
        amask(negstrict[:], [[-1, 128]], 1, ALU.is_gt, NEG, val=0.0)
        amask(negtriu[:], [[1, 128]], -1, ALU.is_ge, NEG, val=0.0)

        mixw_b = sb("mixw_b", [128, D], F32)
        sbqw = sb("sbqw", [128, 1], F32)
        sbkw = sb("sbkw", [128, 1], F32)
        convw = sb("convw_s", [128, 96], F32)
        nexpa = sb("nexpa", [128, NT * 8], F32)
        dtb = sb("dtb", [128, NT * 8], F32)
        gnw_b = sb("gnw_b", [128, DH], F32)
        t_mixw = Tok()
        t_sbw = Tok()
        t_gw = Tok()
        P.dma("sp", mixw_b[:], mixw_d[0:1, :].partition_broadcast(128), writes=[t_mixw])
        P.dma("sp", sbqw[:], sbqw_d[:, :], writes=[t_sbw])
        P.dma("sp", sbkw[:], sbkw_d[:, :], writes=[tk["sbkw"]])
        P.dma("sp", convw[:], convw_d[:, :], writes=[tk["convw"]])
        P.dma("sp", nexpa[:], alog_d[0:1, :].partition_broadcast(128), writes=[tk["nexpa"]])
        P.dma("sp", dtb[:], dtb_d[0:1, :].partition_broadcast(128), writes=[tk["dtb"]])
        P.dma("sp", gnw_b[:], gnw_d[0:1, :].partition_broadcast(128), writes=[tk["gnw"]])
        ts("dve", sbqw[:], sbqw[:], float(DH) ** -0.5, None, ALU.mult, None, [t_sbw], [t_sbw])
        act(nexpa[:], nexpa[:], AF.Exp, [tk["nexpa"]], [tk["nexpa"]])
        ts("dve", nexpa[:], nexpa[:], -1.0, None, ALU.mult, None, [tk["nexpa"]], [tk["nexpa"]])

        psb = [ps(f"psb{i}", [128, 512], F32) for i in range(7)]
        pst = [Tok(excl=True) for _ in range(7)]
        pstr = ps("pstr", [128, 8, 128], BF16)
        t_pstr = Tok(excl=True)

        with ExitStack() as ph:
            CW = 2308
            stg = [sb(f"wstg{i}", [128, CW], F32, ph) for i in range(3)]
            stb = [sb(f"wstb{i}", [128, CW], BF16, ph) for i in range(3)]
            tg = [Tok() for _ in range(3)]
            tb = [Tok() for _ in range(3)]
            ceng = ["dve", "act", "dve"]
            it = 0
            jobs = []
            for c in range(8):
                rs_ = slice(c * 128, (c + 1) * 128)
                for j in range(IN_W // CW):
                    jobs.append((win_d[rs_, j * CW:(j + 1) * CW], winb_d[rs_, j * CW:(j + 1) * CW], CW, None))
                for (s_, d_) in ((wbsb_d, wbsbb_d), (wbgdn_d, wbgdnb_d), (wout_d, woutb_d)):
                    jobs.append((s_[rs_, :], d_[rs_, :], 1024, None))
                jobs.append((wq_d[rs_, :], wqb_d[rs_, :], 2048, None))
            if stage in ("full", "peer"):
                for (s_, c0_) in ((pu_d, 0), (pv_d, D)):
                    for r in range(NEXP // 256):
                        jobs.append((s_[r * 256:(r + 1) * 256, :].rearrange("(t p) d -> p t d", p=128),
                                     puvb_d[r * 256:(r + 1) * 256, c0_:c0_ + D].rearrange("(t p) d -> p t d", p=128), 2048, 2))
            for (s_, d_, w, t3) in jobs:
                i = it % 3
                sv, bv = stg[i][:, 0:w], stb[i][:, 0:w]
                if t3:
                    sv3 = sv.rearrange("p (t d) -> p t d", t=t3)
                    bv3 = bv.rearrange("p (t d) -> p t d", t=t3)
                else:
                    sv3, bv3 = sv, bv
                P.dma("sp", sv3, s_, writes=[tg[i]])
                e = ceng[it % 3]
                if e == "act":
                    acopy(bv, sv, [tg[i]], [tb[i]])
                else:
                    vcopy(e, bv, sv, [tg[i]], [tb[i]])
                P.dma("act", d_, bv3, reads=[tb[i]])
                it += 1
            P.barrier()

        with ExitStack() as mx:
            hT = sb("hT", [128, 8, S], BF16, mx)
            t_hT = [Tok() for _ in range(NT)]
            osbT = sb("osbT", [128, NH, S], BF16, mx)
            t_osb = [[Tok() for _ in range(NG)] for _ in range(NH)]
            ogdnT = sb("ogdnT", [128, NH, S], BF16, mx)
            t_ogdn = [[Tok() for _ in range(NT)] for _ in range(NH)]

            for b in range(NB if stage != "peer" else 0):
                with ExitStack() as ph:
                    xin = [sb(f"xin{i}", [128, D], F32, ph) for i in range(2)]
                    xn = [sb(f"xn{i}", [128, D], BF16, ph) for i in range(2)]
                    junk = sb("junk", [128, D], F32, ph)
                    ss = [sb(f"ss{i}", [128, 1], F32, ph) for i in range(2)]
                    for t in range(NT):
                        i = t % 2
                        r0 = b * S + t * 128
                        P.dma("sp", xin[i][:], x_d[r0:r0 + 128, :], writes=[tk["xin", i]])
                        act(junk[:], xin[i][:], AF.Square, [tk["xin", i]], [tk["junk"], tk["ss", i]], accum_out=ss[i][:])
                        act(ss[i][:], ss[i][:], AF.Ln, [tk["ss", i]], [tk["ss", i]], scale=1.0 / D, bias=EPS)
                        act(ss[i][:], ss[i][:], AF.Exp, [tk["ss", i]], [tk["ss", i]], scale=-0.5)
                        stt("dve", xn[i][:], xin[i][:], ss[i][:, 0:1], mixw_b[:], ALU.mult, ALU.mult,
                            [tk["xin", i], tk["ss", i], t_mixw], [tk["xn", i]])
                        for c in range(8):
                            P.op("pe", lambda pe, i=i, c=c: pe.transpose(out=pstr[:, c, :], in_=xn[i][:, c * 128:(c + 1) * 128],
                                                                         identity=ident_b[:]),
                                 reads=[tk["xn", i], t_const], writes=[t_pstr])
                        acopy(hT[:, :, t * 128:(t + 1) * 128], pstr[:, :, :], [t_pstr], [t_hT[t]])
                    P.barrier()

                if "sb" not in skip:
                  with ExitStack() as ph:
                    m01 = [sb(f"m01_{d}", [128, 512], F32, ph) for d in range(4)]
                    mneg = [sb(f"mneg_{d}", [128, 512], F32, ph) for d in range(4)]
                    t_msk = Tok()
                    for d in range(4):
                        P.op("pool", lambda g, d=d: g.memset(m01[d][:], 1.0), writes=[t_msk])
                        P.op("pool", lambda g, d=d: g.affine_select(out=m01[d][:], in_=m01[d][:], pattern=[[1, 512]],
                                                                     compare_op=ALU.is_gt, fill=0.0, base=-128 * d,
                                                                     channel_multiplier=-1), reads=[t_msk], writes=[t_msk])
                        ts("pool", mneg[d][:], m01[d][:], -1.0, -NEG, ALU.add, ALU.mult, [t_msk], [t_msk])
                    ustrict_r = sb("ustrict_r", [128, 128], F32R, ph)
                    ones_r = sb("ones_r", [128, 128], F32R, ph)
                    vcopy("pool", ustrict_r[:], ustrict[:], [t_const, t_msk], [t_msk])
                    vcopy("pool", ones_r[:], ones_f[:], [t_const, t_msk], [t_msk])
                    wsb = [sb(f"wsb{i}", [128, 8, 384], BF16, ph) for i in range(2)]
                    t_wsb = [Tok() for _ in range(2)]
                    qTs_ = [sb(f"qT{i}", [128, S], BF16, ph) for i in range(2)]
                    kTs_ = [sb(f"kT{i}", [128, S], BF16, ph) for i in range(2)]
                    t_qTs = [[Tok() for _ in range(NG)] for _ in range(2)]
                    t_kTs = [[Tok() for _ in range(NG)] for _ in range(2)]
                    vtms_ = [sb(f"vtm{i}", [128, NT, 128], BF16, ph) for i in range(2)]
                    t_vs = [[Tok() for _ in range(NT)] for _ in range(2)]
                    sqb = [sb(f"sqb{i}", [128, 512], F32, ph) for i in range(2)]
                    t_sqb = [Tok() for _ in range(2)]
                    rsb = [sb(f"rsb{i}", [128, 512], F32, ph) for i in range(2)]
                    t_rsb = [Tok() for _ in range(2)]
                    NR = 5
                    eb = [sb(f"eb{i}", [128, 512], F32, ph) for i in range(NR)]
                    spb = [sb(f"spb{i}", [128, 512], F32R, ph) for i in range(NR)]
                    t1b = [sb(f"t1b{i}", [128, 512], F32, ph) for i in range(NR)]
                    aTb = [sb(f"aTb{i}", [128, 512], BF16, ph) for i in range(NR)]
                    t_eb = [Tok() for _ in range(NR)]
                    t_spb = [Tok() for _ in range(NR)]
                    t_t1b = [Tok() for _ in range(NR)]
                    t_aTb = [Tok() for _ in range(NR)]
                    lacc = [sb(f"lacc{i}", [128, 512], F32R, ph) for i in range(3)]
                    t_lacc = [Tok() for _ in range(3)]
                    def sb_prep(h):
                        wi = h % 2
                        qT_, kT_, vtm_ = qTs_[wi], kTs_[wi], vtms_[wi]
                        for j, off in enumerate((OFF_SBQ, OFF_SBK, OFF_SBV)):
                            src = winb_d[:, off + h * 128: off + (h + 1) * 128].rearrange("(c p) n -> p c n", p=128)
                            P.dma("sp", wsb[wi][:, :, j * 128:(j + 1) * 128], src, writes=[t_wsb[wi]])
                        for which, dst, tdst, wcol, twc in ((0, qT_, t_qTs[wi], sbqw, t_sbw), (1, kT_, t_kTs[wi], sbkw, tk["sbkw"])):
                            for g in range(NG):
                                bk = (which * NG + g) % 2
                                for c in range(8):
                                    mm(psb[5][:], wsb[wi][:, c, which * 128:(which + 1) * 128],
                                       hT[:, c, g * 512:(g + 1) * 512], [t_wsb[wi]] + t_hT[g * 4:(g + 1) * 4], pst[5],
                                       start=(c == 0), stop=(c == 7))
                                act(sqb[bk][:], psb[5][:], AF.Square, [pst[5]], [t_sqb[bk]])
                                mm(psb[6][:], ones_f[:], sqb[bk][:], [t_sqb[bk], t_const], pst[6])
                                act(rsb[bk][:], psb[6][:], AF.Ln, [pst[6]], [t_rsb[bk]], scale=1.0 / DH, bias=EPS)
                                act(rsb[bk][:], rsb[bk][:], AF.Exp, [t_rsb[bk]], [t_rsb[bk]], scale=-0.5)
                                stt("dve", dst[:, g * 512:(g + 1) * 512], psb[5][:], wcol[:, 0:1], rsb[bk][:],
                                    ALU.mult, ALU.mult, [pst[5], t_rsb[bk], twc], [tdst[g]])
                        for t in range(NT):
                            bk = 5 + (t % 2)
                            for c in range(8):
                                mm(psb[bk][:, 0:128], hT[:, c, t * 128:(t + 1) * 128], wsb[wi][:, c, 256:384],
                                   [t_wsb[wi], t_hT[t]], pst[bk], start=(c == 0), stop=(c == 7))
                            acopy(vtm_[:, t, :], psb[bk][:, 0:128], [pst[bk]], [t_vs[wi][t]])

                    def sb_record(h):
                        P.defq = []
                        sb_prep(h)
                        q_ = P.defq
                        P.defq = None
                        return q_

                    for fn_ in sb_record(0):
                        fn_()
                    for h in range(NH):
                        wi = h % 2
                        qT, kT, vtm = qTs_[wi], kTs_[wi], vtms_[wi]
                        t_qT, t_kT, t_v = t_qTs[wi], t_kTs[wi], t_vs[wi]
                        prepq = sb_record(h + 1) if h + 1 < NH else []
                        steps = []
                        for g in range(NG):
                            nkb = 4 * g + 4
                            for kb in range(nkb - 1, -1, -1):
                                steps.append((g, kb, kb == nkb - 1, kb == 0))

                        NS = len(steps)
                        sbanks = (0, 1)

                        def stA1(i):
                            g, kb, first, last = steps[i]
                            r = i % NR
                            sbk = sbanks[i % 2]
                            q0 = g * 512
                            mm(psb[sbk][:], kT[:, kb * 128:(kb + 1) * 128], qT[:, q0:q0 + 512],
                               [t_kT[kb // 4], t_qT[g]], pst[sbk])
                            act(eb[r][:], psb[sbk][:], AF.Exp, [pst[sbk]], [t_eb[r]])
                            act(spb[r][:], eb[r][:], AF.Ln, [t_eb[r]], [t_spb[r]], bias=1.0)

                        def stA2(i):
                            g, kb, first, last = steps[i]
                            r = i % NR
                            sbk = sbanks[i % 2]
                            tt("dve", t1b[r][:], psb[sbk][:], spb[r][:], ALU.subtract, [pst[sbk], t_spb[r]], [t_t1b[r]])
                            if kb >= 4 * g:
                                tt("pool", spb[r][:], spb[r][:], m01[kb - 4 * g][:], ALU.mult, [t_spb[r], t_msk], [t_spb[r]])
                            if not last:
                                lo, ln_ = i % 3, (i + 1) % 3
                                if first:
                                    vcopy("pool", lacc[ln_][:], spb[r][:], [t_spb[r]], [t_lacc[ln_]])
                                else:
                                    tt("pool", lacc[ln_][:], lacc[lo][:], spb[r][:], ALU.add, [t_spb[r], t_lacc[lo]], [t_lacc[ln_]])

                        def stB(i):
                            g, kb, first, last = steps[i]
                            r = i % NR
                            ubk = 2 + (i % 2)
                            mm(psb[ubk][:], ustrict_r[:], spb[r][:], [t_spb[r], t_msk], pst[ubk], start=True, stop=first)
                            if not first:
                                mm(psb[ubk][:], ones_r[:], lacc[i % 3][:], [t_lacc[i % 3], t_msk], pst[ubk], start=False, stop=True)
                            tt("dve", t1b[r][:], t1b[r][:], psb[ubk][:], ALU.subtract, [t_t1b[r], pst[ubk]], [t_t1b[r]])
                            if kb >= 4 * g:
                                tt("pool", t1b[r][:], t1b[r][:], mneg[kb - 4 * g][:], ALU.add, [t_t1b[r], t_msk], [t_t1b[r]])

                        def stC(i):
                            r = i % NR
                            act(aTb[r][:], t1b[r][:], AF.Exp, [t_t1b[r]], [t_aTb[r]])

                        def stD(i):
                            g, kb, first, last = steps[i]
                            r = i % NR
                            ob = 4
                            mm(psb[ob][:], vtm[:, kb, :], aTb[r][:], [t_aTb[r], t_v[kb]], pst[ob], start=first, stop=last)
                            if last:
                                acopy(osbT[:, h, g * 512:(g + 1) * 512], psb[ob][:], [pst[ob]], [t_osb[h][g]])

                        for n in range(-3, NS + 1):
                            for fn_, off in ((stA1, 3), (stA2, 2), (stB, 1), (stC, 0), (stD, -1)):
                                i = n + off
                                if 0 <= i < NS:
                                    fn_(i)
                            for _ in range(7):
                                if prepq:
                                    prepq.pop(0)()
                        while prepq:
                            prepq.pop(0)()
                    P.barrier()

                if "gdn" not in skip:
                  with ExitStack() as ph:
                    wab = sb("wab", [128, 8, 16], BF16, ph)
                    ab_sb = sb("ab_sb", [128, NT * 16], F32, ph)
                    sm = {n: sb("gs_" + n, [128, NT * 8], F32, ph) for n in ("tmp", "beta", "g", "gc", "ngc", "egc", "bg")}
                    t_gsm = Tok()
                    wg = sb("wg", [128, 8, 512], BF16, ph)
                    cst = sb("cst", [128, 3 + S], F32, ph)
                    cvo = sb("cvo", [128, S], F32, ph)
                    QT = sb("QT", [128, S], F32, ph)
                    KT = sb("KT", [128, S], F32, ph)
                    Ktm = sb("Ktm", [128, NT, 128], F32, ph)
                    Vtm = sb("Vtm", [128, NT, 128], F32, ph)
                    zs = sb("zs", [128, NT, 128], BF16, ph)
                    sq5 = sb("sq5", [128, 512], F32, ph)
                    rs5 = sb("rs5", [128, 512], F32, ph)
                    ztmp = sb("ztmp", [128, 128], F32, ph)
                    Sst = sb("Sst", [128, 128], F32, ph)
                    junk1 = sb("junk1", [128, 128], F32, ph)
                    names = ("dg", "tmpa", "tmpb", "dec", "decT", "M0", "M1", "MT0", "MT1", "PT", "attnT", "Vb", "Kbg",
                             "kw", "vcorr", "kcdT", "vnew", "o1", "o")
                    cb = {n: [sb(f"c_{n}{u}", [128, 128], F32, ph) for u in range(4 if n in ("attnT", "kw", "vcorr", "kcdT") else 2)]
                          for n in names}
                    og = [sb(f"c_og{u}", [128, 128], BF16, ph) for u in range(2)]
                    col = {n: [sb(f"k_{n}{u}", [128, 1], F32, ph) for u in range(4)] for n in ("glc", "egl", "ew", "oss")}
                    sbanks_g = ((0, 1), (2, 3), (4, 5, 6))
                    sidx = [0, 0, 0]

                    def pslot(st=2):
                        bi = sbanks_g[st][sidx[st] % len(sbanks_g[st])]
                        sidx[st] += 1
                        return psb[bi][:, 0:128], pst[bi]

                    def pbank(st=2):
                        bi = sbanks_g[st][sidx[st] % len(sbanks_g[st])]
                        sidx[st] += 1
                        return psb[bi], pst[bi]

                    def ck(k):
                        if stop == k:
                            P.dead = True

                    P.op("pool", lambda g: g.memset(cst[:, 0:3], 0.0), writes=[tk["cst"]])
                    P.dma("sp", wab[:], winb_d[:, OFF_GB:OFF_GB + 16].rearrange("(c p) n -> p c n", p=128), writes=[tk["wab"]])
                    for t in range(NT):
                        for c in range(8):
                            mm(psb[6][:, t * 16:(t + 1) * 16], hT[:, c, t * 128:(t + 1) * 128], wab[:, c, :],
                               [tk["wab"], t_hT[t]], pst[6], start=(c == 0), stop=(c == 7))
                    acopy(ab_sb[:], psb[6][:, 0:NT * 16], [pst[6]], [tk["ab"]])
                    ab3 = ab_sb[:].rearrange("p (t k) -> p t k", k=16)
                    v3 = lambda n: sm[n][:].rearrange("p (t k) -> p t k", k=8)
                    act(v3("tmp"), ab3[:, :, 0:8], AF.Exp, [tk["ab"]], [t_gsm], scale=-1.0)
                    ts("dve", sm["tmp"][:], sm["tmp"][:], 1.0, None, ALU.add, None, [t_gsm], [t_gsm])
                    P.op("dve", lambda v: v.reciprocal(out=sm["beta"][:], in_=sm["tmp"][:]), reads=[t_gsm], writes=[t_gsm])
                    tt("dve", v3("g"), ab3[:, :, 8:16], dtb[:].rearrange("p (t k) -> p t k", k=8), ALU.add,
                       [tk["ab"], tk["dtb"]], [t_gsm])
                    act(sm["g"][:], sm["g"][:], AF.Exp, [t_gsm], [t_gsm])
                    act(sm["g"][:], sm["g"][:], AF.Ln, [t_gsm], [t_gsm], bias=1.0)
                    tt("dve", sm["g"][:], sm["g"][:], nexpa[:], ALU.mult, [t_gsm, tk["nexpa"]], [t_gsm])
                    for t in range(NT):
                        mm(psb[6][:, 256 + t * 8:256 + (t + 1) * 8], tri_incl[:], sm["g"][:, t * 8:(t + 1) * 8],
                           [t_gsm, t_const], pst[6])
                    acopy(sm["gc"][:], psb[6][:, 256:256 + NT * 8], [pst[6]], [t_gsm])
                    ts("dve", sm["ngc"][:], sm["gc"][:], -1.0, None, ALU.mult, None, [t_gsm], [t_gsm])
                    act(sm["egc"][:], sm["gc"][:], AF.Exp, [t_gsm], [t_gsm])
                    tt("dve", sm["bg"][:], sm["beta"][:], sm["egc"][:], ALU.mult, [t_gsm], [t_gsm])

                    ck(1)
                    for h in range(NH):
                        for j, off in enumerate((OFF_GQ, OFF_GK, OFF_GV, OFF_GZ)):
                            src = winb_d[:, off + h * 128: off + (h + 1) * 128].rearrange("(c p) n -> p c n", p=128)
                            P.dma("sp", wg[:, :, j * 128:(j + 1) * 128], src, writes=[tk["wg"]])
                        for t in range(NT):
                            for c in range(8):
                                mm(psb[6][:, 0:128], hT[:, c, t * 128:(t + 1) * 128], wg[:, c, 384:512],
                                   [tk["wg"], t_hT[t]], pst[6], start=(c == 0), stop=(c == 7))
                            act(ztmp[:], psb[6][:, 0:128], AF.Silu, [pst[6]], [tk["ztmp"]])
                            tt("pool", zs[:, t, :], ztmp[:], gnw_b[:], ALU.mult, [tk["ztmp"], tk["gnw"]], [tk["zs", t]])
                        ck(2)
                        for which in range(3):
                            for g in range(NG):
                                bk = 5 + (g % 2)
                                for c in range(8):
                                    mm(psb[bk][:], wg[:, c, which * 128:(which + 1) * 128], hT[:, c, g * 512:(g + 1) * 512],
                                       [tk["wg"]] + t_hT[g * 4:(g + 1) * 4], pst[bk], start=(c == 0), stop=(c == 7))
                                acopy(cst[:, 3 + g * 512:3 + (g + 1) * 512], psb[bk][:], [pst[bk]], [tk["cst"]])
                            wc = lambda i: convw[:, (which * 8 + h) * 4 + i:(which * 8 + h) * 4 + i + 1]
                            ts("dve", cvo[:], cst[:, 3:3 + S], wc(3), None, ALU.mult, None, [tk["cst"], tk["convw"]], [tk["cvo"]])
                            for i in range(3):
                                stt("dve", cvo[:], cst[:, i:i + S], wc(i), cvo[:], ALU.mult, ALU.add,
                                    [tk["cst"], tk["convw"], tk["cvo"]], [tk["cvo"]])
                            act(cvo[:], cvo[:], AF.Silu, [tk["cvo"]], [tk["cvo"]])
                            if which < 2:
                                dst = QT if which == 0 else KT
                                for g in range(NG):
                                    gs = slice(g * 512, (g + 1) * 512)
                                    act(sq5[:], cvo[:, gs], AF.Square, [tk["cvo"]], [tk["sq5"]])
                                    mm(psb[5][:], ones_f[:], sq5[:], [tk["sq5"], t_const], pst[5])
                                    act(rs5[:], psb[5][:], AF.Ln, [pst[5]], [tk["rs5"]], bias=EPS)
                                    act(rs5[:], rs5[:], AF.Exp, [tk["rs5"]], [tk["rs5"]], scale=-0.5)
                                    if which == 0:
                                        stt("dve", dst[:, gs], cvo[:, gs], float(DH) ** -0.5, rs5[:], ALU.mult, ALU.mult,
                                            [tk["cvo"], tk["rs5"]], [tk["QT", g]])
                                    else:
                                        tt("dve", dst[:, gs], cvo[:, gs], rs5[:], ALU.mult, [tk["cvo"], tk["rs5"]], [tk["KT", g]])
                            else:
                                for t in range(NT):
                                    sl, ts_ = pslot()
                                    P.op("pe", lambda pe, sl=sl, t=t: pe.transpose(out=sl, in_=cvo[:, t * 128:(t + 1) * 128],
                                                                                 identity=ident_f[:]),
                                         reads=[tk["cvo"], t_const], writes=[ts_])
                                    acopy(Vtm[:, t, :], sl, [ts_], [tk["Vtm", t]])
                        ck(3)
                        for t in range(NT):
                            sl, ts_ = pslot()
                            P.op("pe", lambda pe, sl=sl, t=t: pe.transpose(out=sl, in_=KT[:, t * 128:(t + 1) * 128],
                                                                         identity=ident_f[:]),
                                 reads=[tk["KT", t // 4], t_const], writes=[ts_])
                            vcopy("dve", Ktm[:, t, :], sl, [ts_], [tk["Ktm", t]])
                        ck(4)
                        P.op("pool", lambda g: g.memset(Sst[:], 0.0), writes=[tk["S"]])

                        def chunk_env(t):
                            ci = t * 8 + h
                            e = dict(cs=slice(t * 128, (t + 1) * 128),
                                     gcc=sm["gc"][:, ci:ci + 1], ngcc=sm["ngc"][:, ci:ci + 1], betac=sm["beta"][:, ci:ci + 1],
                                     egcc=sm["egc"][:, ci:ci + 1], bgc=sm["bg"][:, ci:ci + 1],
                                     tQ=tk["QT", t // 4], tK=tk["KT", t // 4])
                            return e

                        HAND = ("attnT", "kw", "vcorr", "kcdT")

                        def tcomp(t):
                            e = chunk_env(t)
                            cs, gcc, ngcc, betac, bgc, tQ, tK = e["cs"], e["gcc"], e["ngcc"], e["betac"], e["bgc"], e["tQ"], e["tK"]
                            u = t % 2
                            u4 = t % 4
                            st = t % 2
                            B = lambda n: cb[n][u4 if n in HAND else u][:]
                            T = lambda n: tk[n, u4 if n in HAND else u]
                            egl_c, ew_c, glc_c = col["egl"][u4], col["ew"][u], col["glc"][u]
                            ts("dve", B("dg"), ident_f[:], gcc, None, ALU.mult, None, [t_gsm, t_const], [T("dg")])
                            gr, t_gr = pslot(st)
                            mm(gr, ones_f[:], B("dg"), [T("dg"), t_const], t_gr)
                            acopy(glc_c[:], gr[:, 127:128], [t_gr], [T("glc")])
                            act(egl_c[:], glc_c[:], AF.Exp, [T("glc")], [tk["egl", u4]])
                            act(ew_c[:], ngcc, AF.Exp, [T("glc"), t_gsm], [T("ew")], bias=glc_c[:, 0:1])
                            stt("dve", B("tmpa"), gr, -1.0, negstrict[:], ALU.mult, ALU.add, [t_gr, t_const], [T("tmpa")])
                            act(B("dec"), B("tmpa"), AF.Exp, [T("tmpa"), t_gsm], [T("dec")], bias=gcc)
                            tt("dve", B("tmpb"), gr, negtriu[:], ALU.add, [t_gr, t_const], [T("tmpb")])
                            act(B("decT"), B("tmpb"), AF.Exp, [T("tmpb"), t_gsm], [T("decT")], bias=ngcc)
                            kk, t_kk = pslot(st)
                            mm(kk, KT[:, cs], KT[:, cs], [tK], t_kk)
                            stt("dve", B("M0"), kk, betac, B("dec"), ALU.mult, ALU.mult, [t_kk, T("dec"), t_gsm], [T("M0")])
                            qk, t_qk = pslot(st)
                            mm(qk, KT[:, cs], QT[:, cs], [tK, tQ], t_qk)
                            tt("dve", B("attnT"), qk, B("decT"), ALU.mult, [t_qk, T("decT")], [T("attnT")])
                            lt, t_lt = pslot(st)
                            P.op("pe", lambda pe, lt=lt, u=u: pe.transpose(out=lt, in_=cb["M0"][u][:], identity=ident_f[:]),
                                 reads=[T("M0"), t_const], writes=[t_lt])
                            acopy(B("MT0"), lt, [t_lt], [T("MT0")])
                            tt("dve", B("PT"), ident_f[:], lt, ALU.subtract, [t_lt, t_const], [T("PT")])
                            cur = 0
                            for k in range(6):
                                nx = 1 - cur
                                Mc, MTc, Mn, MTn = f"M{cur}", f"MT{cur}", f"M{nx}", f"MT{nx}"
                                m2, t_m2 = pslot(st)
                                mm(m2, B(MTc), B(Mc), [T(MTc), T(Mc)], t_m2)
                                if k < 5:
                                    m2t, t_m2t = pslot(st)
                                    mm(m2t, B(Mc), B(MTc), [T(MTc), T(Mc)], t_m2t)
                                acopy(B(Mn), m2, [t_m2], [T(Mn)])
                                if k < 5:
                                    vcopy("dve", B(MTn), m2t, [t_m2t], [T(MTn)])
                                pp, t_pp = pslot(st)
                                mm(pp, B(Mn), B("PT"), [T(Mn), T("PT")], t_pp)
                                tt("dve", B("PT"), B("PT"), pp, ALU.add, [T("PT"), t_pp], [T("PT")])
                                cur = nx
                            ts("pool", B("Vb"), Vtm[:, t, :], betac, None, ALU.mult, None, [tk["Vtm", t], t_gsm], [T("Vb")])
                            ts("pool", B("Kbg"), Ktm[:, t, :], bgc, None, ALU.mult, None, [tk["Ktm", t], t_gsm], [T("Kbg")])
                            ts("pool", B("kw"), Ktm[:, t, :], ew_c[:, 0:1], None, ALU.mult, None,
                               [tk["Ktm", t], T("ew")], [T("kw")])
                            vc, t_vc = pslot(st)
                            mm(vc, B("PT"), B("Vb"), [T("PT"), T("Vb")], t_vc)
                            acopy(B("vcorr"), vc, [t_vc], [T("vcorr")])
                            kc, t_kc = pslot(st)
                            mm(kc, B("Kbg"), B("PT"), [T("PT"), T("Kbg")], t_kc)
                            vcopy("dve", B("kcdT"), kc, [t_kc], [T("kcdT")])

                        def rec(t):
                            e = chunk_env(t)
                            cs, egcc, tQ = e["cs"], e["egcc"], e["tQ"]
                            u = t % 2
                            u4 = t % 4
                            B = lambda n: cb[n][u4 if n in HAND else u][:]
                            T = lambda n: tk[n, u4 if n in HAND else u]
                            vn, t_vn = pslot(2)
                            mm(vn, B("kcdT"), Sst[:], [T("kcdT"), tk["S"]], t_vn)
                            tt("dve", B("vnew"), B("vcorr"), vn, ALU.subtract, [T("vcorr"), t_vn], [T("vnew")])
                            o1p, t_o1p = pslot(2)
                            mm(o1p, QT[:, cs], Sst[:], [tQ, tk["S"]], t_o1p)
                            act(B("o1"), o1p, AF.Identity, [t_o1p, t_gsm], [T("o1")], scale=egcc)
                            sup, t_sup = pslot(2)
                            mm(sup, B("kw"), B("vnew"), [T("kw"), T("vnew")], t_sup)
                            stt("dve", Sst[:], Sst[:], col["egl"][u4][:, 0:1], sup, ALU.mult, ALU.add,
                                [tk["S"], tk["egl", u4], t_sup], [tk["S"]])
                            o2p, t_o2p = pslot(2)
                            mm(o2p, B("attnT"), B("vnew"), [T("attnT"), T("vnew")], t_o2p)
                            tt("dve", B("o"), B("o1"), o2p, ALU.add, [T("o1"), t_o2p], [T("o")])
                            act(junk1[:], B("o"), AF.Square, [T("o")], [tk["junk1"], T("oss")], accum_out=col["oss"][u][:])
                            act(col["oss"][u][:], col["oss"][u][:], AF.Ln, [T("oss")], [T("oss")], scale=1.0 / DH, bias=EPS)
                            act(col["oss"][u][:], col["oss"][u][:], AF.Exp, [T("oss")], [T("oss")], scale=-0.5)
                            stt("dve", og[u][:], B("o"), col["oss"][u][:, 0:1], zs[:, t, :], ALU.mult, ALU.mult,
                                [T("o"), T("oss"), tk["zs", t]], [T("og")])
                            P.op("pe", lambda pe, u=u: pe.transpose(out=pstr[:, u, :], in_=og[u][:], identity=ident_b[:]),
                                 reads=[T("og"), t_const], writes=[t_pstr])
                            acopy(ogdnT[:, h, cs], pstr[:, u, :], [t_pstr], [t_ogdn[h][t]])

                        def record(fn_, *a_):
                            P.defq = []
                            fn_(*a_)
                            q_ = P.defq
                            P.defq = None
                            return q_

                        def zipdrain(qs, ws):
                            while any(qs):
                                for q_, w_ in zip(qs, ws):
                                    for _ in range(w_):
                                        if q_:
                                            q_.pop(0)()

                        zipdrain([record(tcomp, 0), record(tcomp, 1)], [1, 1])
                        for p_ in range(NT // 2):
                            qa = record(tcomp, 2 * p_ + 2) if 2 * p_ + 2 < NT else []
                            qb = record(tcomp, 2 * p_ + 3) if 2 * p_ + 3 < NT else []
                            qc = record(rec, 2 * p_) + record(rec, 2 * p_ + 1)
                            zipdrain([qa, qb, qc], [4, 4, 2])
                    P.dead = False
                    P.barrier()

                if stage in ("sb", "gdn"):
                    with ExitStack() as ph:
                        dbgo = sb("dbgo", [128, 512], F32, ph)
                        src_t = osbT if stage == "sb" else ogdnT
                        for h in range(NH):
                            for g in range(NG):
                                rd = [t_osb[h][g]] if stage == "sb" else t_ogdn[h][g * 4:(g + 1) * 4]
                                vcopy("dve", dbgo[:], src_t[:, h, g * 512:(g + 1) * 512], rd, [tk["dbgo"]])
                                P.dma("sp", dbg_d[b, h * 128:(h + 1) * 128, g * 512:(g + 1) * 512], dbgo[:], reads=[tk["dbgo"]])
                        P.barrier()
                    continue

                with ExitStack() as ph:
                    wout = sb("wout", [128, 8, D], BF16, ph)
                    P.dma("sp", wout[:], woutb_d.rearrange("(c p) n -> p c n", p=128), writes=[tk["wout"]])
                    wbs = [sb(f"wbs{i}", [128, 8, 128], BF16, ph) for i in range(2)]
                    wbg = [sb(f"wbg{i}", [128, 8, 128], BF16, ph) for i in range(2)]
                    wgt = [sb(f"wgt{i}", [128, 8, 256], BF16, ph) for i in range(2)]
                    mT = [sb(f"mT{i}", [128, 8, 512], BF16, ph) for i in range(2)]
                    e1 = [sb(f"e1_{i}", [128, 512], F32, ph) for i in range(2)]
                    e2 = [sb(f"e2_{i}", [128, 512], F32, ph) for i in range(2)]
                    xr = [sb(f"xr{i}", [128, D], F32, ph) for i in range(2)]
                    x1t = [sb(f"x1t{i}", [128, D], F32, ph) for i in range(2)]
                    it = 0
                    for g in range(NG):
                        gs = slice(g * 512, (g + 1) * 512)
                        gi = g % 2
                        for m in range(8):
                            w = it % 2
                            it += 1
                            ms = slice(m * 128, (m + 1) * 128)
                            P.dma("sp", wbs[w][:], wbsbb_d[:, ms].rearrange("(c p) n -> p c n", p=128), writes=[tk["wbs", w]])
                            P.dma("sp", wbg[w][:], wbgdnb_d[:, ms].rearrange("(c p) n -> p c n", p=128), writes=[tk["wbg", w]])
                            P.dma("sp", wgt[w][:, :, 0:128],
                                  winb_d[:, OFF_GSB + m * 128:OFF_GSB + (m + 1) * 128].rearrange("(c p) n -> p c n", p=128),
                                  writes=[tk["wgt", w]])
                            P.dma("sp", wgt[w][:, :, 128:256],
                                  winb_d[:, OFF_GGDN + m * 128:OFF_GGDN + (m + 1) * 128].rearrange("(c p) n -> p c n", p=128),
                                  writes=[tk["wgt", w]])
                            for c in range(8):
                                mm(psb[0][:], wbs[w][:, c, :], osbT[:, c, gs], [tk["wbs", w], t_osb[c][g]], pst[0],
                                   start=(c == 0), stop=(c == 7))
                            for c in range(8):
                                mm(psb[1][:], wbg[w][:, c, :], ogdnT[:, c, gs], [tk["wbg", w]] + t_ogdn[c][g * 4:(g + 1) * 4],
                                   pst[1], start=(c == 0), stop=(c == 7))
                            for c in range(8):
                                mm(psb[2][:], wgt[w][:, c, 0:128], hT[:, c, gs], [tk["wgt", w]] + t_hT[g * 4:(g + 1) * 4],
                                   pst[2], start=(c == 0), stop=(c == 7))
                            for c in range(8):
                                mm(psb[3][:], wgt[w][:, c, 128:256], hT[:, c, gs], [tk["wgt", w]] + t_hT[g * 4:(g + 1) * 4],
                                   pst[3], start=(c == 0), stop=(c == 7))
                            act(e1[w][:], psb[2][:], AF.Sigmoid, [pst[2]], [tk["e1", w]])
                            act(e2[w][:], psb[3][:], AF.Sigmoid, [pst[3]], [tk["e2", w]])
                            tt("dve", e1[w][:], e1[w][:], psb[0][:], ALU.mult, [tk["e1", w], pst[0]], [tk["e1", w]])
                            tt("dve", e2[w][:], e2[w][:], psb[1][:], ALU.mult, [tk["e2", w], pst[1]], [tk["e2", w]])
                            tt("pool", mT[gi][:, m, :], e1[w][:], e2[w][:], ALU.add, [tk["e1", w], tk["e2", w]], [tk["mT", gi]])
                        for tt_ in range(4):
                            t = g * 4 + tt_
                            xi = t % 2
                            r0 = b * S + t * 128
                            P.dma("sp", xr[xi][:], x_d[r0:r0 + 128, :], writes=[tk["xr", xi]])
                            for n in range(2):
                                bk = 4 + n
                                for m in range(8):
                                    mm(psb[bk][:], mT[gi][:, m, tt_ * 128:(tt_ + 1) * 128], wout[:, m, n * 512:(n + 1) * 512],
                                       [tk["mT", gi], tk["wout"]], pst[bk], start=(m == 0), stop=(m == 7))
                                tt("dve", x1t[xi][:, n * 512:(n + 1) * 512], xr[xi][:, n * 512:(n + 1) * 512], psb[bk][:], ALU.add,
                                   [tk["xr", xi], pst[bk]], [tk["x1t", xi]])
                            dst = out_d if stage == "mix" else x1_d
                            P.dma("sp", dst[r0:r0 + 128, :], x1t[xi][:], reads=[tk["x1t", xi]])
                    P.barrier()
            P.barrier()
        P.barrier()

        if stage == "peer":
            dbg = {n: nc.dram_tensor("dbg_" + n, [128, w], F32, kind="ExternalOutput").ap()
                   for n, w in (("esel", 128), ("gate", 128), ("v16", 256), ("i16f", 256))}
            dbg["nogather"] = "gather" in skip
            dbg["nodot"] = "nodot" in skip
            peer_phase(nc, P, tk, sb, ps, psb, pst, pstr, t_pstr, ident_b, ident_f, t_const, TOK,
                       x_d, out_d, ffnw_d, wqb_d, keysT_d, puvb_d, None, mm, act, acopy, vcopy, tt, ts, stt,
                       NTT=([int(k[3:]) for k in skip if k.startswith("ntt")] or [2])[0], dbg=dbg)
            P.barrier()
        if stage == "full":
            peer_phase(nc, P, tk, sb, ps, psb, pst, pstr, t_pstr, ident_b, ident_f, t_const, TOK,
                       x1_d, out_d, ffnw_d, wqb_d, keysT_d, puvb_d, None, mm, act, acopy, vcopy, tt, ts, stt)
            P.barrier()
        print("ninst", P.ninst, "nsem", P.nsem)
    return nc


def _prep_shared(inputs, NT):
    f = lambda k: np.ascontiguousarray(np.asarray(inputs[k], dtype=np.float32))
    cw = f("gdn_conv_w")[0]
    convw = cw.reshape(4, 3, 8, 128).transpose(3, 1, 2, 0).reshape(128, 96)
    k1 = f("peer_keys1")[0]
    k2 = f("peer_keys2")[0]
    keysT = np.stack([k1, k2], axis=1)
    keysT = keysT.transpose(3, 0, 1, 2).reshape(128, 16 * 128)
    return {
        "mix_norm_w": f("mix_norm_w").reshape(1, D),
        "ffn_norm_w": f("ffn_norm_w").reshape(1, D),
        "w_in": f("w_in")[0],
        "sb_q_norm_w": f("sb_q_norm_w").reshape(DH, 1),
        "sb_k_norm_w": f("sb_k_norm_w").reshape(DH, 1),
        "convw": np.ascontiguousarray(convw),
        "a_log_rep": np.ascontiguousarray(np.tile(f("gdn_a_log").reshape(1, 8), (1, NT))),
        "dtb_rep": np.ascontiguousarray(np.tile(f("gdn_dt_bias").reshape(1, 8), (1, NT))),
        "gdn_out_norm_w": f("gdn_out_norm_w").reshape(1, DH),
        "w_branch_sb": f("w_branch_sb")[0],
        "w_branch_gdn": f("w_branch_gdn")[0],
        "w_out": f("w_out")[0],
        "peer_w_q": f("peer_w_q")[0],
        "keysT": np.ascontiguousarray(keysT),
        "peer_u": f("peer_u")[0],
        "peer_v": f("peer_v")[0],
    }


def kernel(**inputs):
    x = np.asarray(inputs["x"], dtype=np.float32)
    B, S, _ = x.shape
    n = 8
    NB = B // n
    nc = build(NB=NB, S=S, stage="full")
    shared = _prep_shared(inputs, S // 128)
    in_maps = []
    for i in range(n):
        m = dict(shared)
        m["x"] = np.ascontiguousarray(x[i * NB:(i + 1) * NB].reshape(NB * S, D))
        in_maps.append(m)
    res = run_bass_kernel_spmd(nc, in_maps, core_ids=list(range(n)))
    outs = [np.asarray(r["out"]).reshape(NB, S, D) for r in res.results]
    return np.concatenate(outs, axis=0).astype(np.float32)
```

```python
import numpy as np
from contextlib import ExitStack
import concourse.bass as bass
import concourse.mybir as mybir
from concourse.bass_utils import run_bass_kernel_spmd

F32 = mybir.dt.float32
F32R = mybir.dt.float32r
BF16 = mybir.dt.bfloat16
I32 = mybir.dt.int32
U32 = mybir.dt.uint32
AF = mybir.ActivationFunctionType
ALU = mybir.AluOpType
AX = mybir.AxisListType

D = 1024
NH = 8
DH = 128
IN_W = 9232
EPS = 1e-6
OFF_SBQ, OFF_SBK, OFF_SBV = 0, 1024, 2048
OFF_GQ, OFF_GK, OFF_GV = 3072, 4096, 5120
OFF_GZ = 6144
OFF_GB = 7168
OFF_GA = 7176
OFF_GSB = 7184
OFF_GGDN = 8208
NEXP = 16384
NEG = -30000.0


class Tok:
    __slots__ = ("w", "r", "sc", "excl")

    def __init__(self, excl=False):
        self.w = None
        self.r = {}
        self.sc = None
        self.excl = excl


class SemCtr:
    LIMIT = 30000

    def __init__(self, prog, name):
        self.prog = prog
        self.name = name
        self.n = 0
        self.sem = None
        self.k = 0

    def next(self, inc):
        if self.sem is None or self.n + inc > self.LIMIT:
            self.sem = self.prog.es.enter_context(self.prog.nc.semaphore(f"{self.name}_{self.k}"))
            self.prog.nsem += 1
            self.k += 1
            self.n = 0
            self.prog.allsems.append(self)
        self.n += inc
        return (self.sem, self.n, self.name)

    def cur(self):
        if self.sem is None or self.n == 0:
            return None
        return (self.sem, self.n, self.name)


class Prog:
    def __init__(self, nc, es):
        self.nc = nc
        self.es = es
        self.engs = {"pe": nc.tensor, "act": nc.scalar, "dve": nc.vector, "pool": nc.gpsimd, "sp": nc.sync}
        self.nsem = 0
        self.allsems = []
        self.ctr = {k: SemCtr(self, "c" + k) for k in self.engs}
        self.seen = {k: {} for k in self.engs}
        self.ninst = 0
        self.dsems = []
        self.dead = False
        self.defq = None
        self.single = {}

    def _wait(self, e, tk, need=None):
        sem, val, _ = tk
        d = self.seen[e]
        key = id(sem)
        if d.get(key, 0) >= val:
            return
        d[key] = val
        if need is not None:
            need.append((sem, val))
            return
        self.engs[e].wait_ge(sem, val)
        self.ninst += 1

    def _deps(self, e, reads, writes, need=None):
        own = "c" + e
        for t in reads:
            if t.w is not None:
                if not (e == "pe" and t.w[2] == own):
                    self._wait(e, t.w, need)
            if t.excl:
                for tk in t.r.values():
                    if tk[2] != own:
                        self._wait(e, tk, need)
        for t in writes:
            if t.w is not None:
                if not (e == "pe" and t.w[2] == own):
                    self._wait(e, t.w, need)
            for tk in t.r.values():
                if tk[2] == own and e == "pe":
                    continue
                self._wait(e, tk, need)

    def _record(self, tk, reads, writes):
        for t in reads:
            t.r[id(tk[0])] = tk
        for t in writes:
            t.w = tk
            t.r = {}

    def op(self, e, fn, reads=(), writes=(), ke=None):
        if self.dead:
            return None
        if self.defq is not None:
            self.defq.append(lambda: self.op(e, fn, reads, writes, ke))
            return None
        need = []
        self._deps(e, reads, writes, need)
        code = getattr(fn, "__code__", None)
        ckey = (e, code, ke)
        attach = bool(need) and e != "pe" and self.single.get(ckey, False)
        for (sem_, val_) in (need[:-1] if attach else need):
            self.engs[e].wait_ge(sem_, val_)
            self.ninst += 1
        n0 = self.nc.n_instructions() if callable(getattr(self.nc, "n_instructions", None)) else None
        inst = fn(self.engs[e])
        if n0 is not None and ckey not in self.single:
            self.single[ckey] = (self.nc.n_instructions() - n0) == 1
        if attach:
            inst._wait_ge(need[-1][0], need[-1][1])
        tk = self.ctr[e].next(1)
        inst.then_inc(tk[0], 1)
        self.ninst += 1
        self._record(tk, reads, writes)
        return tk

    def dma(self, q, out, in_, reads=(), writes=(), sc=None, fn=None):
        if self.dead:
            return None
        if self.defq is not None:
            self.defq.append(lambda: self.dma(q, out, in_, reads, writes, sc, fn))
            return None
        self._deps(q, reads, writes)
        if sc is None:
            t0 = writes[0] if writes else reads[0]
            if t0.sc is None:
                t0.sc = SemCtr(self, "d%d" % len(self.dsems))
                self.dsems.append(t0.sc)
            sc = t0.sc
        if fn is None:
            inst = self.engs[q].dma_start(out=out, in_=in_)
        else:
            inst = fn(self.engs[q])
        tk = sc.next(16)
        inst.then_inc(tk[0], 16)
        self.ninst += 1
        self._record(tk, reads, writes)
        return tk

    def barrier(self, engines=None):
        toks = []
        seen = set()
        for sc in self.allsems:
            c = sc.cur()
            if c is not None and id(c[0]) not in seen:
                seen.add(id(c[0]))
                toks.append(c)
        for e in engines or self.engs:
            for tk in toks:
                if tk[2] == "c" + e:
                    continue
                self._wait(e, tk)


class Ctx:
    pass


def peer_phase(nc, P, tk, sb, ps, psb, pst, pstr, t_pstr, ident_b, ident_f, t_const, TOK,
               x1_d, out_d, ffnw_d, wqb_d, keysT_d, pu_d, pv_d, mm, act, acopy, vcopy, tt, ts, stt, NTT=None, dbg=None):
    NTT = NTT or TOK // 128
    NU = 20
    ND = 4
    G = 4
    with ExitStack() as ph:
        wq = sb("wq", [128, 8, 2048], BF16, ph)
        keysT = sb("keysT_s", [128, 16 * 128], F32, ph)
        ffnw_b = sb("ffnw_b", [128, D], F32, ph)
        iota_i = sb("iota_i", [128, 16], I32, ph)
        iota16 = sb("iota16", [128, 16], F32, ph)
        P.dma("sp", wq[:], wqb_d.rearrange("(c p) n -> p c n", p=128), writes=[tk["wq"]])
        P.dma("sp", keysT[:], keysT_d[:, :], writes=[tk["keysT"]])
        P.dma("sp", ffnw_b[:], ffnw_d[0:1, :].partition_broadcast(128), writes=[tk["ffnw"]])
        P.op("pool", lambda g: g.iota(iota_i[:], pattern=[[1, 16]], base=0, channel_multiplier=0), writes=[tk["iota"]])
        vcopy("pool", iota16[:], iota_i[:], [tk["iota"]], [tk["iota"]])

        acc = [sb(f"acc{i}", [128, D], F32, ph) for i in range(2)]
        h2 = sb("h2", [128, D], F32, ph)
        h2b = [sb(f"h2b{i}", [128, D], BF16, ph) for i in range(2)]
        junkb = sb("junkb", [128, D], BF16, ph)
        junkc = sb("junkc", [128, D], BF16, ph)
        prod = [sb(f"prod{i}", [128, D], BF16, ph) for i in range(3)]
        diag = [sb(f"diag{i}", [128, 128], BF16, ph) for i in range(ND)]
        h2T = sb("h2T", [128, 8, 128], BF16, ph)
        junk = sb("pjunk", [128, D], F32, ph)
        pss = sb("pss", [128, 1], F32, ph)
        qTs = sb("qTs", [128, 16 * 128], F32, ph)
        sc = sb("sc", [128, 16 * 128], F32, ph)
        sc2 = sb("sc2", [128, 128], F32, ph)
        v16 = sb("v16", [128, 256], F32, ph)
        i16u = sb("i16u", [128, 256], U32, ph)
        i16f = sb("i16f", [128, 256], F32, ph)
        cand = sb("cand", [128, 256], F32, ph)
        cand2 = sb("cand2", [128, 256], F32, ph)
        oh = sb("oh", [128, 256], F32, ph)
        c16 = sb("c16", [128, 16], F32, ph)
        ci16u = sb("ci16u", [128, 16], U32, ph)
        hlu = sb("hlu", [128, 32], U32, ph)
        hlf = sb("hlf", [128, 32], F32, ph)
        e12 = sb("e12", [128, 32], F32, ph)
        e16 = sb("e16", [128, 16], F32, ph)
        sml = sb("sml", [128, 4], F32, ph)
        c16a = sb("c16a", [128, 128], F32, ph)
        e128 = sb("e128", [128, 128], F32, ph)
        sm8 = sb("sm8", [128, 16], F32, ph)
        gate = [sb(f"gate{i}", [128, 128], F32, ph) for i in range(2)]
        esel = sb("esel", [128, 128], F32, ph)
        eidx = [sb(f"eidx{i}", [128, 128], I32, ph) for i in range(2)]
        pre = sb("pre", [128, 128], F32, ph)
        coef = sb("coef", [128, 128], F32, ph)
        ubuf = [sb(f"ubuf{i}", [128, 2 * D], BF16, ph) for i in range(NU)]
        banks_a = [0, 1]
        banks_b = [2, 3, 4]
        sidx = [0, 0]

        def bc3(tile, row, off, inner_first):
            if inner_first:
                return bass.AP(tile, off, [[row, 128], [1, 16], [0, 16]])
            return bass.AP(tile, off, [[row, 128], [0, 16], [1, 16]])

        breg = nc.gpsimd.alloc_register("bchk")
        nc.gpsimd.reg_mov(breg, NEXP - 1)
        gi = [0]

        def sel(tile_i):
            a = tile_i % 2
            r0 = tile_i * 128
            A = acc[a]
            P.dma("sp", A[:], x1_d[r0:r0 + 128, :], writes=[tk["acc", a]])
            P.op("dve", lambda v: v.scalar_tensor_tensor(out=junk[:], in0=A[:], scalar=1.0, in1=A[:], op0=ALU.mult, op1=ALU.mult,
                                                         accum_out=pss[:]), reads=[tk["acc", a]], writes=[tk["pjunk"], tk["pss"]])
            act(pss[:], pss[:], AF.Ln, [tk["pss"]], [tk["pss"]], scale=1.0 / D, bias=EPS)
            act(pss[:], pss[:], AF.Exp, [tk["pss"]], [tk["pss"]], scale=-0.5)
            stt("dve", h2[:], A[:], pss[:, 0:1], ffnw_b[:], ALU.mult, ALU.mult, [tk["acc", a], tk["pss"], tk["ffnw"]], [tk["h2"]])
            vcopy("pool", h2b[a][:], h2[:], [tk["h2"]], [tk["h2b", a]])
            for c in range(8):
                P.op("pe", lambda pe, c=c: pe.transpose(out=pstr[:, c, :], in_=h2b[a][:, c * 128:(c + 1) * 128], identity=ident_b[:]),
                     reads=[tk["h2b", a], t_const], writes=[t_pstr])
            vcopy("dve", h2T[:], pstr[:, :, :], [t_pstr], [tk["h2T"]])
            for jg in range(4):
                bk = banks_a[sidx[0] % len(banks_a)]
                sidx[0] += 1
                for jj in range(4):
                    j = jg * 4 + jj
                    for c in range(8):
                        mm(psb[bk][:, jj * 128:(jj + 1) * 128], wq[:, c, j * 128:(j + 1) * 128], h2T[:, c, :],
                           [tk["wq"], tk["h2T"]], pst[bk], start=(c == 0), stop=(c == 7))
                acopy(qTs[:, jg * 512:(jg + 1) * 512], psb[bk][:], [pst[bk]], [tk["qTs", jg]])
            for jg in range(4):
                bk = banks_b[sidx[1] % len(banks_b)]
                sidx[1] += 1
                for jj in range(4):
                    j = jg * 4 + jj
                    mm(psb[bk][:, jj * 128:(jj + 1) * 128], qTs[:, j * 128:(j + 1) * 128], keysT[:, j * 128:(j + 1) * 128],
                       [tk["qTs", jg], tk["keysT"]], pst[bk])
                acopy(sc[:, jg * 512:(jg + 1) * 512], psb[bk][:], [pst[bk]], [tk["sc", jg]])
            for j in range(16):
                scj = sc[:, j * 128:(j + 1) * 128]
                va = v16[:, j * 16:j * 16 + 8]
                vb_ = v16[:, j * 16 + 8:j * 16 + 16]
                P.op("dve", lambda v, va=va, scj=scj: v.max(out=va, in_=scj), reads=[tk["sc", j // 4]], writes=[tk["v16"]])
                P.op("dve", lambda v, va=va, scj=scj, j=j: v.max_index(out=i16u[:, j * 16:j * 16 + 8], in_max=va, in_values=scj),
                     reads=[tk["sc", j // 4], tk["v16"]], writes=[tk["i16u"]])
                P.op("dve", lambda v, va=va, scj=scj: v.match_replace(out=sc2[:], in_to_replace=va, in_values=scj, imm_value=-1e30),
                     reads=[tk["sc", j // 4], tk["v16"]], writes=[tk["sc2"]])
                P.op("dve", lambda v, vb_=vb_: v.max(out=vb_, in_=sc2[:]), reads=[tk["sc2"]], writes=[tk["v16"]])
                P.op("dve", lambda v, vb_=vb_, j=j: v.max_index(out=i16u[:, j * 16 + 8:j * 16 + 16], in_max=vb_, in_values=sc2[:]),
                     reads=[tk["sc2"], tk["v16"]], writes=[tk["i16u"]])
            vcopy("dve", i16f[:], i16u[:], [tk["i16u"]], [tk["i16f"]])
            for h in range(8):
                o1_, o2_ = (2 * h) * 16, (2 * h + 1) * 16
                c3 = cand[:].rearrange("p (i j) -> p i j", j=16)
                tt("dve", c3, bc3(v16, 256, o1_, True), bc3(v16, 256, o2_, False), ALU.add, [tk["v16"]], [tk["cand"]])
                P.op("dve", lambda v: v.max(out=c16[:, 0:8], in_=cand[:]), reads=[tk["cand"]], writes=[tk["c16"]])
                P.op("dve", lambda v: v.max_index(out=ci16u[:, 0:8], in_max=c16[:, 0:8], in_values=cand[:]),
                     reads=[tk["cand"], tk["c16"]], writes=[tk["ci16u"]])
                P.op("dve", lambda v: v.match_replace(out=cand2[:], in_to_replace=c16[:, 0:8], in_values=cand[:], imm_value=-1e30),
                     reads=[tk["cand"], tk["c16"]], writes=[tk["cand2"]])
                P.op("dve", lambda v: v.max(out=c16[:, 8:16], in_=cand2[:]), reads=[tk["cand2"]], writes=[tk["c16"]])
                P.op("dve", lambda v: v.max_index(out=ci16u[:, 8:16], in_max=c16[:, 8:16], in_values=cand2[:]),
                     reads=[tk["cand2"], tk["c16"]], writes=[tk["ci16u"]])
                vcopy("dve", c16a[:, h * 16:(h + 1) * 16], c16[:], [tk["c16"]], [tk["c16a"]])
                P.op("dve", lambda v: v.tensor_single_scalar(out=hlu[:, 0:16], in_=ci16u[:], scalar=4, op=ALU.logical_shift_right),
                     reads=[tk["ci16u"]], writes=[tk["hlu"]])
                P.op("dve", lambda v: v.tensor_single_scalar(out=hlu[:, 16:32], in_=ci16u[:], scalar=15, op=ALU.bitwise_and),
                     reads=[tk["ci16u"]], writes=[tk["hlu"]])
                vcopy("dve", hlf[:], hlu[:], [tk["hlu"]], [tk["hlf"]])
                o3 = oh[:].rearrange("p (k i) -> p k i", i=16)
                for half, off in ((0, o1_), (1, o2_)):
                    tt("dve", o3, bc3(iota16, 16, 0, False), bc3(hlf, 32, half * 16, True), ALU.is_equal,
                       [tk["iota"], tk["hlf"]], [tk["oh"]])
                    tt("dve", o3, o3, bc3(i16f, 256, off, False), ALU.mult, [tk["oh"], tk["i16f"]], [tk["oh"]])
                    P.op("dve", lambda v, half=half: v.tensor_reduce(out=e12[:, half * 16:(half + 1) * 16], in_=o3, axis=AX.X, op=ALU.add),
                         reads=[tk["oh"]], writes=[tk["e12"]])
                stt("dve", esel[:, h * 16:(h + 1) * 16], e12[:, 0:16], 128.0, e12[:, 16:32], ALU.mult, ALU.add,
                    [tk["e12"]], [tk["esel"]])
            c3a = c16a[:].rearrange("p (h k) -> p h k", k=16)
            e3a = e128[:].rearrange("p (h k) -> p h k", k=16)
            tt("dve", e3a, c3a, bass.AP(c16a, 0, [[128, 128], [16, 8], [0, 16]]), ALU.subtract, [tk["c16a"]], [tk["e128"]])
            act(e128[:], e128[:], AF.Exp, [tk["e128"]], [tk["e128"]])
            P.op("dve", lambda v: v.tensor_reduce(out=sm8[:, 0:8], in_=e3a, axis=AX.X, op=ALU.add), reads=[tk["e128"]], writes=[tk["sm8"]])
            P.op("dve", lambda v: v.reciprocal(out=sm8[:, 8:16], in_=sm8[:, 0:8]), reads=[tk["sm8"]], writes=[tk["sm8"]])
            tt("dve", gate[a][:].rearrange("p (h k) -> p h k", k=16), e3a, bass.AP(sm8, 8, [[16, 128], [1, 8], [0, 16]]), ALU.mult,
               [tk["e128"], tk["sm8"]], [tk["gate", a]])
            ei = eidx[a]
            vcopy("dve", ei[:], esel[:], [tk["esel"]], [tk["eidx", a]])
            if dbg is not None and tile_i == 0:
                P.dma("sp", dbg["esel"][:, :], esel[:], reads=[tk["esel"]])
                P.dma("sp", dbg["gate"][:, :], gate[a][:], reads=[tk["gate", a]])
                P.dma("sp", dbg["v16"][:, :], v16[:], reads=[tk["v16"]])
                P.dma("sp", dbg["i16f"][:, :], i16f[:], reads=[tk["i16f"]])

        def record(tile_i):
            P.defq = []
            sel(tile_i)
            q = P.defq
            P.defq = None
            return q

        def drain(q, n):
            while q and n > 0:
                q.pop(0)()
                n -= 1

        drain(record(0), 1 << 30)
        for tile_i in range(NTT):
            a = tile_i % 2
            r0 = tile_i * 128
            A = acc[a]
            ei = eidx[a]
            nq = record(tile_i + 1) if tile_i + 1 < NTT else []
            if dbg is not None and dbg.get("nogather"):
                continue
            slot_buf = {}

            def group_math(grp):
                cols = slice(grp * G, (grp + 1) * G)
                act(coef[:, cols], pre[:, cols], AF.Gelu, [tk["pre", grp]], [tk["coef", grp]])
                tt("dve", coef[:, cols], coef[:, cols], gate[a][:, cols], ALU.mult, [tk["coef", grp], tk["gate", a]], [tk["coef", grp]])
                for s in range(grp * G, (grp + 1) * G):
                    if dbg is not None and dbg.get("nodot"):
                        break
                    k = s % ND
                    ub = slot_buf[s]
                    act(diag[k][:], ident_b[:], AF.Identity, [tk["coef", grp], t_const], [tk["diag", k]], scale=coef[:, s:s + 1])
                    for n in range(2):
                        mm(psb[5 + n][:], diag[k][:], ubuf[ub][:, D + n * 512:D + (n + 1) * 512], [tk["diag", k], tk["ubuf", ub]],
                           pst[5 + n], start=(s == 0), stop=(s == 127))

            for grp in range(128 // G):
                for s in range(grp * G, (grp + 1) * G):
                    ub = gi[0] % NU
                    gi[0] += 1
                    slot_buf[s] = ub
                    P.dma("pool", None, None, reads=[tk["eidx", a]], writes=[tk["ubuf", ub]],
                          fn=lambda g, ub=ub, s=s, ei=ei: g.indirect_dma_start(
                              out=ubuf[ub][:], out_offset=None, in_=pu_d[:, :],
                              in_offset=bass.IndirectOffsetOnAxis(ap=ei[:, s:s + 1], axis=0),
                              bounds_check=breg, oob_is_err=False))
                    if dbg is not None and dbg.get("nodot"):
                        vcopy("dve", pre[:, s:s + 1], ubuf[ub][:, 0:1], [tk["ubuf", ub]], [tk["pre", grp]])
                    elif s % 4 != 3:
                        pk = s % 3
                        tt("dve", prod[pk][:], ubuf[ub][:, 0:D], h2b[a][:], ALU.mult, [tk["ubuf", ub], tk["h2b", a]], [tk["prod", pk]])
                        act(junkb[:], prod[pk][:], AF.Identity, [tk["prod", pk]], [tk["junkb"], tk["pre", grp]], accum_out=pre[:, s:s + 1])
                    else:
                        P.op("dve", lambda v, ub=ub, s=s, a=a: v.scalar_tensor_tensor(
                            out=junkc[:], in0=ubuf[ub][:, 0:D], scalar=1.0, in1=h2b[a][:], op0=ALU.mult, op1=ALU.mult,
                            accum_out=pre[:, s:s + 1]), reads=[tk["ubuf", ub], tk["h2b", a]], writes=[tk["junkc"], tk["pre", grp]])
                    drain(nq, 3)
                if grp > 0:
                    group_math(grp - 1)
            group_math(128 // G - 1)
            for n in range(2):
                tt("dve", A[:, n * 512:(n + 1) * 512], A[:, n * 512:(n + 1) * 512], psb[5 + n][:], ALU.add,
                   [tk["acc", a], pst[5 + n]], [tk["acc", a]])
            P.dma("sp", out_d[r0:r0 + 128, :], A[:], reads=[tk["acc", a]])
            drain(nq, 1 << 30)
        P.barrier()


class _Stop(Exception):
    pass


def build(NB=4, S=2048, stage="full", skip=(), stop=0):
    nc = bass.Bass("TRN2", target_bir_lowering=False)
    NT = S // 128
    NG = S // 512
    TOK = NB * S
    from collections import defaultdict

    def din(name, shape, dt=F32):
        return nc.dram_tensor(name, shape, dt, kind="ExternalInput").ap()

    x_d = din("x", [TOK, D])
    mixw_d = din("mix_norm_w", [1, D])
    ffnw_d = din("ffn_norm_w", [1, D])
    win_d = din("w_in", [D, IN_W])
    sbqw_d = din("sb_q_norm_w", [DH, 1])
    sbkw_d = din("sb_k_norm_w", [DH, 1])
    convw_d = din("convw", [128, 96])
    alog_d = din("a_log_rep", [1, NT * 8])
    dtb_d = din("dtb_rep", [1, NT * 8])
    gnw_d = din("gdn_out_norm_w", [1, DH])
    wbsb_d = din("w_branch_sb", [D, D])
    wbgdn_d = din("w_branch_gdn", [D, D])
    wout_d = din("w_out", [D, D])
    wq_d = din("peer_w_q", [D, 2048])
    keysT_d = din("keysT", [128, 16 * 128])
    pu_d = din("peer_u", [NEXP, D])
    pv_d = din("peer_v", [NEXP, D])
    if stage in ("sb", "gdn"):
        dbg_d = nc.dram_tensor("dbg", [NB, D, S], F32, kind="ExternalOutput").ap()
    else:
        out_d = nc.dram_tensor("out", [TOK, D], F32, kind="ExternalOutput").ap()

    winb_d = nc.dram_tensor("w_in_b", [D, IN_W], BF16, kind="Internal").ap()
    wbsbb_d = nc.dram_tensor("wbsb_b", [D, D], BF16, kind="Internal").ap()
    wbgdnb_d = nc.dram_tensor("wbgdn_b", [D, D], BF16, kind="Internal").ap()
    woutb_d = nc.dram_tensor("wout_b", [D, D], BF16, kind="Internal").ap()
    wqb_d = nc.dram_tensor("wq_b", [D, 2048], BF16, kind="Internal").ap()
    x1_d = nc.dram_tensor("x1_s", [TOK, D], F32, kind="Internal").ap()
    puvb_d = nc.dram_tensor("puv_b", [NEXP, 2 * D], BF16, kind="Internal").ap()

    es = ExitStack()
    with es:
        P = Prog(nc, es)
        tk = defaultdict(Tok)

        uid = [0]

        def sb(name, shape, dt, st=es):
            uid[0] += 1
            return st.enter_context(nc.sbuf_tensor(f"{name}_{uid[0]}", shape, dt))

        def ps(name, shape, dt=F32, st=es):
            return st.enter_context(nc.psum_tensor(name, shape, dt))

        def mm(out, lhsT, rhs, reads, wtok, start=True, stop=True):
            P.op("pe", lambda pe: pe.matmul(out, lhsT=lhsT, rhs=rhs, start=start, stop=stop),
                 reads=reads, writes=[wtok])

        def act(out, in_, func, reads, writes, **kw):
            ke = (str(func),) + tuple(sorted((k, type(v).__name__) for k, v in kw.items()))
            P.op("act", lambda a: a.activation(out=out, in_=in_, func=func, **kw), reads=reads, writes=writes, ke=ke)

        def acopy(out, in_, reads, writes):
            P.op("act", lambda a: a.copy(out=out, in_=in_), reads=reads, writes=writes)

        def vcopy(e, out, in_, reads, writes):
            P.op(e, lambda v: v.tensor_copy(out=out, in_=in_), reads=reads, writes=writes)

        def tt(e, out, in0, in1, op, reads, writes):
            P.op(e, lambda v: v.tensor_tensor(out=out, in0=in0, in1=in1, op=op), reads=reads, writes=writes, ke=(str(op),))

        def ts(e, out, in0, s1, s2, op0, op1, reads, writes):
            ke = (type(s1).__name__, type(s2).__name__, str(op0), str(op1))
            if s2 is None:
                P.op(e, lambda v: v.tensor_scalar(out=out, in0=in0, scalar1=s1, scalar2=None, op0=op0),
                     reads=reads, writes=writes, ke=ke)
            else:
                P.op(e, lambda v: v.tensor_scalar(out=out, in0=in0, scalar1=s1, scalar2=s2, op0=op0, op1=op1),
                     reads=reads, writes=writes, ke=ke)

        def stt(e, out, in0, scalar, in1, op0, op1, reads, writes):
            P.op(e, lambda v: v.scalar_tensor_tensor(out=out, in0=in0, scalar=scalar, in1=in1, op0=op0, op1=op1),
                 reads=reads, writes=writes, ke=(type(scalar).__name__, str(op0), str(op1)))

        ident_f = sb("ident_f", [128, 128], F32)
        ident_b = sb("ident_b", [128, 128], BF16)
        ones_f = sb("ones_f", [128, 128], F32)
        ustrict = sb("ustrict", [128, 128], F32)
        tri_incl = sb("tri_incl", [128, 128], F32)
        negstrict = sb("negstrict", [128, 128], F32)
        negtriu = sb("negtriu", [128, 128], F32)
        t_const = Tok()

        def amask(t, pattern, cm, op, fill, base=0, val=1.0):
            P.op("pool", lambda g: g.memset(t, val), writes=[t_const])
            P.op("pool", lambda g: g.affine_select(out=t, in_=t, pattern=pattern, compare_op=op, fill=fill,
                                                    base=base, channel_multiplier=cm),
                 reads=[t_const], writes=[t_const])

        P.op("pool", lambda g: g.memset(ones_f[:], 1.0), writes=[t_const])
        amask(ident_f[:], [[-1, 128]], 1, ALU.is_equal, 0.0)
        vcopy("pool", ident_b[:], ident_f[:], [t_const], [t_const])
        amask(ustrict[:], [[-1, 128]], 1, ALU.is_gt, 0.0)
        amask(tri_incl[:], [[1, 128]], -1, ALU.is_ge, 0.0)
        amask(negstrict[:], [[-1, 128]], 1, ALU.is_gt, NEG, val=0.0)
        amask(negtriu[:], [[1, 128]], -1, ALU.is_ge, NEG, val=0.0)

        mixw_b = sb("mixw_b", [128, D], F32)
        sbqw = sb("sbqw", [128, 1], F32)
        sbkw = sb("sbkw", [128, 1], F32)
        convw = sb("convw_s", [128, 96], F32)
        nexpa = sb("nexpa", [128, NT * 8], F32)
        dtb = sb("dtb", [128, NT * 8], F32)
        gnw_b = sb("gnw_b", [128, DH], F32)
        t_mixw = Tok()
        t_sbw = Tok()
        t_gw = Tok()
        P.dma("sp", mixw_b[:], mixw_d[0:1, :].partition_broadcast(128), writes=[t_mixw])
        P.dma("sp", sbqw[:], sbqw_d[:, :], writes=[t_sbw])
        P.dma("sp", sbkw[:], sbkw_d[:, :], writes=[tk["sbkw"]])
        P.dma("sp", convw[:], convw_d[:, :], writes=[tk["convw"]])
        P.dma("sp", nexpa[:], alog_d[0:1, :].partition_broadcast(128), writes=[tk["nexpa"]])
        P.dma("sp", dtb[:], dtb_d[0:1, :].partition_broadcast(128), writes=[tk["dtb"]])
        P.dma("sp", gnw_b[:], gnw_d[0:1, :].partition_broadcast(128), writes=[tk["gnw"]])
        ts("dve", sbqw[:], sbqw[:], float(DH) ** -0.5, None, ALU.mult, None, [t_sbw], [t_sbw])
        act(nexpa[:], nexpa[:], AF.Exp, [tk["nexpa"]], [tk["nexpa"]])
        ts("dve", nexpa[:], nexpa[:], -1.0, None, ALU.mult, None, [tk["nexpa"]], [tk["nexpa"]])

        psb = [ps(f"psb{i}", [128, 512], F32) for i in range(7)]
        pst = [Tok(excl=True) for _ in range(7)]
        pstr = ps("pstr", [128, 8, 128], BF16)
        t_pstr = Tok(excl=True)

        with ExitStack() as ph:
            CW = 2308
            stg = [sb(f"wstg{i}", [128, CW], F32, ph) for i in range(3)]
            stb = [sb(f"wstb{i}", [128, CW], BF16, ph) for i in range(3)]
            tg = [Tok() for _ in range(3)]
            tb = [Tok() for _ in range(3)]
            ceng = ["dve", "act", "dve"]
            it = 0
            jobs = []
            for c in range(8):
                rs_ = slice(c * 128, (c + 1) * 128)
                for j in range(IN_W // CW):
                    jobs.append((win_d[rs_, j * CW:(j + 1) * CW], winb_d[rs_, j * CW:(j + 1) * CW], CW, None))
                for (s_, d_) in ((wbsb_d, wbsbb_d), (wbgdn_d, wbgdnb_d), (wout_d, woutb_d)):
                    jobs.append((s_[rs_, :], d_[rs_, :], 1024, None))
                jobs.append((wq_d[rs_, :], wqb_d[rs_, :], 2048, None))
            if stage in ("full", "peer"):
                for (s_, c0_) in ((pu_d, 0), (pv_d, D)):
                    for r in range(NEXP // 256):
                        jobs.append((s_[r * 256:(r + 1) * 256, :].rearrange("(t p) d -> p t d", p=128),
                                     puvb_d[r * 256:(r + 1) * 256, c0_:c0_ + D].rearrange("(t p) d -> p t d", p=128), 2048, 2))
            for (s_, d_, w, t3) in jobs:
                i = it % 3
                sv, bv = stg[i][:, 0:w], stb[i][:, 0:w]
                if t3:
                    sv3 = sv.rearrange("p (t d) -> p t d", t=t3)
                    bv3 = bv.rearrange("p (t d) -> p t d", t=t3)
                else:
                    sv3, bv3 = sv, bv
                P.dma("sp", sv3, s_, writes=[tg[i]])
                e = ceng[it % 3]
                if e == "act":
                    acopy(bv, sv, [tg[i]], [tb[i]])
                else:
                    vcopy(e, bv, sv, [tg[i]], [tb[i]])
                P.dma("act", d_, bv3, reads=[tb[i]])
                it += 1
            P.barrier()

        with ExitStack() as mx:
            hT = sb("hT", [128, 8, S], BF16, mx)
            t_hT = [Tok() for _ in range(NT)]
            osbT = sb("osbT", [128, NH, S], BF16, mx)
            t_osb = [[Tok() for _ in range(NG)] for _ in range(NH)]
            ogdnT = sb("ogdnT", [128, NH, S], BF16, mx)
            t_ogdn = [[Tok() for _ in range(NT)] for _ in range(NH)]

            for b in range(NB if stage != "peer" else 0):
                with ExitStack() as ph:
                    xin = [sb(f"xin{i}", [128, D], F32, ph) for i in range(2)]
                    xn = [sb(f"xn{i}", [128, D], BF16, ph) for i in range(2)]
                    junk = sb("junk", [128, D], F32, ph)
                    ss = [sb(f"ss{i}", [128, 1], F32, ph) for i in range(2)]
                    for t in range(NT):
                        i = t % 2
                        r0 = b * S + t * 128
                        P.dma("sp", xin[i][:], x_d[r0:r0 + 128, :], writes=[tk["xin", i]])
                        act(junk[:], xin[i][:], AF.Square, [tk["xin", i]], [tk["junk"], tk["ss", i]], accum_out=ss[i][:])
                        act(ss[i][:], ss[i][:], AF.Ln, [tk["ss", i]], [tk["ss", i]], scale=1.0 / D, bias=EPS)
                        act(ss[i][:], ss[i][:], AF.Exp, [tk["ss", i]], [tk["ss", i]], scale=-0.5)
                        stt("dve", xn[i][:], xin[i][:], ss[i][:, 0:1], mixw_b[:], ALU.mult, ALU.mult,
                            [tk["xin", i], tk["ss", i], t_mixw], [tk["xn", i]])
                        for c in range(8):
                            P.op("pe", lambda pe, i=i, c=c: pe.transpose(out=pstr[:, c, :], in_=xn[i][:, c * 128:(c + 1) * 128],
                                                                         identity=ident_b[:]),
                                 reads=[tk["xn", i], t_const], writes=[t_pstr])
                        acopy(hT[:, :, t * 128:(t + 1) * 128], pstr[:, :, :], [t_pstr], [t_hT[t]])
                    P.barrier()

                if "sb" not in skip:
                  with ExitStack() as ph:
                    m01 = [sb(f"m01_{d}", [128, 512], F32, ph) for d in range(4)]
                    mneg = [sb(f"mneg_{d}", [128, 512], F32, ph) for d in range(4)]
                    t_msk = Tok()
                    for d in range(4):
                        P.op("pool", lambda g, d=d: g.memset(m01[d][:], 1.0), writes=[t_msk])
                        P.op("pool", lambda g, d=d: g.affine_select(out=m01[d][:], in_=m01[d][:], pattern=[[1, 512]],
                                                                     compare_op=ALU.is_gt, fill=0.0, base=-128 * d,
                                                                     channel_multiplier=-1), reads=[t_msk], writes=[t_msk])
                        ts("pool", mneg[d][:], m01[d][:], -1.0, -NEG, ALU.add, ALU.mult, [t_msk], [t_msk])
                    ustrict_r = sb("ustrict_r", [128, 128], F32R, ph)
                    ones_r = sb("ones_r", [128, 128], F32R, ph)
                    vcopy("pool", ustrict_r[:], ustrict[:], [t_const, t_msk], [t_msk])
                    vcopy("pool", ones_r[:], ones_f[:], [t_const, t_msk], [t_msk])
                    wsb = [sb(f"wsb{i}", [128, 8, 384], BF16, ph) for i in range(2)]
                    t_wsb = [Tok() for _ in range(2)]
                    qTs_ = [sb(f"qT{i}", [128, S], BF16, ph) for i in range(2)]
                    kTs_ = [sb(f"kT{i}", [128, S], BF16, ph) for i in range(2)]
                    t_qTs = [[Tok() for _ in range(NG)] for _ in range(2)]
                    t_kTs = [[Tok() for _ in range(NG)] for _ in range(2)]
                    vtms_ = [sb(f"vtm{i}", [128, NT, 128], BF16, ph) for i in range(2)]
                    t_vs = [[Tok() for _ in range(NT)] for _ in range(2)]
                    sqb = [sb(f"sqb{i}", [128, 512], F32, ph) for i in range(2)]
                    t_sqb = [Tok() for _ in range(2)]
                    rsb = [sb(f"rsb{i}", [128, 512], F32, ph) for i in range(2)]
                    t_rsb = [Tok() for _ in range(2)]
                    NR = 5
                    eb = [sb(f"eb{i}", [128, 512], F32, ph) for i in range(NR)]
                    spb = [sb(f"spb{i}", [128, 512], F32R, ph) for i in range(NR)]
                    t1b = [sb(f"t1b{i}", [128, 512], F32, ph) for i in range(NR)]
                    aTb = [sb(f"aTb{i}", [128, 512], BF16, ph) for i in range(NR)]
                    t_eb = [Tok() for _ in range(NR)]
                    t_spb = [Tok() for _ in range(NR)]
                    t_t1b = [Tok() for _ in range(NR)]
                    t_aTb = [Tok() for _ in range(NR)]
                    lacc = [sb(f"lacc{i}", [128, 512], F32R, ph) for i in range(3)]
                    t_lacc = [Tok() for _ in range(3)]
                    def sb_prep(h):
                        wi = h % 2
                        qT_, kT_, vtm_ = qTs_[wi], kTs_[wi], vtms_[wi]
                        for j, off in enumerate((OFF_SBQ, OFF_SBK, OFF_SBV)):
                            src = winb_d[:, off + h * 128: off + (h + 1) * 128].rearrange("(c p) n -> p c n", p=128)
                            P.dma("sp", wsb[wi][:, :, j * 128:(j + 1) * 128], src, writes=[t_wsb[wi]])
                        for which, dst, tdst, wcol, twc in ((0, qT_, t_qTs[wi], sbqw, t_sbw), (1, kT_, t_kTs[wi], sbkw, tk["sbkw"])):
                            for g in range(NG):
                                bk = (which * NG + g) % 2
                                for c in range(8):
                                    mm(psb[5][:], wsb[wi][:, c, which * 128:(which + 1) * 128],
                                       hT[:, c, g * 512:(g + 1) * 512], [t_wsb[wi]] + t_hT[g * 4:(g + 1) * 4], pst[5],
                                       start=(c == 0), stop=(c == 7))
                                act(sqb[bk][:], psb[5][:], AF.Square, [pst[5]], [t_sqb[bk]])
                                mm(psb[6][:], ones_f[:], sqb[bk][:], [t_sqb[bk], t_const], pst[6])
                                act(rsb[bk][:], psb[6][:], AF.Ln, [pst[6]], [t_rsb[bk]], scale=1.0 / DH, bias=EPS)
                                act(rsb[bk][:], rsb[bk][:], AF.Exp, [t_rsb[bk]], [t_rsb[bk]], scale=-0.5)
                                stt("dve", dst[:, g * 512:(g + 1) * 512], psb[5][:], wcol[:, 0:1], rsb[bk][:],
                                    ALU.mult, ALU.mult, [pst[5], t_rsb[bk], twc], [tdst[g]])
                        for t in range(NT):
                            bk = 5 + (t % 2)
                            for c in range(8):
                                mm(psb[bk][:, 0:128], hT[:, c, t * 128:(t + 1) * 128], wsb[wi][:, c, 256:384],
                                   [t_wsb[wi], t_hT[t]], pst[bk], start=(c == 0), stop=(c == 7))
                            acopy(vtm_[:, t, :], psb[bk][:, 0:128], [pst[bk]], [t_vs[wi][t]])

                    def sb_record(h):
                        P.defq = []
                        sb_prep(h)
                        q_ = P.defq
                        P.defq = None
                        return q_

                    for fn_ in sb_record(0):
                        fn_()
                    for h in range(NH):
                        wi = h % 2
                        qT, kT, vtm = qTs_[wi], kTs_[wi], vtms_[wi]
                        t_qT, t_kT, t_v = t_qTs[wi], t_kTs[wi], t_vs[wi]
                        prepq = sb_record(h + 1) if h + 1 < NH else []
                        steps = []
                        for g in range(NG):
                            nkb = 4 * g + 4
                            for kb in range(nkb - 1, -1, -1):
                                steps.append((g, kb, kb == nkb - 1, kb == 0))

                        NS = len(steps)
                        sbanks = (0, 1)

                        def stA1(i):
                            g, kb, first, last = steps[i]
                            r = i % NR
                            sbk = sbanks[i % 2]
                            q0 = g * 512
                            mm(psb[sbk][:], kT[:, kb * 128:(kb + 1) * 128], qT[:, q0:q0 + 512],
                               [t_kT[kb // 4], t_qT[g]], pst[sbk])
                            act(eb[r][:], psb[sbk][:], AF.Exp, [pst[sbk]], [t_eb[r]])
                            act(spb[r][:], eb[r][:], AF.Ln, [t_eb[r]], [t_spb[r]], bias=1.0)

                        def stA2(i):
                            g, kb, first, last = steps[i]
                            r = i % NR
                            sbk = sbanks[i % 2]
                            tt("dve", t1b[r][:], psb[sbk][:], spb[r][:], ALU.subtract, [pst[sbk], t_spb[r]], [t_t1b[r]])
                            if kb >= 4 * g:
                                tt("pool", spb[r][:], spb[r][:], m01[kb - 4 * g][:], ALU.mult, [t_spb[r], t_msk], [t_spb[r]])
                            if not last:
                                lo, ln_ = i % 3, (i + 1) % 3
                                if first:
                                    vcopy("pool", lacc[ln_][:], spb[r][:], [t_spb[r]], [t_lacc[ln_]])
                                else:
                                    tt("pool", lacc[ln_][:], lacc[lo][:], spb[r][:], ALU.add, [t_spb[r], t_lacc[lo]], [t_lacc[ln_]])

                        def stB(i):
                            g, kb, first, last = steps[i]
                            r = i % NR
                            ubk = 2 + (i % 2)
                            mm(psb[ubk][:], ustrict_r[:], spb[r][:], [t_spb[r], t_msk], pst[ubk], start=True, stop=first)
                            if not first:
                                mm(psb[ubk][:], ones_r[:], lacc[i % 3][:], [t_lacc[i % 3], t_msk], pst[ubk], start=False, stop=True)
                            tt("dve", t1b[r][:], t1b[r][:], psb[ubk][:], ALU.subtract, [t_t1b[r], pst[ubk]], [t_t1b[r]])
                            if kb >= 4 * g:
                                tt("pool", t1b[r][:], t1b[r][:], mneg[kb - 4 * g][:], ALU.add, [t_t1b[r], t_msk], [t_t1b[r]])

                        def stC(i):
                            r = i % NR
                            act(aTb[r][:], t1b[r][:], AF.Exp, [t_t1b[r]], [t_aTb[r]])

                        def stD(i):
                            g, kb, first, last = steps[i]
                            r = i % NR
                            ob = 4
                            mm(psb[ob][:], vtm[:, kb, :], aTb[r][:], [t_aTb[r], t_v[kb]], pst[ob], start=first, stop=last)
                            if last:
                                acopy(osbT[:, h, g * 512:(g + 1) * 512], psb[ob][:], [pst[ob]], [t_osb[h][g]])

                        for n in range(-3, NS + 1):
                            for fn_, off in ((stA1, 3), (stA2, 2), (stB, 1), (stC, 0), (stD, -1)):
                                i = n + off
                                if 0 <= i < NS:
                                    fn_(i)
                            for _ in range(7):
                                if prepq:
                                    prepq.pop(0)()
                        while prepq:
                            prepq.pop(0)()
                    P.barrier()

                if "gdn" not in skip:
                  with ExitStack() as ph:
                    wab = sb("wab", [128, 8, 16], BF16, ph)
                    ab_sb = sb("ab_sb", [128, NT * 16], F32, ph)
                    sm = {n: sb("gs_" + n, [128, NT * 8], F32, ph) for n in ("tmp", "beta", "g", "gc", "ngc", "egc", "bg")}
                    t_gsm = Tok()
                    wg = sb("wg", [128, 8, 512], BF16, ph)
                    cst = sb("cst", [128, 3 + S], F32, ph)
                    cvo = sb("cvo", [128, S], F32, ph)
                    QT = sb("QT", [128, S], F32, ph)
                    KT = sb("KT", [128, S], F32, ph)
                    Ktm = sb("Ktm", [128, NT, 128], F32, ph)
                    Vtm = sb("Vtm", [128, NT, 128], F32, ph)
                    zs = sb("zs", [128, NT, 128], BF16, ph)
                    sq5 = sb("sq5", [128, 512], F32, ph)
                    rs5 = sb("rs5", [128, 512], F32, ph)
                    ztmp = sb("ztmp", [128, 128], F32, ph)
                    Sst = sb("Sst", [128, 128], F32, ph)
                    junk1 = sb("junk1", [128, 128], F32, ph)
                    names = ("dg", "tmpa", "tmpb", "dec", "decT", "M0", "M1", "MT0", "MT1", "PT", "attnT", "Vb", "Kbg",
                             "kw", "vcorr", "kcdT", "vnew", "o1", "o")
                    cb = {n: [sb(f"c_{n}{u}", [128, 128], F32, ph) for u in range(4 if n in ("attnT", "kw", "vcorr", "kcdT") else 2)]
                          for n in names}
                    og = [sb(f"c_og{u}", [128, 128], BF16, ph) for u in range(2)]
                    col = {n: [sb(f"k_{n}{u}", [128, 1], F32, ph) for u in range(4)] for n in ("glc", "egl", "ew", "oss")}
                    sbanks_g = ((0, 1), (2, 3), (4, 5, 6))
                    sidx = [0, 0, 0]

                    def pslot(st=2):
                        bi = sbanks_g[st][sidx[st] % len(sbanks_g[st])]
                        sidx[st] += 1
                        return psb[bi][:, 0:128], pst[bi]

                    def pbank(st=2):
                        bi = sbanks_g[st][sidx[st] % len(sbanks_g[st])]
                        sidx[st] += 1
                        return psb[bi], pst[bi]

                    def ck(k):
                        if stop == k:
                            P.dead = True

                    P.op("pool", lambda g: g.memset(cst[:, 0:3], 0.0), writes=[tk["cst"]])
                    P.dma("sp", wab[:], winb_d[:, OFF_GB:OFF_GB + 16].rearrange("(c p) n -> p c n", p=128), writes=[tk["wab"]])
                    for t in range(NT):
                        for c in range(8):
                            mm(psb[6][:, t * 16:(t + 1) * 16], hT[:, c, t * 128:(t + 1) * 128], wab[:, c, :],
                               [tk["wab"], t_hT[t]], pst[6], start=(c == 0), stop=(c == 7))
                    acopy(ab_sb[:], psb[6][:, 0:NT * 16], [pst[6]], [tk["ab"]])
                    ab3 = ab_sb[:].rearrange("p (t k) -> p t k", k=16)
                    v3 = lambda n: sm[n][:].rearrange("p (t k) -> p t k", k=8)
                    act(v3("tmp"), ab3[:, :, 0:8], AF.Exp, [tk["ab"]], [t_gsm], scale=-1.0)
                    ts("dve", sm["tmp"][:], sm["tmp"][:], 1.0, None, ALU.add, None, [t_gsm], [t_gsm])
                    P.op("dve", lambda v: v.reciprocal(out=sm["beta"][:], in_=sm["tmp"][:]), reads=[t_gsm], writes=[t_gsm])
                    tt("dve", v3("g"), ab3[:, :, 8:16], dtb[:].rearrange("p (t k) -> p t k", k=8), ALU.add,
                       [tk["ab"], tk["dtb"]], [t_gsm])
                    act(sm["g"][:], sm["g"][:], AF.Exp, [t_gsm], [t_gsm])
                    act(sm["g"][:], sm["g"][:], AF.Ln, [t_gsm], [t_gsm], bias=1.0)
                    tt("dve", sm["g"][:], sm["g"][:], nexpa[:], ALU.mult, [t_gsm, tk["nexpa"]], [t_gsm])
                    for t in range(NT):
                        mm(psb[6][:, 256 + t * 8:256 + (t + 1) * 8], tri_incl[:], sm["g"][:, t * 8:(t + 1) * 8],
                           [t_gsm, t_const], pst[6])
                    acopy(sm["gc"][:], psb[6][:, 256:256 + NT * 8], [pst[6]], [t_gsm])
                    ts("dve", sm["ngc"][:], sm["gc"][:], -1.0, None, ALU.mult, None, [t_gsm], [t_gsm])
                    act(sm["egc"][:], sm["gc"][:], AF.Exp, [t_gsm], [t_gsm])
                    tt("dve", sm["bg"][:], sm["beta"][:], sm["egc"][:], ALU.mult, [t_gsm], [t_gsm])

                    ck(1)
                    for h in range(NH):
                        for j, off in enumerate((OFF_GQ, OFF_GK, OFF_GV, OFF_GZ)):
                            src = winb_d[:, off + h * 128: off + (h + 1) * 128].rearrange("(c p) n -> p c n", p=128)
                            P.dma("sp", wg[:, :, j * 128:(j + 1) * 128], src, writes=[tk["wg"]])
                        for t in range(NT):
                            for c in range(8):
                                mm(psb[6][:, 0:128], hT[:, c, t * 128:(t + 1) * 128], wg[:, c, 384:512],
                                   [tk["wg"], t_hT[t]], pst[6], start=(c == 0), stop=(c == 7))
                            act(ztmp[:], psb[6][:, 0:128], AF.Silu, [pst[6]], [tk["ztmp"]])
                            tt("pool", zs[:, t, :], ztmp[:], gnw_b[:], ALU.mult, [tk["ztmp"], tk["gnw"]], [tk["zs", t]])
                        ck(2)
                        for which in range(3):
                            for g in range(NG):
                                bk = 5 + (g % 2)
                                for c in range(8):
                                    mm(psb[bk][:], wg[:, c, which * 128:(which + 1) * 128], hT[:, c, g * 512:(g + 1) * 512],
                                       [tk["wg"]] + t_hT[g * 4:(g + 1) * 4], pst[bk], start=(c == 0), stop=(c == 7))
                                acopy(cst[:, 3 + g * 512:3 + (g + 1) * 512], psb[bk][:], [pst[bk]], [tk["cst"]])
                            wc = lambda i: convw[:, (which * 8 + h) * 4 + i:(which * 8 + h) * 4 + i + 1]
                            ts("dve", cvo[:], cst[:, 3:3 + S], wc(3), None, ALU.mult, None, [tk["cst"], tk["convw"]], [tk["cvo"]])
                            for i in range(3):
                                stt("dve", cvo[:], cst[:, i:i + S], wc(i), cvo[:], ALU.mult, ALU.add,
                                    [tk["cst"], tk["convw"], tk["cvo"]], [tk["cvo"]])
                            act(cvo[:], cvo[:], AF.Silu, [tk["cvo"]], [tk["cvo"]])
                            if which < 2:
                                dst = QT if which == 0 else KT
                                for g in range(NG):
                                    gs = slice(g * 512, (g + 1) * 512)
                                    act(sq5[:], cvo[:, gs], AF.Square, [tk["cvo"]], [tk["sq5"]])
                                    mm(psb[5][:], ones_f[:], sq5[:], [tk["sq5"], t_const], pst[5])
                                    act(rs5[:], psb[5][:], AF.Ln, [pst[5]], [tk["rs5"]], bias=EPS)
                                    act(rs5[:], rs5[:], AF.Exp, [tk["rs5"]], [tk["rs5"]], scale=-0.5)
                                    if which == 0:
                                        stt("dve", dst[:, gs], cvo[:, gs], float(DH) ** -0.5, rs5[:], ALU.mult, ALU.mult,
                                            [tk["cvo"], tk["rs5"]], [tk["QT", g]])
                                    else:
                                        tt("dve", dst[:, gs], cvo[:, gs], rs5[:], ALU.mult, [tk["cvo"], tk["rs5"]], [tk["KT", g]])
                            else:
                                for t in range(NT):
                                    sl, ts_ = pslot()
                                    P.op("pe", lambda pe, sl=sl, t=t: pe.transpose(out=sl, in_=cvo[:, t * 128:(t + 1) * 128],
                                                                                 identity=ident_f[:]),
                                         reads=[tk["cvo"], t_const], writes=[ts_])
                                    acopy(Vtm[:, t, :], sl, [ts_], [tk["Vtm", t]])
                        ck(3)
                        for t in range(NT):
                            sl, ts_ = pslot()
                            P.op("pe", lambda pe, sl=sl, t=t: pe.transpose(out=sl, in_=KT[:, t * 128:(t + 1) * 128],
                                                                         identity=ident_f[:]),
                                 reads=[tk["KT", t // 4], t_const], writes=[ts_])
                            vcopy("dve", Ktm[:, t, :], sl, [ts_], [tk["Ktm", t]])
                        ck(4)
                        P.op("pool", lambda g: g.memset(Sst[:], 0.0), writes=[tk["S"]])

                        def chunk_env(t):
                            ci = t * 8 + h
                            e = dict(cs=slice(t * 128, (t + 1) * 128),
                                     gcc=sm["gc"][:, ci:ci + 1], ngcc=sm["ngc"][:, ci:ci + 1], betac=sm["beta"][:, ci:ci + 1],
                                     egcc=sm["egc"][:, ci:ci + 1], bgc=sm["bg"][:, ci:ci + 1],
                                     tQ=tk["QT", t // 4], tK=tk["KT", t // 4])
                            return e

                        HAND = ("attnT", "kw", "vcorr", "kcdT")

                        def tcomp(t):
                            e = chunk_env(t)
                            cs, gcc, ngcc, betac, bgc, tQ, tK = e["cs"], e["gcc"], e["ngcc"], e["betac"], e["bgc"], e["tQ"], e["tK"]
                            u = t % 2
                            u4 = t % 4
                            st = t % 2
                            B = lambda n: cb[n][u4 if n in HAND else u][:]
                            T = lambda n: tk[n, u4 if n in HAND else u]
                            egl_c, ew_c, glc_c = col["egl"][u4], col["ew"][u], col["glc"][u]
                            ts("dve", B("dg"), ident_f[:], gcc, None, ALU.mult, None, [t_gsm, t_const], [T("dg")])
                            gr, t_gr = pslot(st)
                            mm(gr, ones_f[:], B("dg"), [T("dg"), t_const], t_gr)
                            acopy(glc_c[:], gr[:, 127:128], [t_gr], [T("glc")])
                            act(egl_c[:], glc_c[:], AF.Exp, [T("glc")], [tk["egl", u4]])
                            act(ew_c[:], ngcc, AF.Exp, [T("glc"), t_gsm], [T("ew")], bias=glc_c[:, 0:1])
                            stt("dve", B("tmpa"), gr, -1.0, negstrict[:], ALU.mult, ALU.add, [t_gr, t_const], [T("tmpa")])
                            act(B("dec"), B("tmpa"), AF.Exp, [T("tmpa"), t_gsm], [T("dec")], bias=gcc)
                            tt("dve", B("tmpb"), gr, negtriu[:], ALU.add, [t_gr, t_const], [T("tmpb")])
                            act(B("decT"), B("tmpb"), AF.Exp, [T("tmpb"), t_gsm], [T("decT")], bias=ngcc)
                            kk, t_kk = pslot(st)
                            mm(kk, KT[:, cs], KT[:, cs], [tK], t_kk)
                            stt("dve", B("M0"), kk, betac, B("dec"), ALU.mult, ALU.mult, [t_kk, T("dec"), t_gsm], [T("M0")])
                            qk, t_qk = pslot(st)
                            mm(qk, KT[:, cs], QT[:, cs], [tK, tQ], t_qk)
                            tt("dve", B("attnT"), qk, B("decT"), ALU.mult, [t_qk, T("decT")], [T("attnT")])
                            lt, t_lt = pslot(st)
                            P.op("pe", lambda pe, lt=lt, u=u: pe.transpose(out=lt, in_=cb["M0"][u][:], identity=ident_f[:]),
                                 reads=[T("M0"), t_const], writes=[t_lt])
                            acopy(B("MT0"), lt, [t_lt], [T("MT0")])
                            tt("dve", B("PT"), ident_f[:], lt, ALU.subtract, [t_lt, t_const], [T("PT")])
                            cur = 0
                            for k in range(6):
                                nx = 1 - cur
                                Mc, MTc, Mn, MTn = f"M{cur}", f"MT{cur}", f"M{nx}", f"MT{nx}"
                                m2, t_m2 = pslot(st)
                                mm(m2, B(MTc), B(Mc), [T(MTc), T(Mc)], t_m2)
                                if k < 5:
                                    m2t, t_m2t = pslot(st)
                                    mm(m2t, B(Mc), B(MTc), [T(MTc), T(Mc)], t_m2t)
                                acopy(B(Mn), m2, [t_m2], [T(Mn)])
                                if k < 5:
                                    vcopy("dve", B(MTn), m2t, [t_m2t], [T(MTn)])
                                pp, t_pp = pslot(st)
                                mm(pp, B(Mn), B("PT"), [T(Mn), T("PT")], t_pp)
                                tt("dve", B("PT"), B("PT"), pp, ALU.add, [T("PT"), t_pp], [T("PT")])
                                cur = nx
                            ts("pool", B("Vb"), Vtm[:, t, :], betac, None, ALU.mult, None, [tk["Vtm", t], t_gsm], [T("Vb")])
                            ts("pool", B("Kbg"), Ktm[:, t, :], bgc, None, ALU.mult, None, [tk["Ktm", t], t_gsm], [T("Kbg")])
                            ts("pool", B("kw"), Ktm[:, t, :], ew_c[:, 0:1], None, ALU.mult, None,
                               [tk["Ktm", t], T("ew")], [T("kw")])
                            vc, t_vc = pslot(st)
                            mm(vc, B("PT"), B("Vb"), [T("PT"), T("Vb")], t_vc)
                            acopy(B("vcorr"), vc, [t_vc], [T("vcorr")])
                            kc, t_kc = pslot(st)
                            mm(kc, B("Kbg"), B("PT"), [T("PT"), T("Kbg")], t_kc)
                            vcopy("dve", B("kcdT"), kc, [t_kc], [T("kcdT")])

                        def rec(t):
                            e = chunk_env(t)
                            cs, egcc, tQ = e["cs"], e["egcc"], e["tQ"]
                            u = t % 2
                            u4 = t % 4
                            B = lambda n: cb[n][u4 if n in HAND else u][:]
                            T = lambda n: tk[n, u4 if n in HAND else u]
                            vn, t_vn = pslot(2)
                            mm(vn, B("kcdT"), Sst[:], [T("kcdT"), tk["S"]], t_vn)
                            tt("dve", B("vnew"), B("vcorr"), vn, ALU.subtract, [T("vcorr"), t_vn], [T("vnew")])
                            o1p, t_o1p = pslot(2)
                            mm(o1p, QT[:, cs], Sst[:], [tQ, tk["S"]], t_o1p)
                            act(B("o1"), o1p, AF.Identity, [t_o1p, t_gsm], [T("o1")], scale=egcc)
                            sup, t_sup = pslot(2)
                            mm(sup, B("kw"), B("vnew"), [T("kw"), T("vnew")], t_sup)
                            stt("dve", Sst[:], Sst[:], col["egl"][u4][:, 0:1], sup, ALU.mult, ALU.add,
                                [tk["S"], tk["egl", u4], t_sup], [tk["S"]])
                            o2p, t_o2p = pslot(2)
                            mm(o2p, B("attnT"), B("vnew"), [T("attnT"), T("vnew")], t_o2p)
                            tt("dve", B("o"), B("o1"), o2p, ALU.add, [T("o1"), t_o2p], [T("o")])
                            act(junk1[:], B("o"), AF.Square, [T("o")], [tk["junk1"], T("oss")], accum_out=col["oss"][u][:])
                            act(col["oss"][u][:], col["oss"][u][:], AF.Ln, [T("oss")], [T("oss")], scale=1.0 / DH, bias=EPS)
                            act(col["oss"][u][:], col["oss"][u][:], AF.Exp, [T("oss")], [T("oss")], scale=-0.5)
                            stt("dve", og[u][:], B("o"), col["oss"][u][:, 0:1], zs[:, t, :], ALU.mult, ALU.mult,
                                [T("o"), T("oss"), tk["zs", t]], [T("og")])
                            P.op("pe", lambda pe, u=u: pe.transpose(out=pstr[:, u, :], in_=og[u][:], identity=ident_b[:]),
                                 reads=[T("og"), t_const], writes=[t_pstr])
                            acopy(ogdnT[:, h, cs], pstr[:, u, :], [t_pstr], [t_ogdn[h][t]])

                        def record(fn_, *a_):
                            P.defq = []
                            fn_(*a_)
                            q_ = P.defq
                            P.defq = None
                            return q_

                        def zipdrain(qs, ws):
                            while any(qs):
                                for q_, w_ in zip(qs, ws):
                                    for _ in range(w_):
                                        if q_:
                                            q_.pop(0)()

                        zipdrain([record(tcomp, 0), record(tcomp, 1)], [1, 1])
                        for p_ in range(NT // 2):
                            qa = record(tcomp, 2 * p_ + 2) if 2 * p_ + 2 < NT else []
                            qb = record(tcomp, 2 * p_ + 3) if 2 * p_ + 3 < NT else []
                            qc = record(rec, 2 * p_) + record(rec, 2 * p_ + 1)
                            zipdrain([qa, qb, qc], [4, 4, 2])
                    P.dead = False
                    P.barrier()

                if stage in ("sb", "gdn"):
                    with ExitStack() as ph:
                        dbgo = sb("dbgo", [128, 512], F32, ph)
                        src_t = osbT if stage == "sb" else ogdnT
                        for h in range(NH):
                            for g in range(NG):
                                rd = [t_osb[h][g]] if stage == "sb" else t_ogdn[h][g * 4:(g + 1) * 4]
                                vcopy("dve", dbgo[:], src_t[:, h, g * 512:(g + 1) * 512], rd, [tk["dbgo"]])
                                P.dma("sp", dbg_d[b, h * 128:(h + 1) * 128, g * 512:(g + 1) * 512], dbgo[:], reads=[tk["dbgo"]])
                        P.barrier()
                    continue

                with ExitStack() as ph:
                    wout = sb("wout", [128, 8, D], BF16, ph)
                    P.dma("sp", wout[:], woutb_d.rearrange("(c p) n -> p c n", p=128), writes=[tk["wout"]])
                    wbs = [sb(f"wbs{i}", [128, 8, 128], BF16, ph) for i in range(2)]
                    wbg = [sb(f"wbg{i}", [128, 8, 128], BF16, ph) for i in range(2)]
                    wgt = [sb(f"wgt{i}", [128, 8, 256], BF16, ph) for i in range(2)]
                    mT = [sb(f"mT{i}", [128, 8, 512], BF16, ph) for i in range(2)]
                    e1 = [sb(f"e1_{i}", [128, 512], F32, ph) for i in range(2)]
                    e2 = [sb(f"e2_{i}", [128, 512], F32, ph) for i in range(2)]
                    xr = [sb(f"xr{i}", [128, D], F32, ph) for i in range(2)]
                    x1t = [sb(f"x1t{i}", [128, D], F32, ph) for i in range(2)]
                    it = 0
                    for g in range(NG):
                        gs = slice(g * 512, (g + 1) * 512)
                        gi = g % 2
                        for m in range(8):
                            w = it % 2
                            it += 1
                            ms = slice(m * 128, (m + 1) * 128)
                            P.dma("sp", wbs[w][:], wbsbb_d[:, ms].rearrange("(c p) n -> p c n", p=128), writes=[tk["wbs", w]])
                            P.dma("sp", wbg[w][:], wbgdnb_d[:, ms].rearrange("(c p) n -> p c n", p=128), writes=[tk["wbg", w]])
                            P.dma("sp", wgt[w][:, :, 0:128],
                                  winb_d[:, OFF_GSB + m * 128:OFF_GSB + (m + 1) * 128].rearrange("(c p) n -> p c n", p=128),
                                  writes=[tk["wgt", w]])
                            P.dma("sp", wgt[w][:, :, 128:256],
                                  winb_d[:, OFF_GGDN + m * 128:OFF_GGDN + (m + 1) * 128].rearrange("(c p) n -> p c n", p=128),
                                  writes=[tk["wgt", w]])
                            for c in range(8):
                                mm(psb[0][:], wbs[w][:, c, :], osbT[:, c, gs], [tk["wbs", w], t_osb[c][g]], pst[0],
                                   start=(c == 0), stop=(c == 7))
                            for c in range(8):
                                mm(psb[1][:], wbg[w][:, c, :], ogdnT[:, c, gs], [tk["wbg", w]] + t_ogdn[c][g * 4:(g + 1) * 4],
                                   pst[1], start=(c == 0), stop=(c == 7))
                            for c in range(8):
                                mm(psb[2][:], wgt[w][:, c, 0:128], hT[:, c, gs], [tk["wgt", w]] + t_hT[g * 4:(g + 1) * 4],
                                   pst[2], start=(c == 0), stop=(c == 7))
                            for c in range(8):
                                mm(psb[3][:], wgt[w][:, c, 128:256], hT[:, c, gs], [tk["wgt", w]] + t_hT[g * 4:(g + 1) * 4],
                                   pst[3], start=(c == 0), stop=(c == 7))
                            act(e1[w][:], psb[2][:], AF.Sigmoid, [pst[2]], [tk["e1", w]])
                            act(e2[w][:], psb[3][:], AF.Sigmoid, [pst[3]], [tk["e2", w]])
                            tt("dve", e1[w][:], e1[w][:], psb[0][:], ALU.mult, [tk["e1", w], pst[0]], [tk["e1", w]])
                            tt("dve", e2[w][:], e2[w][:], psb[1][:], ALU.mult, [tk["e2", w], pst[1]], [tk["e2", w]])
                            tt("pool", mT[gi][:, m, :], e1[w][:], e2[w][:], ALU.add, [tk["e1", w], tk["e2", w]], [tk["mT", gi]])
                        for tt_ in range(4):
                            t = g * 4 + tt_
                            xi = t % 2
                            r0 = b * S + t * 128
                            P.dma("sp", xr[xi][:], x_d[r0:r0 + 128, :], writes=[tk["xr", xi]])
                            for n in range(2):
                                bk = 4 + n
                                for m in range(8):
                                    mm(psb[bk][:], mT[gi][:, m, tt_ * 128:(tt_ + 1) * 128], wout[:, m, n * 512:(n + 1) * 512],
                                       [tk["mT", gi], tk["wout"]], pst[bk], start=(m == 0), stop=(m == 7))
                                tt("dve", x1t[xi][:, n * 512:(n + 1) * 512], xr[xi][:, n * 512:(n + 1) * 512], psb[bk][:], ALU.add,
                                   [tk["xr", xi], pst[bk]], [tk["x1t", xi]])
                            dst = out_d if stage == "mix" else x1_d
                            P.dma("sp", dst[r0:r0 + 128, :], x1t[xi][:], reads=[tk["x1t", xi]])
                    P.barrier()
            P.barrier()
        P.barrier()

        if stage == "peer":
            dbg = {n: nc.dram_tensor("dbg_" + n, [128, w], F32, kind="ExternalOutput").ap()
                   for n, w in (("esel", 128), ("gate", 128), ("v16", 256), ("i16f", 256))}
            dbg["nogather"] = "gather" in skip
            dbg["nodot"] = "nodot" in skip
            peer_phase(nc, P, tk, sb, ps, psb, pst, pstr, t_pstr, ident_b, ident_f, t_const, TOK,
                       x_d, out_d, ffnw_d, wqb_d, keysT_d, puvb_d, None, mm, act, acopy, vcopy, tt, ts, stt,
                       NTT=([int(k[3:]) for k in skip if k.startswith("ntt")] or [2])[0], dbg=dbg)
            P.barrier()
        if stage == "full":
            peer_phase(nc, P, tk, sb, ps, psb, pst, pstr, t_pstr, ident_b, ident_f, t_const, TOK,
                       x1_d, out_d, ffnw_d, wqb_d, keysT_d, puvb_d, None, mm, act, acopy, vcopy, tt, ts, stt)
            P.barrier()
        print("ninst", P.ninst, "nsem", P.nsem)
    return nc


def _prep_shared(inputs, NT):
    f = lambda k: np.ascontiguousarray(np.asarray(inputs[k], dtype=np.float32))
    cw = f("gdn_conv_w")[0]
    convw = cw.reshape(4, 3, 8, 128).transpose(3, 1, 2, 0).reshape(128, 96)
    k1 = f("peer_keys1")[0]
    k2 = f("peer_keys2")[0]
    keysT = np.stack([k1, k2], axis=1)
    keysT = keysT.transpose(3, 0, 1, 2).reshape(128, 16 * 128)
    return {
        "mix_norm_w": f("mix_norm_w").reshape(1, D),
        "ffn_norm_w": f("ffn_norm_w").reshape(1, D),
        "w_in": f("w_in")[0],
        "sb_q_norm_w": f("sb_q_norm_w").reshape(DH, 1),
        "sb_k_norm_w": f("sb_k_norm_w").reshape(DH, 1),
        "convw": np.ascontiguousarray(convw),
        "a_log_rep": np.ascontiguousarray(np.tile(f("gdn_a_log").reshape(1, 8), (1, NT))),
        "dtb_rep": np.ascontiguousarray(np.tile(f("gdn_dt_bias").reshape(1, 8), (1, NT))),
        "gdn_out_norm_w": f("gdn_out_norm_w").reshape(1, DH),
        "w_branch_sb": f("w_branch_sb")[0],
        "w_branch_gdn": f("w_branch_gdn")[0],
        "w_out": f("w_out")[0],
        "peer_w_q": f("peer_w_q")[0],
        "keysT": np.ascontiguousarray(keysT),
        "peer_u": f("peer_u")[0],
        "peer_v": f("peer_v")[0],
    }


def kernel(**inputs):
    x = np.asarray(inputs["x"], dtype=np.float32)
    B, S, _ = x.shape
    n = 8
    NB = B // n
    nc = build(NB=NB, S=S, stage="full")
    shared = _prep_shared(inputs, S // 128)
    in_maps = []
    for i in range(n):
        m = dict(shared)
        m["x"] = np.ascontiguousarray(x[i * NB:(i + 1) * NB].reshape(NB * S, D))
        in_maps.append(m)
    res = run_bass_kernel_spmd(nc, in_maps, core_ids=list(range(n)))
    outs = [np.asarray(r["out"]).reshape(NB, S, D) for r in res.results]
    return np.concatenate(outs, axis=0).astype(np.float32)
```

```python
import numpy as np
from contextlib import ExitStack
import concourse.bass as bass
import concourse.mybir as mybir
from concourse.bass_utils import run_bass_kernel_spmd

F32 = mybir.dt.float32
F32R = mybir.dt.float32r
BF16 = mybir.dt.bfloat16
I32 = mybir.dt.int32
U32 = mybir.dt.uint32
AF = mybir.ActivationFunctionType
ALU = mybir.AluOpType
AX = mybir.AxisListType

D = 1024
NH = 8
DH = 128
IN_W = 9232
EPS = 1e-6
OFF_SBQ, OFF_SBK, OFF_SBV = 0, 1024, 2048
OFF_GQ, OFF_GK, OFF_GV = 3072, 4096, 5120
OFF_GZ = 6144
OFF_GB = 7168
OFF_GA = 7176
OFF_GSB = 7184
OFF_GGDN = 8208
NEXP = 16384
NEG = -30000.0


class Tok:
    __slots__ = ("w", "r", "sc", "excl")

    def __init__(self, excl=False):
        self.w = None
        self.r = {}
        self.sc = None
        self.excl = excl


class SemCtr:
    LIMIT = 30000

    def __init__(self, prog, name):
        self.prog = prog
        self.name = name
        self.n = 0
        self.sem = None
        self.k = 0

    def next(self, inc):
        if self.sem is None or self.n + inc > self.LIMIT:
            self.sem = self.prog.es.enter_context(self.prog.nc.semaphore(f"{self.name}_{self.k}"))
            self.prog.nsem += 1
            self.k += 1
            self.n = 0
            self.prog.allsems.append(self)
        self.n += inc
        return (self.sem, self.n, self.name)

    def cur(self):
        if self.sem is None or self.n == 0:
            return None
        return (self.sem, self.n, self.name)


class Prog:
    def __init__(self, nc, es):
        self.nc = nc
        self.es = es
        self.engs = {"pe": nc.tensor, "act": nc.scalar, "dve": nc.vector, "pool": nc.gpsimd, "sp": nc.sync}
        self.nsem = 0
        self.allsems = []
        self.ctr = {k: SemCtr(self, "c" + k) for k in self.engs}
        self.seen = {k: {} for k in self.engs}
        self.ninst = 0
        self.dsems = []
        self.dead = False
        self.defq = None
        self.single = {}

    def _wait(self, e, tk, need=None):
        sem, val, _ = tk
        d = self.seen[e]
        key = id(sem)
        if d.get(key, 0) >= val:
            return
        d[key] = val
        if need is not None:
            need.append((sem, val))
            return
        self.engs[e].wait_ge(sem, val)
        self.ninst += 1

    def _deps(self, e, reads, writes, need=None):
        own = "c" + e
        for t in reads:
            if t.w is not None:
                if not (e == "pe" and t.w[2] == own):
                    self._wait(e, t.w, need)
            if t.excl:
                for tk in t.r.values():
                    if tk[2] != own:
                        self._wait(e, tk, need)
        for t in writes:
            if t.w is not None:
                if not (e == "pe" and t.w[2] == own):
                    self._wait(e, t.w, need)
            for tk in t.r.values():
                if tk[2] == own and e == "pe":
                    continue
                self._wait(e, tk, need)

    def _record(self, tk, reads, writes):
        for t in reads:
            t.r[id(tk[0])] = tk
        for t in writes:
            t.w = tk
            t.r = {}

    def op(self, e, fn, reads=(), writes=(), ke=None):
        if self.dead:
            return None
        if self.defq is not None:
            self.defq.append(lambda: self.op(e, fn, reads, writes, ke))
            return None
        need = []
        self._deps(e, reads, writes, need)
        code = getattr(fn, "__code__", None)
        ckey = (e, code, ke)
        attach = bool(need) and e != "pe" and self.single.get(ckey, False)
        for (sem_, val_) in (need[:-1] if attach else need):
            self.engs[e].wait_ge(sem_, val_)
            self.ninst += 1
        n0 = self.nc.n_instructions() if callable(getattr(self.nc, "n_instructions", None)) else None
        inst = fn(self.engs[e])
        if n0 is not None and ckey not in self.single:
            self.single[ckey] = (self.nc.n_instructions() - n0) == 1
        if attach:
            inst._wait_ge(need[-1][0], need[-1][1])
        tk = self.ctr[e].next(1)
        inst.then_inc(tk[0], 1)
        self.ninst += 1
        self._record(tk, reads, writes)
        return tk

    def dma(self, q, out, in_, reads=(), writes=(), sc=None, fn=None):
        if self.dead:
            return None
        if self.defq is not None:
            self.defq.append(lambda: self.dma(q, out, in_, reads, writes, sc, fn))
            return None
        self._deps(q, reads, writes)
        if sc is None:
            t0 = writes[0] if writes else reads[0]
            if t0.sc is None:
                t0.sc = SemCtr(self, "d%d" % len(self.dsems))
                self.dsems.append(t0.sc)
            sc = t0.sc
        if fn is None:
            inst = self.engs[q].dma_start(out=out, in_=in_)
        else:
            inst = fn(self.engs[q])
        tk = sc.next(16)
        inst.then_inc(tk[0], 16)
        self.ninst += 1
        self._record(tk, reads, writes)
        return tk

    def barrier(self, engines=None):
        toks = []
        seen = set()
        for sc in self.allsems:
            c = sc.cur()
            if c is not None and id(c[0]) not in seen:
                seen.add(id(c[0]))
                toks.append(c)
        for e in engines or self.engs:
            for tk in toks:
                if tk[2] == "c" + e:
                    continue
                self._wait(e, tk)


class Ctx:
    pass


def peer_phase(nc, P, tk, sb, ps, psb, pst, pstr, t_pstr, ident_b, ident_f, t_const, TOK,
               x1_d, out_d, ffnw_d, wqb_d, keysT_d, pu_d, pv_d, mm, act, acopy, vcopy, tt, ts, stt, NTT=None, dbg=None):
    NTT = NTT or TOK // 128
    NU = 20
    ND = 4
    G = 4
    with ExitStack() as ph:
        wq = sb("wq", [128, 8, 2048], BF16, ph)
        keysT = sb("keysT_s", [128, 16 * 128], F32, ph)
        ffnw_b = sb("ffnw_b", [128, D], F32, ph)
        iota_i = sb("iota_i", [128, 16], I32, ph)
        iota16 = sb("iota16", [128, 16], F32, ph)
        P.dma("sp", wq[:], wqb_d.rearrange("(c p) n -> p c n", p=128), writes=[tk["wq"]])
        P.dma("sp", keysT[:], keysT_d[:, :], writes=[tk["keysT"]])
        P.dma("sp", ffnw_b[:], ffnw_d[0:1, :].partition_broadcast(128), writes=[tk["ffnw"]])
        P.op("pool", lambda g: g.iota(iota_i[:], pattern=[[1, 16]], base=0, channel_multiplier=0), writes=[tk["iota"]])
        vcopy("pool", iota16[:], iota_i[:], [tk["iota"]], [tk["iota"]])

        acc = [sb(f"acc{i}", [128, D], F32, ph) for i in range(2)]
        h2 = sb("h2", [128, D], F32, ph)
        h2b = [sb(f"h2b{i}", [128, D], BF16, ph) for i in range(2)]
        junkb = sb("junkb", [128, D], BF16, ph)
        junkc = sb("junkc", [128, D], BF16, ph)
        prod = [sb(f"prod{i}", [128, D], BF16, ph) for i in range(3)]
        diag = [sb(f"diag{i}", [128, 128], BF16, ph) for i in range(ND)]
        h2T = sb("h2T", [128, 8, 128], BF16, ph)
        junk = sb("pjunk", [128, D], F32, ph)
        pss = sb("pss", [128, 1], F32, ph)
        qTs = sb("qTs", [128, 16 * 128], F32, ph)
        sc = sb("sc", [128, 16 * 128], F32, ph)
        sc2 = sb("sc2", [128, 128], F32, ph)
        v16 = sb("v16", [128, 256], F32, ph)
        i16u = sb("i16u", [128, 256], U32, ph)
        i16f = sb("i16f", [128, 256], F32, ph)
        cand = sb("cand", [128, 256], F32, ph)
        cand2 = sb("cand2", [128, 256], F32, ph)
        oh = sb("oh", [128, 256], F32, ph)
        c16 = sb("c16", [128, 16], F32, ph)
        ci16u = sb("ci16u", [128, 16], U32, ph)
        hlu = sb("hlu", [128, 32], U32, ph)
        hlf = sb("hlf", [128, 32], F32, ph)
        e12 = sb("e12", [128, 32], F32, ph)
        e16 = sb("e16", [128, 16], F32, ph)
        sml = sb("sml", [128, 4], F32, ph)
        c16a = sb("c16a", [128, 128], F32, ph)
        e128 = sb("e128", [128, 128], F32, ph)
        sm8 = sb("sm8", [128, 16], F32, ph)
        gate = [sb(f"gate{i}", [128, 128], F32, ph) for i in range(2)]
        esel = sb("esel", [128, 128], F32, ph)
        eidx = [sb(f"eidx{i}", [128, 128], I32, ph) for i in range(2)]
        pre = sb("pre", [128, 128], F32, ph)
        coef = sb("coef", [128, 128], F32, ph)
        ubuf = [sb(f"ubuf{i}", [128, 2 * D], BF16, ph) for i in range(NU)]
        banks_a = [0, 1]
        banks_b = [2, 3, 4]
        sidx = [0, 0]

        def bc3(tile, row, off, inner_first):
            if inner_first:
                return bass.AP(tile, off, [[row, 128], [1, 16], [0, 16]])
            return bass.AP(tile, off, [[row, 128], [0, 16], [1, 16]])

        breg = nc.gpsimd.alloc_register("bchk")
        nc.gpsimd.reg_mov(breg, NEXP - 1)
        gi = [0]

        def sel(tile_i):
            a = tile_i % 2
            r0 = tile_i * 128
            A = acc[a]
            P.dma("sp", A[:], x1_d[r0:r0 + 128, :], writes=[tk["acc", a]])
            P.op("dve", lambda v: v.scalar_tensor_tensor(out=junk[:], in0=A[:], scalar=1.0, in1=A[:], op0=ALU.mult, op1=ALU.mult,
                                                         accum_out=pss[:]), reads=[tk["acc", a]], writes=[tk["pjunk"], tk["pss"]])
            act(pss[:], pss[:], AF.Ln, [tk["pss"]], [tk["pss"]], scale=1.0 / D, bias=EPS)
            act(pss[:], pss[:], AF.Exp, [tk["pss"]], [tk["pss"]], scale=-0.5)
            stt("dve", h2[:], A[:], pss[:, 0:1], ffnw_b[:], ALU.mult, ALU.mult, [tk["acc", a], tk["pss"], tk["ffnw"]], [tk["h2"]])
            vcopy("pool", h2b[a][:], h2[:], [tk["h2"]], [tk["h2b", a]])
            for c in range(8):
                P.op("pe", lambda pe, c=c: pe.transpose(out=pstr[:, c, :], in_=h2b[a][:, c * 128:(c + 1) * 128], identity=ident_b[:]),
                     reads=[tk["h2b", a], t_const], writes=[t_pstr])
            vcopy("dve", h2T[:], pstr[:, :, :], [t_pstr], [tk["h2T"]])
            for jg in range(4):
                bk = banks_a[sidx[0] % len(banks_a)]
                sidx[0] += 1
                for jj in range(4):
                    j = jg * 4 + jj
                    for c in range(8):
                        mm(psb[bk][:, jj * 128:(jj + 1) * 128], wq[:, c, j * 128:(j + 1) * 128], h2T[:, c, :],
                           [tk["wq"], tk["h2T"]], pst[bk], start=(c == 0), stop=(c == 7))
                acopy(qTs[:, jg * 512:(jg + 1) * 512], psb[bk][:], [pst[bk]], [tk["qTs", jg]])
            for jg in range(4):
                bk = banks_b[sidx[1] % len(banks_b)]
                sidx[1] += 1
                for jj in range(4):
                    j = jg * 4 + jj
                    mm(psb[bk][:, jj * 128:(jj + 1) * 128], qTs[:, j * 128:(j + 1) * 128], keysT[:, j * 128:(j + 1) * 128],
                       [tk["qTs", jg], tk["keysT"]], pst[bk])
                acopy(sc[:, jg * 512:(jg + 1) * 512], psb[bk][:], [pst[bk]], [tk["sc", jg]])
            for j in range(16):
                scj = sc[:, j * 128:(j + 1) * 128]
                va = v16[:, j * 16:j * 16 + 8]
                vb_ = v16[:, j * 16 + 8:j * 16 + 16]
                P.op("dve", lambda v, va=va, scj=scj: v.max(out=va, in_=scj), reads=[tk["sc", j // 4]], writes=[tk["v16"]])
                P.op("dve", lambda v, va=va, scj=scj, j=j: v.max_index(out=i16u[:, j * 16:j * 16 + 8], in_max=va, in_values=scj),
                     reads=[tk["sc", j // 4], tk["v16"]], writes=[tk["i16u"]])
                P.op("dve", lambda v, va=va, scj=scj: v.match_replace(out=sc2[:], in_to_replace=va, in_values=scj, imm_value=-1e30),
                     reads=[tk["sc", j // 4], tk["v16"]], writes=[tk["sc2"]])
                P.op("dve", lambda v, vb_=vb_: v.max(out=vb_, in_=sc2[:]), reads=[tk["sc2"]], writes=[tk["v16"]])
                P.op("dve", lambda v, vb_=vb_, j=j: v.max_index(out=i16u[:, j * 16 + 8:j * 16 + 16], in_max=vb_, in_values=sc2[:]),
                     reads=[tk["sc2"], tk["v16"]], writes=[tk["i16u"]])
            vcopy("dve", i16f[:], i16u[:], [tk["i16u"]], [tk["i16f"]])
            for h in range(8):
                o1_, o2_ = (2 * h) * 16, (2 * h + 1) * 16
                c3 = cand[:].rearrange("p (i j) -> p i j", j=16)
                tt("dve", c3, bc3(v16, 256, o1_, True), bc3(v16, 256, o2_, False), ALU.add, [tk["v16"]], [tk["cand"]])
                P.op("dve", lambda v: v.max(out=c16[:, 0:8], in_=cand[:]), reads=[tk["cand"]], writes=[tk["c16"]])
                P.op("dve", lambda v: v.max_index(out=ci16u[:, 0:8], in_max=c16[:, 0:8], in_values=cand[:]),
                     reads=[tk["cand"], tk["c16"]], writes=[tk["ci16u"]])
                P.op("dve", lambda v: v.match_replace(out=cand2[:], in_to_replace=c16[:, 0:8], in_values=cand[:], imm_value=-1e30),
                     reads=[tk["cand"], tk["c16"]], writes=[tk["cand2"]])
                P.op("dve", lambda v: v.max(out=c16[:, 8:16], in_=cand2[:]), reads=[tk["cand2"]], writes=[tk["c16"]])
                P.op("dve", lambda v: v.max_index(out=ci16u[:, 8:16], in_max=c16[:, 8:16], in_values=cand2[:]),
                     reads=[tk["cand2"], tk["c16"]], writes=[tk["ci16u"]])
                vcopy("dve", c16a[:, h * 16:(h + 1) * 16], c16[:], [tk["c16"]], [tk["c16a"]])
                P.op("dve", lambda v: v.tensor_single_scalar(out=hlu[:, 0:16], in_=ci16u[:], scalar=4, op=ALU.logical_shift_right),
                     reads=[tk["ci16u"]], writes=[tk["hlu"]])
                P.op("dve", lambda v: v.tensor_single_scalar(out=hlu[:, 16:32], in_=ci16u[:], scalar=15, op=ALU.bitwise_and),
                     reads=[tk["ci16u"]], writes=[tk["hlu"]])
                vcopy("dve", hlf[:], hlu[:], [tk["hlu"]], [tk["hlf"]])
                o3 = oh[:].rearrange("p (k i) -> p k i", i=16)
                for half, off in ((0, o1_), (1, o2_)):
                    tt("dve", o3, bc3(iota16, 16, 0, False), bc3(hlf, 32, half * 16, True), ALU.is_equal,
                       [tk["iota"], tk["hlf"]], [tk["oh"]])
                    tt("dve", o3, o3, bc3(i16f, 256, off, False), ALU.mult, [tk["oh"], tk["i16f"]], [tk["oh"]])
                    P.op("dve", lambda v, half=half: v.tensor_reduce(out=e12[:, half * 16:(half + 1) * 16], in_=o3, axis=AX.X, op=ALU.add),
                         reads=[tk["oh"]], writes=[tk["e12"]])
                stt("dve", esel[:, h * 16:(h + 1) * 16], e12[:, 0:16], 128.0, e12[:, 16:32], ALU.mult, ALU.add,
                    [tk["e12"]], [tk["esel"]])
            c3a = c16a[:].rearrange("p (h k) -> p h k", k=16)
            e3a = e128[:].rearrange("p (h k) -> p h k", k=16)
            tt("dve", e3a, c3a, bass.AP(c16a, 0, [[128, 128], [16, 8], [0, 16]]), ALU.subtract, [tk["c16a"]], [tk["e128"]])
            act(e128[:], e128[:], AF.Exp, [tk["e128"]], [tk["e128"]])
            P.op("dve", lambda v: v.tensor_reduce(out=sm8[:, 0:8], in_=e3a, axis=AX.X, op=ALU.add), reads=[tk["e128"]], writes=[tk["sm8"]])
            P.op("dve", lambda v: v.reciprocal(out=sm8[:, 8:16], in_=sm8[:, 0:8]), reads=[tk["sm8"]], writes=[tk["sm8"]])
            tt("dve", gate[a][:].rearrange("p (h k) -> p h k", k=16), e3a, bass.AP(sm8, 8, [[16, 128], [1, 8], [0, 16]]), ALU.mult,
               [tk["e128"], tk["sm8"]], [tk["gate", a]])
            ei = eidx[a]
            vcopy("dve", ei[:], esel[:], [tk["esel"]], [tk["eidx", a]])
            if dbg is not None and tile_i == 0:
                P.dma("sp", dbg["esel"][:, :], esel[:], reads=[tk["esel"]])
                P.dma("sp", dbg["gate"][:, :], gate[a][:], reads=[tk["gate", a]])
                P.dma("sp", dbg["v16"][:, :], v16[:], reads=[tk["v16"]])
                P.dma("sp", dbg["i16f"][:, :], i16f[:], reads=[tk["i16f"]])

        def record(tile_i):
            P.defq = []
            sel(tile_i)
            q = P.defq
            P.defq = None
            return q

        def drain(q, n):
            while q and n > 0:
                q.pop(0)()
                n -= 1

        drain(record(0), 1 << 30)
        for tile_i in range(NTT):
            a = tile_i % 2
            r0 = tile_i * 128
            A = acc[a]
            ei = eidx[a]
            nq = record(tile_i + 1) if tile_i + 1 < NTT else []
            if dbg is not None and dbg.get("nogather"):
                continue
            slot_buf = {}

            def group_math(grp):
                cols = slice(grp * G, (grp + 1) * G)
                act(coef[:, cols], pre[:, cols], AF.Gelu, [tk["pre", grp]], [tk["coef", grp]])
                tt("dve", coef[:, cols], coef[:, cols], gate[a][:, cols], ALU.mult, [tk["coef", grp], tk["gate", a]], [tk["coef", grp]])
                for s in range(grp * G, (grp + 1) * G):
                    if dbg is not None and dbg.get("nodot"):
                        break
                    k = s % ND
                    ub = slot_buf[s]
                    act(diag[k][:], ident_b[:], AF.Identity, [tk["coef", grp], t_const], [tk["diag", k]], scale=coef[:, s:s + 1])
                    for n in range(2):
                        mm(psb[5 + n][:], diag[k][:], ubuf[ub][:, D + n * 512:D + (n + 1) * 512], [tk["diag", k], tk["ubuf", ub]],
                           pst[5 + n], start=(s == 0), stop=(s == 127))

            for grp in range(128 // G):
                for s in range(grp * G, (grp + 1) * G):
                    ub = gi[0] % NU
                    gi[0] += 1
                    slot_buf[s] = ub
                    P.dma("pool", None, None, reads=[tk["eidx", a]], writes=[tk["ubuf", ub]],
                          fn=lambda g, ub=ub, s=s, ei=ei: g.indirect_dma_start(
                              out=ubuf[ub][:], out_offset=None, in_=pu_d[:, :],
                              in_offset=bass.IndirectOffsetOnAxis(ap=ei[:, s:s + 1], axis=0),
                              bounds_check=breg, oob_is_err=False))
                    if dbg is not None and dbg.get("nodot"):
                        vcopy("dve", pre[:, s:s + 1], ubuf[ub][:, 0:1], [tk["ubuf", ub]], [tk["pre", grp]])
                    elif s % 8 != 7:
                        pk = s % 3
                        tt("dve", prod[pk][:], ubuf[ub][:, 0:D], h2b[a][:], ALU.mult, [tk["ubuf", ub], tk["h2b", a]], [tk["prod", pk]])
                        act(junkb[:], prod[pk][:], AF.Identity, [tk["prod", pk]], [tk["junkb"], tk["pre", grp]], accum_out=pre[:, s:s + 1])
                    else:
                        P.op("dve", lambda v, ub=ub, s=s, a=a: v.scalar_tensor_tensor(
                            out=junkc[:], in0=ubuf[ub][:, 0:D], scalar=1.0, in1=h2b[a][:], op0=ALU.mult, op1=ALU.mult,
                            accum_out=pre[:, s:s + 1]), reads=[tk["ubuf", ub], tk["h2b", a]], writes=[tk["junkc"], tk["pre", grp]])
                    drain(nq, 3)
                if grp > 0:
                    group_math(grp - 1)
            group_math(128 // G - 1)
            for n in range(2):
                tt("dve", A[:, n * 512:(n + 1) * 512], A[:, n * 512:(n + 1) * 512], psb[5 + n][:], ALU.add,
                   [tk["acc", a], pst[5 + n]], [tk["acc", a]])
            P.dma("sp", out_d[r0:r0 + 128, :], A[:], reads=[tk["acc", a]])
            drain(nq, 1 << 30)
        P.barrier()


class _Stop(Exception):
    pass


def build(NB=4, S=2048, stage="full", skip=(), stop=0):
    nc = bass.Bass("TRN2", target_bir_lowering=False)
    NT = S // 128
    NG = S // 512
    TOK = NB * S
    from collections import defaultdict

    def din(name, shape, dt=F32):
        return nc.dram_tensor(name, shape, dt, kind="ExternalInput").ap()

    x_d = din("x", [TOK, D])
    mixw_d = din("mix_norm_w", [1, D])
    ffnw_d = din("ffn_norm_w", [1, D])
    win_d = din("w_in", [D, IN_W])
    sbqw_d = din("sb_q_norm_w", [DH, 1])
    sbkw_d = din("sb_k_norm_w", [DH, 1])
    convw_d = din("convw", [128, 96])
    alog_d = din("a_log_rep", [1, NT * 8])
    dtb_d = din("dtb_rep", [1, NT * 8])
    gnw_d = din("gdn_out_norm_w", [1, DH])
    wbsb_d = din("w_branch_sb", [D, D])
    wbgdn_d = din("w_branch_gdn", [D, D])
    wout_d = din("w_out", [D, D])
    wq_d = din("peer_w_q", [D, 2048])
    keysT_d = din("keysT", [128, 16 * 128])
    pu_d = din("peer_u", [NEXP, D])
    pv_d = din("peer_v", [NEXP, D])
    if stage in ("sb", "gdn"):
        dbg_d = nc.dram_tensor("dbg", [NB, D, S], F32, kind="ExternalOutput").ap()
    else:
        out_d = nc.dram_tensor("out", [TOK, D], F32, kind="ExternalOutput").ap()

    winb_d = nc.dram_tensor("w_in_b", [D, IN_W], BF16, kind="Internal").ap()
    wbsbb_d = nc.dram_tensor("wbsb_b", [D, D], BF16, kind="Internal").ap()
    wbgdnb_d = nc.dram_tensor("wbgdn_b", [D, D], BF16, kind="Internal").ap()
    woutb_d = nc.dram_tensor("wout_b", [D, D], BF16, kind="Internal").ap()
    wqb_d = nc.dram_tensor("wq_b", [D, 2048], BF16, kind="Internal").ap()
    x1_d = nc.dram_tensor("x1_s", [TOK, D], F32, kind="Internal").ap()
    puvb_d = nc.dram_tensor("puv_b", [NEXP, 2 * D], BF16, kind="Internal").ap()

    es = ExitStack()
    with es:
        P = Prog(nc, es)
        tk = defaultdict(Tok)

        uid = [0]

        def sb(name, shape, dt, st=es):
            uid[0] += 1
            return st.enter_context(nc.sbuf_tensor(f"{name}_{uid[0]}", shape, dt))

        def ps(name, shape, dt=F32, st=es):
            return st.enter_context(nc.psum_tensor(name, shape, dt))

        def mm(out, lhsT, rhs, reads, wtok, start=True, stop=True):
            P.op("pe", lambda pe: pe.matmul(out, lhsT=lhsT, rhs=rhs, start=start, stop=stop),
                 reads=reads, writes=[wtok])

        def act(out, in_, func, reads, writes, **kw):
            ke = (str(func),) + tuple(sorted((k, type(v).__name__) for k, v in kw.items()))
            P.op("act", lambda a: a.activation(out=out, in_=in_, func=func, **kw), reads=reads, writes=writes, ke=ke)

        def acopy(out, in_, reads, writes):
            P.op("act", lambda a: a.copy(out=out, in_=in_), reads=reads, writes=writes)

        def vcopy(e, out, in_, reads, writes):
            P.op(e, lambda v: v.tensor_copy(out=out, in_=in_), reads=reads, writes=writes)

        def tt(e, out, in0, in1, op, reads, writes):
            P.op(e, lambda v: v.tensor_tensor(out=out, in0=in0, in1=in1, op=op), reads=reads, writes=writes, ke=(str(op),))

        def ts(e, out, in0, s1, s2, op0, op1, reads, writes):
            ke = (type(s1).__name__, type(s2).__name__, str(op0), str(op1))
            if s2 is None:
                P.op(e, lambda v: v.tensor_scalar(out=out, in0=in0, scalar1=s1, scalar2=None, op0=op0),
                     reads=reads, writes=writes, ke=ke)
            else:
                P.op(e, lambda v: v.tensor_scalar(out=out, in0=in0, scalar1=s1, scalar2=s2, op0=op0, op1=op1),
                     reads=reads, writes=writes, ke=ke)

        def stt(e, out, in0, scalar, in1, op0, op1, reads, writes):
            P.op(e, lambda v: v.scalar_tensor_tensor(out=out, in0=in0, scalar=scalar, in1=in1, op0=op0, op1=op1),
                 reads=reads, writes=writes, ke=(type(scalar).__name__, str(op0), str(op1)))

        ident_f = sb("ident_f", [128, 128], F32)
        ident_b = sb("ident_b", [128, 128], BF16)
        ones_f = sb("ones_f", [128, 128], F32)
        ustrict = sb("ustrict", [128, 128], F32)
        tri_incl = sb("tri_incl", [128, 128], F32)
        negstrict = sb("negstrict", [128, 128], F32)
        negtriu = sb("negtriu", [128, 128], F32)
        t_const = Tok()

        def amask(t, pattern, cm, op, fill, base=0, val=1.0):
            P.op("pool", lambda g: g.memset(t, val), writes=[t_const])
            P.op("pool", lambda g: g.affine_select(out=t, in_=t, pattern=pattern, compare_op=op, fill=fill,
                                                    base=base, channel_multiplier=cm),
                 reads=[t_const], writes=[t_const])

        P.op("pool", lambda g: g.memset(ones_f[:], 1.0), writes=[t_const])
        amask(ident_f[:], [[-1, 128]], 1, ALU.is_equal, 0.0)
        vcopy("pool", ident_b[:], ident_f[:], [t_const], [t_const])
        amask(ustrict[:], [[-1, 128]], 1, ALU.is_gt, 0.0)
        amask(tri_incl[:], [[1, 128]], -1, ALU.is_ge, 0.0)
        amask(negstrict[:], [[-1, 128]], 1, ALU.is_gt, NEG, val=0.0)
        amask(negtriu[:], [[1, 128]], -1, ALU.is_ge, NEG, val=0.0)

        mixw_b = sb("mixw_b", [128, D], F32)
        sbqw = sb("sbqw", [128, 1], F32)
        sbkw = sb("sbkw", [128, 1], F32)
        convw = sb("convw_s", [128, 96], F32)
        nexpa = sb("nexpa", [128, NT * 8], F32)
        dtb = sb("dtb", [128, NT * 8], F32)
        gnw_b = sb("gnw_b", [128, DH], F32)
        t_mixw = Tok()
        t_sbw = Tok()
        t_gw = Tok()
        P.dma("sp", mixw_b[:], mixw_d[0:1, :].partition_broadcast(128), writes=[t_mixw])
        P.dma("sp", sbqw[:], sbqw_d[:, :], writes=[t_sbw])
        P.dma("sp", sbkw[:], sbkw_d[:, :], writes=[tk["sbkw"]])
        P.dma("sp", convw[:], convw_d[:, :], writes=[tk["convw"]])
        P.dma("sp", nexpa[:], alog_d[0:1, :].partition_broadcast(128), writes=[tk["nexpa"]])
        P.dma("sp", dtb[:], dtb_d[0:1, :].partition_broadcast(128), writes=[tk["dtb"]])
        P.dma("sp", gnw_b[:], gnw_d[0:1, :].partition_broadcast(128), writes=[tk["gnw"]])
        ts("dve", sbqw[:], sbqw[:], float(DH) ** -0.5, None, ALU.mult, None, [t_sbw], [t_sbw])
        act(nexpa[:], nexpa[:], AF.Exp, [tk["nexpa"]], [tk["nexpa"]])
        ts("dve", nexpa[:], nexpa[:], -1.0, None, ALU.mult, None, [tk["nexpa"]], [tk["nexpa"]])

        psb = [ps(f"psb{i}", [128, 512], F32) for i in range(7)]
        pst = [Tok(excl=True) for _ in range(7)]
        pstr = ps("pstr", [128, 8, 128], BF16)
        t_pstr = Tok(excl=True)

        with ExitStack() as ph:
            CW = 2308
            stg = [sb(f"wstg{i}", [128, CW], F32, ph) for i in range(3)]
            stb = [sb(f"wstb{i}", [128, CW], BF16, ph) for i in range(3)]
            tg = [Tok() for _ in range(3)]
            tb = [Tok() for _ in range(3)]
            ceng = ["dve", "act", "dve"]
            it = 0
            jobs = []
            for c in range(8):
                rs_ = slice(c * 128, (c + 1) * 128)
                for j in range(IN_W // CW):
                    jobs.append((win_d[rs_, j * CW:(j + 1) * CW], winb_d[rs_, j * CW:(j + 1) * CW], CW, None))
                for (s_, d_) in ((wbsb_d, wbsbb_d), (wbgdn_d, wbgdnb_d), (wout_d, woutb_d)):
                    jobs.append((s_[rs_, :], d_[rs_, :], 1024, None))
                jobs.append((wq_d[rs_, :], wqb_d[rs_, :], 2048, None))
            if stage in ("full", "peer"):
                for (s_, c0_) in ((pu_d, 0), (pv_d, D)):
                    for r in range(NEXP // 256):
                        jobs.append((s_[r * 256:(r + 1) * 256, :].rearrange("(t p) d -> p t d", p=128),
                                     puvb_d[r * 256:(r + 1) * 256, c0_:c0_ + D].rearrange("(t p) d -> p t d", p=128), 2048, 2))
            for (s_, d_, w, t3) in jobs:
                i = it % 3
                sv, bv = stg[i][:, 0:w], stb[i][:, 0:w]
                if t3:
                    sv3 = sv.rearrange("p (t d) -> p t d", t=t3)
                    bv3 = bv.rearrange("p (t d) -> p t d", t=t3)
                else:
                    sv3, bv3 = sv, bv
                P.dma("sp", sv3, s_, writes=[tg[i]])
                e = ceng[it % 3]
                if e == "act":
                    acopy(bv, sv, [tg[i]], [tb[i]])
                else:
                    vcopy(e, bv, sv, [tg[i]], [tb[i]])
                P.dma("act", d_, bv3, reads=[tb[i]])
                it += 1
            P.barrier()

        with ExitStack() as mx:
            hT = sb("hT", [128, 8, S], BF16, mx)
            t_hT = [Tok() for _ in range(NT)]
            osbT = sb("osbT", [128, NH, S], BF16, mx)
            t_osb = [[Tok() for _ in range(NG)] for _ in range(NH)]
            ogdnT = sb("ogdnT", [128, NH, S], BF16, mx)
            t_ogdn = [[Tok() for _ in range(NT)] for _ in range(NH)]

            for b in range(NB if stage != "peer" else 0):
                with ExitStack() as ph:
                    xin = [sb(f"xin{i}", [128, D], F32, ph) for i in range(2)]
                    xn = [sb(f"xn{i}", [128, D], BF16, ph) for i in range(2)]
                    junk = sb("junk", [128, D], F32, ph)
                    ss = [sb(f"ss{i}", [128, 1], F32, ph) for i in range(2)]
                    for t in range(NT):
                        i = t % 2
                        r0 = b * S + t * 128
                        P.dma("sp", xin[i][:], x_d[r0:r0 + 128, :], writes=[tk["xin", i]])
                        act(junk[:], xin[i][:], AF.Square, [tk["xin", i]], [tk["junk"], tk["ss", i]], accum_out=ss[i][:])
                        act(ss[i][:], ss[i][:], AF.Ln, [tk["ss", i]], [tk["ss", i]], scale=1.0 / D, bias=EPS)
                        act(ss[i][:], ss[i][:], AF.Exp, [tk["ss", i]], [tk["ss", i]], scale=-0.5)
                        stt("dve", xn[i][:], xin[i][:], ss[i][:, 0:1], mixw_b[:], ALU.mult, ALU.mult,
                            [tk["xin", i], tk["ss", i], t_mixw], [tk["xn", i]])
                        for c in range(8):
                            P.op("pe", lambda pe, i=i, c=c: pe.transpose(out=pstr[:, c, :], in_=xn[i][:, c * 128:(c + 1) * 128],
                                                                         identity=ident_b[:]),
                                 reads=[tk["xn", i], t_const], writes=[t_pstr])
                        acopy(hT[:, :, t * 128:(t + 1) * 128], pstr[:, :, :], [t_pstr], [t_hT[t]])
                    P.barrier()

                if "sb" not in skip:
                  with ExitStack() as ph:
                    m01 = [sb(f"m01_{d}", [128, 512], F32, ph) for d in range(4)]
                    mneg = [sb(f"mneg_{d}", [128, 512], F32, ph) for d in range(4)]
                    t_msk = Tok()
                    for d in range(4):
                        P.op("pool", lambda g, d=d: g.memset(m01[d][:], 1.0), writes=[t_msk])
                        P.op("pool", lambda g, d=d: g.affine_select(out=m01[d][:], in_=m01[d][:], pattern=[[1, 512]],
                                                                     compare_op=ALU.is_gt, fill=0.0, base=-128 * d,
                                                                     channel_multiplier=-1), reads=[t_msk], writes=[t_msk])
                        ts("pool", mneg[d][:], m01[d][:], -1.0, -NEG, ALU.add, ALU.mult, [t_msk], [t_msk])
                    ustrict_r = sb("ustrict_r", [128, 128], F32R, ph)
                    ones_r = sb("ones_r", [128, 128], F32R, ph)
                    vcopy("pool", ustrict_r[:], ustrict[:], [t_const, t_msk], [t_msk])
                    vcopy("pool", ones_r[:], ones_f[:], [t_const, t_msk], [t_msk])
                    wsb = [sb(f"wsb{i}", [128, 8, 384], BF16, ph) for i in range(2)]
                    t_wsb = [Tok() for _ in range(2)]
                    qTs_ = [sb(f"qT{i}", [128, S], BF16, ph) for i in range(2)]
                    kTs_ = [sb(f"kT{i}", [128, S], BF16, ph) for i in range(2)]
                    t_qTs = [[Tok() for _ in range(NG)] for _ in range(2)]
                    t_kTs = [[Tok() for _ in range(NG)] for _ in range(2)]
                    vtms_ = [sb(f"vtm{i}", [128, NT, 128], BF16, ph) for i in range(2)]
                    t_vs = [[Tok() for _ in range(NT)] for _ in range(2)]
                    sqb = [sb(f"sqb{i}", [128, 512], F32, ph) for i in range(2)]
                    t_sqb = [Tok() for _ in range(2)]
                    rsb = [sb(f"rsb{i}", [128, 512], F32, ph) for i in range(2)]
                    t_rsb = [Tok() for _ in range(2)]
                    NR = 5
                    eb = [sb(f"eb{i}", [128, 512], F32, ph) for i in range(NR)]
                    spb = [sb(f"spb{i}", [128, 512], F32R, ph) for i in range(NR)]
                    t1b = [sb(f"t1b{i}", [128, 512], F32, ph) for i in range(NR)]
                    aTb = [sb(f"aTb{i}", [128, 512], BF16, ph) for i in range(NR)]
                    t_eb = [Tok() for _ in range(NR)]
                    t_spb = [Tok() for _ in range(NR)]
                    t_t1b = [Tok() for _ in range(NR)]
                    t_aTb = [Tok() for _ in range(NR)]
                    lacc = [sb(f"lacc{i}", [128, 512], F32R, ph) for i in range(3)]
                    t_lacc = [Tok() for _ in range(3)]
                    def sb_prep(h):
                        wi = h % 2
                        qT_, kT_, vtm_ = qTs_[wi], kTs_[wi], vtms_[wi]
                        for j, off in enumerate((OFF_SBQ, OFF_SBK, OFF_SBV)):
                            src = winb_d[:, off + h * 128: off + (h + 1) * 128].rearrange("(c p) n -> p c n", p=128)
                            P.dma("sp", wsb[wi][:, :, j * 128:(j + 1) * 128], src, writes=[t_wsb[wi]])
                        for which, dst, tdst, wcol, twc in ((0, qT_, t_qTs[wi], sbqw, t_sbw), (1, kT_, t_kTs[wi], sbkw, tk["sbkw"])):
                            for g in range(NG):
                                bk = (which * NG + g) % 2
                                for c in range(8):
                                    mm(psb[5][:], wsb[wi][:, c, which * 128:(which + 1) * 128],
                                       hT[:, c, g * 512:(g + 1) * 512], [t_wsb[wi]] + t_hT[g * 4:(g + 1) * 4], pst[5],
                                       start=(c == 0), stop=(c == 7))
                                act(sqb[bk][:], psb[5][:], AF.Square, [pst[5]], [t_sqb[bk]])
                                mm(psb[6][:], ones_f[:], sqb[bk][:], [t_sqb[bk], t_const], pst[6])
                                act(rsb[bk][:], psb[6][:], AF.Ln, [pst[6]], [t_rsb[bk]], scale=1.0 / DH, bias=EPS)
                                act(rsb[bk][:], rsb[bk][:], AF.Exp, [t_rsb[bk]], [t_rsb[bk]], scale=-0.5)
                                stt("dve", dst[:, g * 512:(g + 1) * 512], psb[5][:], wcol[:, 0:1], rsb[bk][:],
                                    ALU.mult, ALU.mult, [pst[5], t_rsb[bk], twc], [tdst[g]])
                        for t in range(NT):
                            bk = 5 + (t % 2)
                            for c in range(8):
                                mm(psb[bk][:, 0:128], hT[:, c, t * 128:(t + 1) * 128], wsb[wi][:, c, 256:384],
                                   [t_wsb[wi], t_hT[t]], pst[bk], start=(c == 0), stop=(c == 7))
                            acopy(vtm_[:, t, :], psb[bk][:, 0:128], [pst[bk]], [t_vs[wi][t]])

                    def sb_record(h):
                        P.defq = []
                        sb_prep(h)
                        q_ = P.defq
                        P.defq = None
                        return q_

                    for fn_ in sb_record(0):
                        fn_()
                    for h in range(NH):
                        wi = h % 2
                        qT, kT, vtm = qTs_[wi], kTs_[wi], vtms_[wi]
                        t_qT, t_kT, t_v = t_qTs[wi], t_kTs[wi], t_vs[wi]
                        prepq = sb_record(h + 1) if h + 1 < NH else []
                        steps = []
                        for g in range(NG):
                            nkb = 4 * g + 4
                            for kb in range(nkb - 1, -1, -1):
                                steps.append((g, kb, kb == nkb - 1, kb == 0))

                        NS = len(steps)
                        sbanks = (0, 1)

                        def stA1(i):
                            g, kb, first, last = steps[i]
                            r = i % NR
                            sbk = sbanks[i % 2]
                            q0 = g * 512
                            mm(psb[sbk][:], kT[:, kb * 128:(kb + 1) * 128], qT[:, q0:q0 + 512],
                               [t_kT[kb // 4], t_qT[g]], pst[sbk])
                            act(eb[r][:], psb[sbk][:], AF.Exp, [pst[sbk]], [t_eb[r]])
                            act(spb[r][:], eb[r][:], AF.Ln, [t_eb[r]], [t_spb[r]], bias=1.0)

                        def stA2(i):
                            g, kb, first, last = steps[i]
                            r = i % NR
                            sbk = sbanks[i % 2]
                            tt("dve", t1b[r][:], psb[sbk][:], spb[r][:], ALU.subtract, [pst[sbk], t_spb[r]], [t_t1b[r]])
                            if kb >= 4 * g:
                                tt("pool", spb[r][:], spb[r][:], m01[kb - 4 * g][:], ALU.mult, [t_spb[r], t_msk], [t_spb[r]])
                            if not last:
                                lo, ln_ = i % 3, (i + 1) % 3
                                if first:
                                    vcopy("pool", lacc[ln_][:], spb[r][:], [t_spb[r]], [t_lacc[ln_]])
                                else:
                                    tt("pool", lacc[ln_][:], lacc[lo][:], spb[r][:], ALU.add, [t_spb[r], t_lacc[lo]], [t_lacc[ln_]])

                        def stB(i):
                            g, kb, first, last = steps[i]
                            r = i % NR
                            ubk = 2 + (i % 2)
                            mm(psb[ubk][:], ustrict_r[:], spb[r][:], [t_spb[r], t_msk], pst[ubk], start=True, stop=first)
                            if not first:
                                mm(psb[ubk][:], ones_r[:], lacc[i % 3][:], [t_lacc[i % 3], t_msk], pst[ubk], start=False, stop=True)
                            tt("dve", t1b[r][:], t1b[r][:], psb[ubk][:], ALU.subtract, [t_t1b[r], pst[ubk]], [t_t1b[r]])
                            if kb >= 4 * g:
                                tt("pool", t1b[r][:], t1b[r][:], mneg[kb - 4 * g][:], ALU.add, [t_t1b[r], t_msk], [t_t1b[r]])

                        def stC(i):
                            r = i % NR
                            act(aTb[r][:], t1b[r][:], AF.Exp, [t_t1b[r]], [t_aTb[r]])

                        def stD(i):
                            g, kb, first, last = steps[i]
                            r = i % NR
                            ob = 4
                            mm(psb[ob][:], vtm[:, kb, :], aTb[r][:], [t_aTb[r], t_v[kb]], pst[ob], start=first, stop=last)
                            if last:
                                acopy(osbT[:, h, g * 512:(g + 1) * 512], psb[ob][:], [pst[ob]], [t_osb[h][g]])

                        for n in range(-3, NS + 1):
                            for fn_, off in ((stA1, 3), (stA2, 2), (stB, 1), (stC, 0), (stD, -1)):
                                i = n + off
                                if 0 <= i < NS:
                                    fn_(i)
                            for _ in range(5):
                                if prepq:
                                    prepq.pop(0)()
                        while prepq:
                            prepq.pop(0)()
                    P.barrier()

                if "gdn" not in skip:
                  with ExitStack() as ph:
                    wab = sb("wab", [128, 8, 16], BF16, ph)
                    ab_sb = sb("ab_sb", [128, NT * 16], F32, ph)
                    sm = {n: sb("gs_" + n, [128, NT * 8], F32, ph) for n in ("tmp", "beta", "g", "gc", "ngc", "egc", "bg")}
                    t_gsm = Tok()
                    wg = sb("wg", [128, 8, 512], BF16, ph)
                    cst = sb("cst", [128, 3 + S], F32, ph)
                    cvo = sb("cvo", [128, S], F32, ph)
                    QT = sb("QT", [128, S], F32, ph)
                    KT = sb("KT", [128, S], F32, ph)
                    Ktm = sb("Ktm", [128, NT, 128], F32, ph)
                    Vtm = sb("Vtm", [128, NT, 128], F32, ph)
                    zs = sb("zs", [128, NT, 128], BF16, ph)
                    sq5 = sb("sq5", [128, 512], F32, ph)
                    rs5 = sb("rs5", [128, 512], F32, ph)
                    ztmp = sb("ztmp", [128, 128], F32, ph)
                    Sst = sb("Sst", [128, 128], F32, ph)
                    junk1 = sb("junk1", [128, 128], F32, ph)
                    names = ("dg", "tmpa", "tmpb", "dec", "decT", "M0", "M1", "MT0", "MT1", "PT", "attnT", "Vb", "Kbg",
                             "kw", "vcorr", "kcdT", "vnew", "o1", "o")
                    cb = {n: [sb(f"c_{n}{u}", [128, 128], F32, ph) for u in range(4 if n in ("attnT", "kw", "vcorr", "kcdT") else 2)]
                          for n in names}
                    og = [sb(f"c_og{u}", [128, 128], BF16, ph) for u in range(2)]
                    col = {n: [sb(f"k_{n}{u}", [128, 1], F32, ph) for u in range(4)] for n in ("glc", "egl", "ew", "oss")}
                    sbanks_g = ((0, 1), (2, 3), (4, 5, 6))
                    sidx = [0, 0, 0]

                    def pslot(st=2):
                        bi = sbanks_g[st][sidx[st] % len(sbanks_g[st])]
                        sidx[st] += 1
                        return psb[bi][:, 0:128], pst[bi]

                    def pbank(st=2):
                        bi = sbanks_g[st][sidx[st] % len(sbanks_g[st])]
                        sidx[st] += 1
                        return psb[bi], pst[bi]

                    def ck(k):
                        if stop == k:
                            P.dead = True

                    P.op("pool", lambda g: g.memset(cst[:, 0:3], 0.0), writes=[tk["cst"]])
                    P.dma("sp", wab[:], winb_d[:, OFF_GB:OFF_GB + 16].rearrange("(c p) n -> p c n", p=128), writes=[tk["wab"]])
                    for t in range(NT):
                        for c in range(8):
                            mm(psb[6][:, t * 16:(t + 1) * 16], hT[:, c, t * 128:(t + 1) * 128], wab[:, c, :],
                               [tk["wab"], t_hT[t]], pst[6], start=(c == 0), stop=(c == 7))
                    acopy(ab_sb[:], psb[6][:, 0:NT * 16], [pst[6]], [tk["ab"]])
                    ab3 = ab_sb[:].rearrange("p (t k) -> p t k", k=16)
                    v3 = lambda n: sm[n][:].rearrange("p (t k) -> p t k", k=8)
                    act(v3("tmp"), ab3[:, :, 0:8], AF.Exp, [tk["ab"]], [t_gsm], scale=-1.0)
                    ts("dve", sm["tmp"][:], sm["tmp"][:], 1.0, None, ALU.add, None, [t_gsm], [t_gsm])
                    P.op("dve", lambda v: v.reciprocal(out=sm["beta"][:], in_=sm["tmp"][:]), reads=[t_gsm], writes=[t_gsm])
                    tt("dve", v3("g"), ab3[:, :, 8:16], dtb[:].rearrange("p (t k) -> p t k", k=8), ALU.add,
                       [tk["ab"], tk["dtb"]], [t_gsm])
                    act(sm["g"][:], sm["g"][:], AF.Exp, [t_gsm], [t_gsm])
                    act(sm["g"][:], sm["g"][:], AF.Ln, [t_gsm], [t_gsm], bias=1.0)
                    tt("dve", sm["g"][:], sm["g"][:], nexpa[:], ALU.mult, [t_gsm, tk["nexpa"]], [t_gsm])
                    for t in range(NT):
                        mm(psb[6][:, 256 + t * 8:256 + (t + 1) * 8], tri_incl[:], sm["g"][:, t * 8:(t + 1) * 8],
                           [t_gsm, t_const], pst[6])
                    acopy(sm["gc"][:], psb[6][:, 256:256 + NT * 8], [pst[6]], [t_gsm])
                    ts("dve", sm["ngc"][:], sm["gc"][:], -1.0, None, ALU.mult, None, [t_gsm], [t_gsm])
                    act(sm["egc"][:], sm["gc"][:], AF.Exp, [t_gsm], [t_gsm])
                    tt("dve", sm["bg"][:], sm["beta"][:], sm["egc"][:], ALU.mult, [t_gsm], [t_gsm])

                    ck(1)
                    for h in range(NH):
                        for j, off in enumerate((OFF_GQ, OFF_GK, OFF_GV, OFF_GZ)):
                            src = winb_d[:, off + h * 128: off + (h + 1) * 128].rearrange("(c p) n -> p c n", p=128)
                            P.dma("sp", wg[:, :, j * 128:(j + 1) * 128], src, writes=[tk["wg"]])
                        for t in range(NT):
                            for c in range(8):
                                mm(psb[6][:, 0:128], hT[:, c, t * 128:(t + 1) * 128], wg[:, c, 384:512],
                                   [tk["wg"], t_hT[t]], pst[6], start=(c == 0), stop=(c == 7))
                            act(ztmp[:], psb[6][:, 0:128], AF.Silu, [pst[6]], [tk["ztmp"]])
                            tt("pool", zs[:, t, :], ztmp[:], gnw_b[:], ALU.mult, [tk["ztmp"], tk["gnw"]], [tk["zs", t]])
                        ck(2)
                        for which in range(3):
                            for g in range(NG):
                                bk = 5 + (g % 2)
                                for c in range(8):
                                    mm(psb[bk][:], wg[:, c, which * 128:(which + 1) * 128], hT[:, c, g * 512:(g + 1) * 512],
                                       [tk["wg"]] + t_hT[g * 4:(g + 1) * 4], pst[bk], start=(c == 0), stop=(c == 7))
                                acopy(cst[:, 3 + g * 512:3 + (g + 1) * 512], psb[bk][:], [pst[bk]], [tk["cst"]])
                            wc = lambda i: convw[:, (which * 8 + h) * 4 + i:(which * 8 + h) * 4 + i + 1]
                            ts("dve", cvo[:], cst[:, 3:3 + S], wc(3), None, ALU.mult, None, [tk["cst"], tk["convw"]], [tk["cvo"]])
                            for i in range(3):
                                stt("dve", cvo[:], cst[:, i:i + S], wc(i), cvo[:], ALU.mult, ALU.add,
                                    [tk["cst"], tk["convw"], tk["cvo"]], [tk["cvo"]])
                            act(cvo[:], cvo[:], AF.Silu, [tk["cvo"]], [tk["cvo"]])
                            if which < 2:
                                dst = QT if which == 0 else KT
                                for g in range(NG):
                                    gs = slice(g * 512, (g + 1) * 512)
                                    act(sq5[:], cvo[:, gs], AF.Square, [tk["cvo"]], [tk["sq5"]])
                                    mm(psb[5][:], ones_f[:], sq5[:], [tk["sq5"], t_const], pst[5])
                                    act(rs5[:], psb[5][:], AF.Ln, [pst[5]], [tk["rs5"]], bias=EPS)
                                    act(rs5[:], rs5[:], AF.Exp, [tk["rs5"]], [tk["rs5"]], scale=-0.5)
                                    if which == 0:
                                        stt("dve", dst[:, gs], cvo[:, gs], float(DH) ** -0.5, rs5[:], ALU.mult, ALU.mult,
                                            [tk["cvo"], tk["rs5"]], [tk["QT", g]])
                                    else:
                                        tt("dve", dst[:, gs], cvo[:, gs], rs5[:], ALU.mult, [tk["cvo"], tk["rs5"]], [tk["KT", g]])
                            else:
                                for t in range(NT):
                                    sl, ts_ = pslot()
                                    P.op("pe", lambda pe, sl=sl, t=t: pe.transpose(out=sl, in_=cvo[:, t * 128:(t + 1) * 128],
                                                                                 identity=ident_f[:]),
                                         reads=[tk["cvo"], t_const], writes=[ts_])
                                    acopy(Vtm[:, t, :], sl, [ts_], [tk["Vtm", t]])
                        ck(3)
                        for t in range(NT):
                            sl, ts_ = pslot()
                            P.op("pe", lambda pe, sl=sl, t=t: pe.transpose(out=sl, in_=KT[:, t * 128:(t + 1) * 128],
                                                                         identity=ident_f[:]),
                                 reads=[tk["KT", t // 4], t_const], writes=[ts_])
                            vcopy("dve", Ktm[:, t, :], sl, [ts_], [tk["Ktm", t]])
                        ck(4)
                        P.op("pool", lambda g: g.memset(Sst[:], 0.0), writes=[tk["S"]])

                        def chunk_env(t):
                            ci = t * 8 + h
                            e = dict(cs=slice(t * 128, (t + 1) * 128),
                                     gcc=sm["gc"][:, ci:ci + 1], ngcc=sm["ngc"][:, ci:ci + 1], betac=sm["beta"][:, ci:ci + 1],
                                     egcc=sm["egc"][:, ci:ci + 1], bgc=sm["bg"][:, ci:ci + 1],
                                     tQ=tk["QT", t // 4], tK=tk["KT", t // 4])
                            return e

                        HAND = ("attnT", "kw", "vcorr", "kcdT")

                        def tcomp(t):
                            e = chunk_env(t)
                            cs, gcc, ngcc, betac, bgc, tQ, tK = e["cs"], e["gcc"], e["ngcc"], e["betac"], e["bgc"], e["tQ"], e["tK"]
                            u = t % 2
                            u4 = t % 4
                            st = t % 2
                            B = lambda n: cb[n][u4 if n in HAND else u][:]
                            T = lambda n: tk[n, u4 if n in HAND else u]
                            egl_c, ew_c, glc_c = col["egl"][u4], col["ew"][u], col["glc"][u]
                            ts("dve", B("dg"), ident_f[:], gcc, None, ALU.mult, None, [t_gsm, t_const], [T("dg")])
                            gr, t_gr = pslot(st)
                            mm(gr, ones_f[:], B("dg"), [T("dg"), t_const], t_gr)
                            acopy(glc_c[:], gr[:, 127:128], [t_gr], [T("glc")])
                            act(egl_c[:], glc_c[:], AF.Exp, [T("glc")], [tk["egl", u4]])
                            act(ew_c[:], ngcc, AF.Exp, [T("glc"), t_gsm], [T("ew")], bias=glc_c[:, 0:1])
                            stt("dve", B("tmpa"), gr, -1.0, negstrict[:], ALU.mult, ALU.add, [t_gr, t_const], [T("tmpa")])
                            act(B("dec"), B("tmpa"), AF.Exp, [T("tmpa"), t_gsm], [T("dec")], bias=gcc)
                            tt("dve", B("tmpb"), gr, negtriu[:], ALU.add, [t_gr, t_const], [T("tmpb")])
                            act(B("decT"), B("tmpb"), AF.Exp, [T("tmpb"), t_gsm], [T("decT")], bias=ngcc)
                            kk, t_kk = pslot(st)
                            mm(kk, KT[:, cs], KT[:, cs], [tK], t_kk)
                            stt("dve", B("M0"), kk, betac, B("dec"), ALU.mult, ALU.mult, [t_kk, T("dec"), t_gsm], [T("M0")])
                            qk, t_qk = pslot(st)
                            mm(qk, KT[:, cs], QT[:, cs], [tK, tQ], t_qk)
                            tt("dve", B("attnT"), qk, B("decT"), ALU.mult, [t_qk, T("decT")], [T("attnT")])
                            lt, t_lt = pslot(st)
                            P.op("pe", lambda pe, lt=lt, u=u: pe.transpose(out=lt, in_=cb["M0"][u][:], identity=ident_f[:]),
                                 reads=[T("M0"), t_const], writes=[t_lt])
                            acopy(B("MT0"), lt, [t_lt], [T("MT0")])
                            tt("dve", B("PT"), ident_f[:], lt, ALU.subtract, [t_lt, t_const], [T("PT")])
                            cur = 0
                            for k in range(6):
                                nx = 1 - cur
                                Mc, MTc, Mn, MTn = f"M{cur}", f"MT{cur}", f"M{nx}", f"MT{nx}"
                                m2, t_m2 = pslot(st)
                                mm(m2, B(MTc), B(Mc), [T(MTc), T(Mc)], t_m2)
                                if k < 5:
                                    m2t, t_m2t = pslot(st)
                                    mm(m2t, B(Mc), B(MTc), [T(MTc), T(Mc)], t_m2t)
                                acopy(B(Mn), m2, [t_m2], [T(Mn)])
                                if k < 5:
                                    vcopy("dve", B(MTn), m2t, [t_m2t], [T(MTn)])
                                pp, t_pp = pslot(st)
                                mm(pp, B(Mn), B("PT"), [T(Mn), T("PT")], t_pp)
                                tt("dve", B("PT"), B("PT"), pp, ALU.add, [T("PT"), t_pp], [T("PT")])
                                cur = nx
                            ts("pool", B("Vb"), Vtm[:, t, :], betac, None, ALU.mult, None, [tk["Vtm", t], t_gsm], [T("Vb")])
                            ts("pool", B("Kbg"), Ktm[:, t, :], bgc, None, ALU.mult, None, [tk["Ktm", t], t_gsm], [T("Kbg")])
                            ts("pool", B("kw"), Ktm[:, t, :], ew_c[:, 0:1], None, ALU.mult, None,
                               [tk["Ktm", t], T("ew")], [T("kw")])
                            vc, t_vc = pslot(st)
                            mm(vc, B("PT"), B("Vb"), [T("PT"), T("Vb")], t_vc)
                            acopy(B("vcorr"), vc, [t_vc], [T("vcorr")])
                            kc, t_kc = pslot(st)
                            mm(kc, B("Kbg"), B("PT"), [T("PT"), T("Kbg")], t_kc)
                            vcopy("dve", B("kcdT"), kc, [t_kc], [T("kcdT")])

                        def rec(t):
                            e = chunk_env(t)
                            cs, egcc, tQ = e["cs"], e["egcc"], e["tQ"]
                            u = t % 2
                            u4 = t % 4
                            B = lambda n: cb[n][u4 if n in HAND else u][:]
                            T = lambda n: tk[n, u4 if n in HAND else u]
                            vn, t_vn = pslot(2)
                            mm(vn, B("kcdT"), Sst[:], [T("kcdT"), tk["S"]], t_vn)
                            tt("dve", B("vnew"), B("vcorr"), vn, ALU.subtract, [T("vcorr"), t_vn], [T("vnew")])
                            o1p, t_o1p = pslot(2)
                            mm(o1p, QT[:, cs], Sst[:], [tQ, tk["S"]], t_o1p)
                            act(B("o1"), o1p, AF.Identity, [t_o1p, t_gsm], [T("o1")], scale=egcc)
                            sup, t_sup = pslot(2)
                            mm(sup, B("kw"), B("vnew"), [T("kw"), T("vnew")], t_sup)
                            stt("dve", Sst[:], Sst[:], col["egl"][u4][:, 0:1], sup, ALU.mult, ALU.add,
                                [tk["S"], tk["egl", u4], t_sup], [tk["S"]])
                            o2p, t_o2p = pslot(2)
                            mm(o2p, B("attnT"), B("vnew"), [T("attnT"), T("vnew")], t_o2p)
                            tt("dve", B("o"), B("o1"), o2p, ALU.add, [T("o1"), t_o2p], [T("o")])
                            act(junk1[:], B("o"), AF.Square, [T("o")], [tk["junk1"], T("oss")], accum_out=col["oss"][u][:])
                            act(col["oss"][u][:], col["oss"][u][:], AF.Ln, [T("oss")], [T("oss")], scale=1.0 / DH, bias=EPS)
                            act(col["oss"][u][:], col["oss"][u][:], AF.Exp, [T("oss")], [T("oss")], scale=-0.5)
                            stt("dve", og[u][:], B("o"), col["oss"][u][:, 0:1], zs[:, t, :], ALU.mult, ALU.mult,
                                [T("o"), T("oss"), tk["zs", t]], [T("og")])
                            P.op("pe", lambda pe, u=u: pe.transpose(out=pstr[:, u, :], in_=og[u][:], identity=ident_b[:]),
                                 reads=[T("og"), t_const], writes=[t_pstr])
                            acopy(ogdnT[:, h, cs], pstr[:, u, :], [t_pstr], [t_ogdn[h][t]])

                        def record(fn_, *a_):
                            P.defq = []
                            fn_(*a_)
                            q_ = P.defq
                            P.defq = None
                            return q_

                        def zipdrain(qs, ws):
                            while any(qs):
                                for q_, w_ in zip(qs, ws):
                                    for _ in range(w_):
                                        if q_:
                                            q_.pop(0)()

                        zipdrain([record(tcomp, 0), record(tcomp, 1)], [1, 1])
                        for p_ in range(NT // 2):
                            qa = record(tcomp, 2 * p_ + 2) if 2 * p_ + 2 < NT else []
                            qb = record(tcomp, 2 * p_ + 3) if 2 * p_ + 3 < NT else []
                            qc = record(rec, 2 * p_) + record(rec, 2 * p_ + 1)
                            zipdrain([qa, qb, qc], [2, 2, 1])
                    P.dead = False
                    P.barrier()

                if stage in ("sb", "gdn"):
                    with ExitStack() as ph:
                        dbgo = sb("dbgo", [128, 512], F32, ph)
                        src_t = osbT if stage == "sb" else ogdnT
                        for h in range(NH):
                            for g in range(NG):
                                rd = [t_osb[h][g]] if stage == "sb" else t_ogdn[h][g * 4:(g + 1) * 4]
                                vcopy("dve", dbgo[:], src_t[:, h, g * 512:(g + 1) * 512], rd, [tk["dbgo"]])
                                P.dma("sp", dbg_d[b, h * 128:(h + 1) * 128, g * 512:(g + 1) * 512], dbgo[:], reads=[tk["dbgo"]])
                        P.barrier()
                    continue

                with ExitStack() as ph:
                    wout = sb("wout", [128, 8, D], BF16, ph)
                    P.dma("sp", wout[:], woutb_d.rearrange("(c p) n -> p c n", p=128), writes=[tk["wout"]])
                    wbs = [sb(f"wbs{i}", [128, 8, 128], BF16, ph) for i in range(2)]
                    wbg = [sb(f"wbg{i}", [128, 8, 128], BF16, ph) for i in range(2)]
                    wgt = [sb(f"wgt{i}", [128, 8, 256], BF16, ph) for i in range(2)]
                    mT = [sb(f"mT{i}", [128, 8, 512], BF16, ph) for i in range(2)]
                    e1 = [sb(f"e1_{i}", [128, 512], F32, ph) for i in range(2)]
                    e2 = [sb(f"e2_{i}", [128, 512], F32, ph) for i in range(2)]
                    xr = [sb(f"xr{i}", [128, D], F32, ph) for i in range(2)]
                    x1t = [sb(f"x1t{i}", [128, D], F32, ph) for i in range(2)]
                    it = 0
                    for g in range(NG):
                        gs = slice(g * 512, (g + 1) * 512)
                        gi = g % 2
                        for m in range(8):
                            w = it % 2
                            it += 1
                            ms = slice(m * 128, (m + 1) * 128)
                            P.dma("sp", wbs[w][:], wbsbb_d[:, ms].rearrange("(c p) n -> p c n", p=128), writes=[tk["wbs", w]])
                            P.dma("sp", wbg[w][:], wbgdnb_d[:, ms].rearrange("(c p) n -> p c n", p=128), writes=[tk["wbg", w]])
                            P.dma("sp", wgt[w][:, :, 0:128],
                                  winb_d[:, OFF_GSB + m * 128:OFF_GSB + (m + 1) * 128].rearrange("(c p) n -> p c n", p=128),
                                  writes=[tk["wgt", w]])
                            P.dma("sp", wgt[w][:, :, 128:256],
                                  winb_d[:, OFF_GGDN + m * 128:OFF_GGDN + (m + 1) * 128].rearrange("(c p) n -> p c n", p=128),
                                  writes=[tk["wgt", w]])
                            for c in range(8):
                                mm(psb[0][:], wbs[w][:, c, :], osbT[:, c, gs], [tk["wbs", w], t_osb[c][g]], pst[0],
                                   start=(c == 0), stop=(c == 7))
                            for c in range(8):
                                mm(psb[1][:], wbg[w][:, c, :], ogdnT[:, c, gs], [tk["wbg", w]] + t_ogdn[c][g * 4:(g + 1) * 4],
                                   pst[1], start=(c == 0), stop=(c == 7))
                            for c in range(8):
                                mm(psb[2][:], wgt[w][:, c, 0:128], hT[:, c, gs], [tk["wgt", w]] + t_hT[g * 4:(g + 1) * 4],
                                   pst[2], start=(c == 0), stop=(c == 7))
                            for c in range(8):
                                mm(psb[3][:], wgt[w][:, c, 128:256], hT[:, c, gs], [tk["wgt", w]] + t_hT[g * 4:(g + 1) * 4],
                                   pst[3], start=(c == 0), stop=(c == 7))
                            act(e1[w][:], psb[2][:], AF.Sigmoid, [pst[2]], [tk["e1", w]])
                            act(e2[w][:], psb[3][:], AF.Sigmoid, [pst[3]], [tk["e2", w]])
                            tt("dve", e1[w][:], e1[w][:], psb[0][:], ALU.mult, [tk["e1", w], pst[0]], [tk["e1", w]])
                            tt("dve", e2[w][:], e2[w][:], psb[1][:], ALU.mult, [tk["e2", w], pst[1]], [tk["e2", w]])
                            tt("pool", mT[gi][:, m, :], e1[w][:], e2[w][:], ALU.add, [tk["e1", w], tk["e2", w]], [tk["mT", gi]])
                        for tt_ in range(4):
                            t = g * 4 + tt_
                            xi = t % 2
                            r0 = b * S + t * 128
                            P.dma("sp", xr[xi][:], x_d[r0:r0 + 128, :], writes=[tk["xr", xi]])
                            for n in range(2):
                                bk = 4 + n
                                for m in range(8):
                                    mm(psb[bk][:], mT[gi][:, m, tt_ * 128:(tt_ + 1) * 128], wout[:, m, n * 512:(n + 1) * 512],
                                       [tk["mT", gi], tk["wout"]], pst[bk], start=(m == 0), stop=(m == 7))
                                tt("dve", x1t[xi][:, n * 512:(n + 1) * 512], xr[xi][:, n * 512:(n + 1) * 512], psb[bk][:], ALU.add,
                                   [tk["xr", xi], pst[bk]], [tk["x1t", xi]])
                            dst = out_d if stage == "mix" else x1_d
                            P.dma("sp", dst[r0:r0 + 128, :], x1t[xi][:], reads=[tk["x1t", xi]])
                    P.barrier()
            P.barrier()
        P.barrier()

        if stage == "peer":
            dbg = {n: nc.dram_tensor("dbg_" + n, [128, w], F32, kind="ExternalOutput").ap()
                   for n, w in (("esel", 128), ("gate", 128), ("v16", 256), ("i16f", 256))}
            dbg["nogather"] = "gather" in skip
            dbg["nodot"] = "nodot" in skip
            peer_phase(nc, P, tk, sb, ps, psb, pst, pstr, t_pstr, ident_b, ident_f, t_const, TOK,
                       x_d, out_d, ffnw_d, wqb_d, keysT_d, puvb_d, None, mm, act, acopy, vcopy, tt, ts, stt,
                       NTT=([int(k[3:]) for k in skip if k.startswith("ntt")] or [2])[0], dbg=dbg)
            P.barrier()
        if stage == "full":
            peer_phase(nc, P, tk, sb, ps, psb, pst, pstr, t_pstr, ident_b, ident_f, t_const, TOK,
                       x1_d, out_d, ffnw_d, wqb_d, keysT_d, puvb_d, None, mm, act, acopy, vcopy, tt, ts, stt)
            P.barrier()
        print("ninst", P.ninst, "nsem", P.nsem)
    return nc


def _prep_shared(inputs, NT):
    f = lambda k: np.ascontiguousarray(np.asarray(inputs[k], dtype=np.float32))
    cw = f("gdn_conv_w")[0]
    convw = cw.reshape(4, 3, 8, 128).transpose(3, 1, 2, 0).reshape(128, 96)
    k1 = f("peer_keys1")[0]
    k2 = f("peer_keys2")[0]
    keysT = np.stack([k1, k2], axis=1)
    keysT = keysT.transpose(3, 0, 1, 2).reshape(128, 16 * 128)
    return {
        "mix_norm_w": f("mix_norm_w").reshape(1, D),
        "ffn_norm_w": f("ffn_norm_w").reshape(1, D),
        "w_in": f("w_in")[0],
        "sb_q_norm_w": f("sb_q_norm_w").reshape(DH, 1),
        "sb_k_norm_w": f("sb_k_norm_w").reshape(DH, 1),
        "convw": np.ascontiguousarray(convw),
        "a_log_rep": np.ascontiguousarray(np.tile(f("gdn_a_log").reshape(1, 8), (1, NT))),
        "dtb_rep": np.ascontiguousarray(np.tile(f("gdn_dt_bias").reshape(1, 8), (1, NT))),
        "gdn_out_norm_w": f("gdn_out_norm_w").reshape(1, DH),
        "w_branch_sb": f("w_branch_sb")[0],
        "w_branch_gdn": f("w_branch_gdn")[0],
        "w_out": f("w_out")[0],
        "peer_w_q": f("peer_w_q")[0],
        "keysT": np.ascontiguousarray(keysT),
        "peer_u": f("peer_u")[0],
        "peer_v": f("peer_v")[0],
    }


def kernel(**inputs):
    x = np.asarray(inputs["x"], dtype=np.float32)
    B, S, _ = x.shape
    n = 8
    NB = B // n
    nc = build(NB=NB, S=S, stage="full")
    shared = _prep_shared(inputs, S // 128)
    in_maps = []
    for i in range(n):
        m = dict(shared)
        m["x"] = np.ascontiguousarray(x[i * NB:(i + 1) * NB].reshape(NB * S, D))
        in_maps.append(m)
    res = run_bass_kernel_spmd(nc, in_maps, core_ids=list(range(n)))
    outs = [np.asarray(r["out"]).reshape(NB, S, D) for r in res.results]
    return np.concatenate(outs, axis=0).astype(np.float32)
```
